# Optimizing a Trainium2 kernel written in Bass

```python
import math
import jax, jax.numpy as jnp
from jax import lax
import numpy as np

D_MODEL = 1024
BATCH = 8
SEQ = 2048
DEPTH = 2

N_EVEN = (DEPTH + 1) // 2
N_ODD = DEPTH // 2
NORM_EPS = 1e-6

MIX_WIDTH = D_MODEL
A_WIDTH = D_MODEL // 2
A_HEAD_DIM = 128
A_HEADS = A_WIDTH // A_HEAD_DIM
A_CHUNK = 32
B_WIDTH = MIX_WIDTH - A_WIDTH
B_HEAD_DIM = 64
B_HEADS = B_WIDTH // B_HEAD_DIM
B_DECAY_LORA = 64
B_AAA_LORA = 64
B_GATE_LORA = 128
B_LN_EPS = 1e-5 * B_HEAD_DIM
A_SIZES = [A_WIDTH] * 5
B_SIZES = [B_WIDTH] * 3 + [B_DECAY_LORA, B_DECAY_LORA, B_AAA_LORA, B_GATE_LORA]
A_COLS = sum(A_SIZES)
B_COLS = sum(B_SIZES)
AB_COLS = A_COLS + B_COLS

C_HEADS = 8
C_HEAD_DIM = 64
C_QK_WIDTH = C_HEADS * 2 * C_HEAD_DIM
C_V_WIDTH = C_HEADS * 2 * C_HEAD_DIM
C_QKV_COLS = 2 * C_QK_WIDTH + C_V_WIDTH
Q_BLOCK = 128
REL_BUCKETS = 32
REL_MAX_DISTANCE = 128

N_EXPERTS = 32
TOP_K = 4
D_EXPERT = D_MODEL
SWIGLU_LIMIT = 7.0
SWIGLU_ALPHA = 1.702

kernel_name = 'hybrid_hgrn2_rwkv7_diffattn_moe_encoder'


def _split(t, sizes):
    return jnp.split(t, [int(v) for v in np.cumsum(sizes)[:-1]], axis=-1)


def _rmsnorm(x, g, eps=NORM_EPS):
    xf = x.astype(jnp.float32)
    y = xf * lax.rsqrt(jnp.mean(xf * xf, axis=-1, keepdims=True) + eps)
    return (y * g.astype(jnp.float32)).astype(x.dtype)


def _modulate(x, g, shift, scale):
    return _rmsnorm(x, g) * (1.0 + scale) + shift


def _centred_shift(t):
    prev = jnp.pad(t[:, :-1], ((0, 0), (1, 0), (0, 0)))
    nxt = jnp.pad(t[:, 1:], ((0, 0), (0, 1), (0, 0)))
    return 0.5 * (prev + nxt)


def _gla_chunked(q, k, v, log_f):
    B_, S_, H_, dk = q.shape
    dv = v.shape[-1]
    n = S_ // A_CHUNK
    rs = lambda t: t.reshape(B_, n, A_CHUNK, H_, t.shape[-1])
    q, k, v, log_f = rs(q), rs(k), rs(v), rs(log_f)
    b = jnp.cumsum(log_f, axis=2)
    b_last = b[:, :, -1:]
    q_in = q * jnp.exp(b)
    k_in = k * jnp.exp(-b)
    k_st = k * jnp.exp(b_last - b)
    scores = jnp.einsum('bnthk,bnshk->bnhts', q_in, k_in)
    mask = jnp.tril(jnp.ones((A_CHUNK, A_CHUNK), dtype=bool))
    o_intra = jnp.einsum('bnhts,bnshv->bnthv', jnp.where(mask, scores, 0.0), v)
    d_state = jnp.einsum('bnshk,bnshv->bnhkv', k_st, v)
    chunk_decay = jnp.exp(b_last[:, :, 0])

    def step(state, inp):
        ds, dec = inp
        return dec[..., None] * state + ds, state

    s0 = jnp.zeros((B_, H_, dk, dv), jnp.float32)
    _, s_in = lax.scan(step, s0, (jnp.moveaxis(d_state, 1, 0), jnp.moveaxis(chunk_decay, 1, 0)))
    s_in = jnp.moveaxis(s_in, 0, 1)
    o_inter = jnp.einsum('bnthk,bnhkv->bnthv', q_in, s_in)
    return (o_intra + o_inter).reshape(B_, S_, H_, dv)


def _hgrn2_mix(p_a, lb, norm_g):
    B_, S_, _ = p_a.shape
    q, f_fwd, f_bwd, i, g = _split(p_a.astype(jnp.float32), A_SIZES)
    q = jax.nn.silu(q)
    heads = lambda t: t.reshape(B_, S_, A_HEADS, A_HEAD_DIM)

    def direction(f_logit, lb_dir, rev):
        fg = lb_dir + (1.0 - lb_dir) * jax.nn.sigmoid(f_logit)
        args = [heads(t) for t in (q, 1.0 - fg, i, jnp.log(fg))]
        if rev:
            args = [jnp.flip(t, 1) for t in args]
        o = _gla_chunked(*args)
        return jnp.flip(o, 1) if rev else o

    o = direction(f_fwd, lb[0], False) + direction(f_bwd, lb[1], True)
    o = _rmsnorm(o, norm_g) * jax.nn.silu(heads(g))
    return o.reshape(B_, S_, A_WIDTH)


def _rwkv7_mix(p_b, w0, w2, a0, a2, g2, k_k, k_a, r_k, ln_g, ln_b):
    B_, S_, _ = p_b.shape
    r, k, v, wlo_f, wlo_b, alo, glo = _split(p_b.astype(jnp.float32), B_SIZES)
    wlo = jnp.stack([wlo_f, wlo_b], 0)
    w = -jax.nn.softplus(-(w0[:, None, None, :] + jnp.einsum('nbsl,nlc->nbsc', jnp.tanh(wlo), w2))) - 0.5
    decay = jnp.exp(-jnp.exp(w))
    a = jax.nn.sigmoid(a0 + alo @ a2)
    g = jax.nn.sigmoid(glo) @ g2
    heads = lambda t: t.reshape(B_, S_, B_HEADS, B_HEAD_DIM)
    kk = heads(k * k_k)
    kk = kk / jnp.maximum(jnp.sqrt(jnp.sum(kk * kk, axis=-1, keepdims=True)), 1e-12)
    k = k * (1.0 + (a - 1.0) * k_a)
    r, k, v, a = heads(r), heads(k), heads(v), heads(a)
    decay = decay.reshape(2, B_, S_, B_HEADS, B_HEAD_DIM)

    both = lambda t: jnp.stack([t, jnp.flip(t, 1)], 0)
    time_major = lambda t: jnp.moveaxis(t, 2, 0)
    w_dir = jnp.stack([decay[0], jnp.flip(decay[1], 1)], 0)
    xs = (time_major(both(r)), time_major(w_dir), time_major(both(k)),
          time_major(both(v)), time_major(both(kk)), time_major(both(kk * a)))

    def step(state, inp):
        r_t, w_t, k_t, v_t, kk_t, b_t = inp
        sa = jnp.einsum('...vk,...k->...v', state, -kk_t)
        state = (state * w_t[..., None, :] + sa[..., :, None] * b_t[..., None, :]
                 + v_t[..., :, None] * k_t[..., None, :])
        return state, jnp.einsum('...vk,...k->...v', state, r_t)

    s0 = jnp.zeros((2, B_, B_HEADS, B_HEAD_DIM, B_HEAD_DIM), jnp.float32)
    _, y = lax.scan(step, s0, xs)
    y = jnp.moveaxis(y, 0, 2)
    y = y[0] + jnp.flip(y[1], 1)
    mean = jnp.mean(y, axis=-1, keepdims=True)
    var = jnp.mean(jnp.square(y - mean), axis=-1, keepdims=True)
    yn = (y - mean) * lax.rsqrt(var + B_LN_EPS)
    yn = yn * ln_g.reshape(B_HEADS, B_HEAD_DIM) + ln_b.reshape(B_HEADS, B_HEAD_DIM)
    bonus = jnp.sum(r * k * r_k.reshape(B_HEADS, B_HEAD_DIM), axis=-1, keepdims=True) * v
    return (yn + bonus).reshape(B_, S_, B_WIDTH) * g


def _ab_mixer(h, w_in, w_out, lb, hgrn_norm_g, mu, w0, w2, a0, a2, g2, k_k, k_a, r_k, ln_g, ln_b):
    p = h @ w_in
    p_a, p_b = p[..., :A_COLS], p[..., A_COLS:]
    p_b = p_b + mu * (_centred_shift(p_b) - p_b)
    y = jnp.concatenate([_hgrn2_mix(p_a, lb, hgrn_norm_g),
                         _rwkv7_mix(p_b, w0, w2, a0, a2, g2, k_k, k_a, r_k, ln_g, ln_b)], axis=-1)
    return y @ w_out


def _t5_bucket(rel):
    nb = REL_BUCKETS // 2
    max_exact = nb // 2
    bucket = jnp.where(rel > 0, nb, 0)
    n = jnp.abs(rel)
    nf = jnp.maximum(n, 1).astype(jnp.float32)
    large = max_exact + (jnp.log(nf / max_exact) / math.log(REL_MAX_DISTANCE / max_exact)
                         * (nb - max_exact)).astype(jnp.int32)
    large = jnp.minimum(large, nb - 1)
    return bucket + jnp.where(n < max_exact, n, large)


def _diff_attention(h, w_in, w_out, lam, subln_g, rel_table, layer_idx):
    B_, S_, _ = h.shape
    qkv = h @ w_in
    q, k, v = _split(qkv, [C_QK_WIDTH, C_QK_WIDTH, C_V_WIDTH])
    q = q.reshape(B_, S_, C_HEADS, 2, C_HEAD_DIM)
    k = k.reshape(B_, S_, C_HEADS, 2, C_HEAD_DIM)
    v = v.reshape(B_, S_, C_HEADS, 2 * C_HEAD_DIM)
    lam = lam.astype(jnp.float32)
    lam_init = 0.8 - 0.6 * math.exp(-0.3 * layer_idx)
    lam_full = jnp.exp(jnp.sum(lam[0] * lam[1])) - jnp.exp(jnp.sum(lam[2] * lam[3])) + lam_init
    scale = C_HEAD_DIM ** -0.5
    nb = S_ // Q_BLOCK
    q_blocks = jnp.moveaxis(q.reshape(B_, nb, Q_BLOCK, C_HEADS, 2, C_HEAD_DIM), 1, 0)
    pos = jnp.arange(S_, dtype=jnp.int32)
    q_pos = pos.reshape(nb, Q_BLOCK)

    def block(args):
        qb, qp = args
        bucket = _t5_bucket(pos[None, :] - qp[:, None])
        bias = jnp.transpose(rel_table[bucket], (2, 0, 1)).astype(jnp.float32)
        logits = jnp.einsum('bqhmd,bkhmd->bmhqk', qb, k).astype(jnp.float32) * scale + bias
        p = jax.nn.softmax(logits, axis=-1)
        attn = p[:, 0] - lam_full * p[:, 1]
        return jnp.einsum('bhqk,bkhv->bqhv', attn.astype(v.dtype), v)

    o = lax.map(block, (q_blocks, q_pos))
    o = jnp.moveaxis(o, 0, 1).reshape(B_, S_, C_HEADS, 2 * C_HEAD_DIM)
    o = _rmsnorm(o, subln_g) * (1.0 - lam_init)
    return o.reshape(B_, S_, C_V_WIDTH) @ w_out


def _moe_ffn(h, router_w, router_b, w1, b1, w2, b2):
    B_, S_, D_ = h.shape
    t = h.reshape(-1, D_)
    logits = (t @ router_w + router_b).astype(jnp.float32)
    top_val, top_idx = lax.top_k(logits, TOP_K)
    top_w = jax.nn.softmax(top_val, axis=-1)
    gates = jnp.sum(jax.nn.one_hot(top_idx, N_EXPERTS, dtype=jnp.float32) * top_w[..., None], axis=1)

    def expert_step(acc, params):
        w1_e, b1_e, w2_e, b2_e, gate_e = params
        hh = t @ w1_e + b1_e
        x_glu = jnp.minimum(hh[:, ::2], SWIGLU_LIMIT)
        x_lin = jnp.clip(hh[:, 1::2], -SWIGLU_LIMIT, SWIGLU_LIMIT)
        act = x_glu * jax.nn.sigmoid(SWIGLU_ALPHA * x_glu) * (x_lin + 1.0)
        out = act @ w2_e + b2_e
        return acc + gate_e[:, None] * out.astype(jnp.float32), None

    acc0 = jnp.zeros((t.shape[0], D_), jnp.float32)
    acc, _ = lax.scan(expert_step, acc0, (w1, b1, w2, b2, gates.T))
    return acc.reshape(B_, S_, D_)


def setup_inputs(seed: int = 0) -> dict:
    key = jax.random.key(seed)
    keys = iter(jax.random.split(key, 48))

    def nrm(shape, scale):
        return jax.random.normal(next(keys), shape, jnp.float32) * scale

    def unif(shape):
        return jax.random.uniform(next(keys), shape, jnp.float32)

    L, NE, NO, D = DEPTH, N_EVEN, N_ODD, D_MODEL
    return {
        'x': nrm((BATCH, SEQ, D), 1.0),
        'c': nrm((BATCH, D), 1.0),
        'ada_w': nrm((L, D, 6 * D), 0.5 * D ** -0.5),
        'ada_b': nrm((L, 6 * D), 0.02),
        'norm_mix_g': 1.0 + nrm((L, D), 0.02),
        'norm_ffn_g': 1.0 + nrm((L, D), 0.02),
        'router_w': nrm((L, D, N_EXPERTS), D ** -0.5),
        'router_b': nrm((L, N_EXPERTS), 0.01),
        'moe_w1': nrm((L, N_EXPERTS, D, 2 * D_EXPERT), D ** -0.5),
        'moe_b1': nrm((L, N_EXPERTS, 2 * D_EXPERT), 0.02),
        'moe_w2': nrm((L, N_EXPERTS, D_EXPERT, D), D_EXPERT ** -0.5),
        'moe_b2': nrm((L, N_EXPERTS, D), 0.02),
        'ab_w_in': nrm((NE, D, AB_COLS), D ** -0.5),
        'ab_w_out': nrm((NE, MIX_WIDTH, D), MIX_WIDTH ** -0.5),
        'hgrn_lb': nrm((2, NE + 1, A_WIDTH), 0.5),
        'hgrn_norm_g': 1.0 + nrm((NE, A_HEAD_DIM), 0.02),
        'rwkv_mu': unif((NE, B_COLS)),
        'rwkv_w0': -2.0 + nrm((NE, 2, B_WIDTH), 0.5),
        'rwkv_w2': nrm((NE, 2, B_DECAY_LORA, B_WIDTH), 0.1),
        'rwkv_a0': nrm((NE, B_WIDTH), 0.5),
        'rwkv_a2': nrm((NE, B_AAA_LORA, B_WIDTH), B_AAA_LORA ** -0.5),
        'rwkv_g2': nrm((NE, B_GATE_LORA, B_WIDTH), B_GATE_LORA ** -0.5),
        'rwkv_k_k': 0.85 + nrm((NE, B_WIDTH), 0.05),
        'rwkv_k_a': 1.0 + nrm((NE, B_WIDTH), 0.05),
        'rwkv_r_k': nrm((NE, B_WIDTH), 0.1),
        'rwkv_ln_g': 1.0 + nrm((NE, B_WIDTH), 0.02),
        'rwkv_ln_b': nrm((NE, B_WIDTH), 0.02),
        'attn_w_in': nrm((NO, D, C_QKV_COLS), D ** -0.5),
        'attn_w_out': nrm((NO, C_V_WIDTH, D), C_V_WIDTH ** -0.5),
        'attn_lambda': nrm((NO, 4, C_HEAD_DIM), 0.1),
        'attn_subln_g': 1.0 + nrm((NO, 2 * C_HEAD_DIM), 0.02),
        'rel_bias_table': nrm((REL_BUCKETS, C_HEADS), 0.5),
        'final_norm_g': 1.0 + nrm((D,), 0.02),
    }


def reference(x, c, ada_w, ada_b, norm_mix_g, norm_ffn_g, router_w, router_b, moe_w1, moe_b1,
              moe_w2, moe_b2, ab_w_in, ab_w_out, hgrn_lb, hgrn_norm_g, rwkv_mu, rwkv_w0, rwkv_w2,
              rwkv_a0, rwkv_a2, rwkv_g2, rwkv_k_k, rwkv_k_a, rwkv_r_k, rwkv_ln_g, rwkv_ln_b,
              attn_w_in, attn_w_out, attn_lambda, attn_subln_g, rel_bias_table, final_norm_g):
    cond = jax.nn.silu(c.astype(jnp.float32))
    lb_all = jnp.cumsum(jax.nn.softmax(hgrn_lb.astype(jnp.float32), axis=1), axis=1)
    for layer in range(DEPTH):
        mod = cond @ ada_w[layer].astype(jnp.float32) + ada_b[layer]
        sh_m, sc_m, g_m, sh_f, sc_f, g_f = [m[:, None, :] for m in jnp.split(mod, 6, axis=-1)]
        h = _modulate(x, norm_mix_g[layer], sh_m, sc_m)
        j = layer // 2
        if layer % 2 == 0:
            y = _ab_mixer(h, ab_w_in[j], ab_w_out[j], lb_all[:, j], hgrn_norm_g[j], rwkv_mu[j],
                          rwkv_w0[j], rwkv_w2[j], rwkv_a0[j], rwkv_a2[j], rwkv_g2[j], rwkv_k_k[j],
                          rwkv_k_a[j], rwkv_r_k[j], rwkv_ln_g[j], rwkv_ln_b[j])
        else:
            y = _diff_attention(h, attn_w_in[j], attn_w_out[j], attn_lambda[j], attn_subln_g[j],
                                rel_bias_table, layer)
        x = x + (g_m * y).astype(x.dtype)
        h = _modulate(x, norm_ffn_g[layer], sh_f, sc_f)
        y = _moe_ffn(h, router_w[layer], router_b[layer], moe_w1[layer], moe_b1[layer],
                     moe_w2[layer], moe_b2[layer])
        x = x + (g_f * y).astype(x.dtype)
    return _rmsnorm(x, final_norm_g)
```

```python
import numpy as np
import concourse.bass as bass
import concourse.mybir as mybir

F32 = mybir.dt.float32
BF16 = mybir.dt.bfloat16
ALU = mybir.AluOpType
AF = mybir.ActivationFunctionType
AX = mybir.AxisListType

ENGS = ("pe", "dve", "act", "pool", "sp")
SEM_CAP = 16000


class Dep:
    __slots__ = ("name", "w", "r")

    def __init__(self, name=""):
        self.name = name
        self.w = None
        self.r = []


class Op:
    __slots__ = ("eng", "fn", "deps", "sig", "ev", "isdma", "dkey")

    def __init__(self, eng, fn, isdma, dkey):
        self.eng = eng
        self.fn = fn
        self.deps = []
        self.sig = False
        self.ev = None
        self.isdma = isdma
        self.dkey = dkey


class Prog:
    def __init__(self, nc):
        self.nc = nc
        self.ops = {e: [] for e in ENGS}
        self.dma_cnt = {}
        self.fence_ops = []
        self.need = {}

    def fence(self):
        f = []
        for e in ENGS:
            for o in reversed(self.ops[e]):
                if not o.isdma:
                    f.append(o)
                    break
        last = {}
        for e in ENGS:
            for o in self.ops[e]:
                if o.isdma:
                    last[o.dkey] = o
        f.extend(last.values())
        self.fence_ops = f
        self.need = {e: True for e in ENGS}

    def op(self, eng, fn, rd=(), wr=(), dma=None):
        o = Op(eng, fn, dma is not None, dma)
        deps = []
        for d in rd:
            if d.w is not None:
                deps.append(d.w)
        for d in wr:
            if d.w is not None:
                deps.append(d.w)
            deps.extend(d.r)
        seen = set()
        for y in deps:
            if id(y) in seen or y is o:
                continue
            seen.add(id(y))
            if (not y.isdma) and (not o.isdma) and y.eng == eng == "pe":
                continue
            o.deps.append(y)
            y.sig = True
        if self.need.get(eng):
            self.need[eng] = False
            for y in self.fence_ops:
                if id(y) in seen or y is o:
                    continue
                seen.add(id(y))
                o.deps.append(y)
                y.sig = True
        for d in rd:
            d.r.append(o)
        for d in wr:
            d.w = o
            d.r = []
        if o.isdma:
            o.sig = True
        self.ops[eng].append(o)
        return o

    def emit(self, final_waits=()):
        nc = self.nc
        from contextlib import ExitStack
        nsig = {}
        for e in ENGS:
            c = 0
            for o in self.ops[e]:
                if o.isdma:
                    k = o.dkey
                    self.dma_cnt[k] = self.dma_cnt.get(k, 0) + 16
                    o.ev = ("dma:" + str(k), self.dma_cnt[k])
                elif o.sig:
                    o.ev = ("%s:%d" % (e, c // SEM_CAP), c % SEM_CAP + 1)
                    c += 1
            nsig[e] = c
        semnames = set()
        for e in ENGS:
            for o in self.ops[e]:
                if o.ev is not None:
                    semnames.add(o.ev[0])
        semnames = sorted(semnames)
        self.n_sems = len(semnames)
        self.semnames = semnames
        with ExitStack() as st:
            sems = {}
            for i, n in enumerate(semnames):
                sems[n] = st.enter_context(nc.semaphore("s%d" % i))
            block = st.enter_context(nc.Block())
            prog = self

            def run(ename, eng):
                waited = {}
                for o in prog.ops[ename]:
                    for y in o.deps:
                        s, v = y.ev
                        if waited.get(s, 0) < v:
                            eng.wait_ge(sems[s], v)
                            waited[s] = v
                    ins = o.fn(eng)
                    if o.ev is not None:
                        ins.then_inc(sems[o.ev[0]], 16 if o.isdma else 1)
                if ename == "sp":
                    for y in final_waits:
                        s, v = y.ev
                        if waited.get(s, 0) < v:
                            eng.wait_ge(sems[s], v)
                            waited[s] = v

            @block.tensor
            def _(eng):
                run("pe", eng)

            @block.vector
            def _(eng):
                run("dve", eng)

            @block.scalar
            def _(eng):
                run("act", eng)

            @block.gpsimd
            def _(eng):
                run("pool", eng)

            @block.sync
            def _(eng):
                run("sp", eng)


from contextlib import ExitStack
import numpy as np

S = 2048
D = 1024
NE = 32
TB = 512
NTB = S // TB


class Ctx:
    pass


def build(cfg):
    nc = bass.Bass("TRN2", target_bir_lowering=False)
    P = Prog(nc)
    dr = {}

    def din(name, shape, dt=F32):
        dr[name] = nc.dram_tensor(name, list(shape), dt, kind="ExternalInput").ap()
        return dr[name]

    xT = din("xT", [D, S])
    cT = din("cT", [128, 8])
    ada_w = din("ada_w", [2, D, 6 * D])
    ada_bT = din("ada_bT", [128, 96])
    ngT = din("ngT", [128, 40])
    rw = din("rw", [2, 128, 8 * NE])
    rb = din("rb", [2, 128, NE])
    w1 = din("w1", [2, cfg.get("nexp", NE), D, 2 * D])
    b1T = din("b1T", [2, 128, NE * 16])
    w2 = din("w2", [2, cfg.get("nexp", NE), D, D])
    b2 = din("b2", [2, NE, D])
    ident = din("ident", [128, 128])
    sel = din("sel", [NE, NE * 128])
    din("attn_w_in", [D, 3 * D])
    din("attn_w_out", [D, D])
    din("lamb", [128, 256])
    din("sublnT", [128, 1])
    din("cfar", [128, 16])
    din("reltab", [32, 8])
    din("onehot", [32, 4096])
    din("antiident", [128, 128])
    din("ab_w_in", [D, 4416])
    din("ab_w_out", [D, D])
    din("lbT", [128, 16])
    din("hngT", [128, 1])
    din("rwp", [64, 88])
    din("rwp2", [128, 3])
    din("rw2", [128, 512])
    din("ra2", [64, 512])
    din("rg2", [128, 512])
    din("masks", [128, 1536])
    outT = nc.dram_tensor("outT", [D, S], F32, kind="ExternalOutput").ap()
    dbg = {}
    for name, shape in cfg.get("dbg", {}).items():
        dbg[name] = nc.dram_tensor("dbg_" + name, list(shape), F32, kind="ExternalOutput").ap()

    out_dmas = []
    es = ExitStack()
    with es:
        uid = [0]

        def sb(name, shape, dt=F32, stack=es):
            uid[0] += 1
            return stack.enter_context(nc.sbuf_tensor("%s_%d" % (name, uid[0]), list(shape), dt))

        x = sb("x", [128, 8, S]); dx = [[Dep() for _ in range(NTB)] for _ in range(8)]
        banks = [es.enter_context(nc.psum_tensor("bank%d" % i, [128, 512], F32)) for i in range(8)]
        dbk = [Dep("bank%d" % i) for i in range(8)]
        ones_bf = sb("ones_bf", [128, 128], BF16); d_ones = Dep()
        identf = sb("identf", [128, 128]); d_ident = Dep()
        modT = sb("modT", [128, 96]); d_mod = Dep()
        ng = sb("ng", [128, 40]); d_ng = Dep()
        gsm = sb("gsm", [128, 40]); d_gs = Dep()
        epsc = sb("epsc", [128, 1]); d_eps = Dep()

        def dma(q, out, in_, rd, wr, key):
            return P.op(q, lambda e: e.dma_start(out=out, in_=in_), rd=rd, wr=wr, dma=key)

        def mm(out, lhsT, rhs, start, stop, rd, wr):
            return P.op("pe", lambda e: e.matmul(out, lhsT=lhsT, rhs=rhs, start=start, stop=stop), rd=rd, wr=wr)

        def act(out, in_, func, rd, wr, bias=0.0, scale=1.0):
            return P.op("act", lambda e: e.activation(out=out, in_=in_, func=func, bias=bias, scale=scale), rd=rd, wr=wr)

        def tt(eng, out, in0, in1, op, rd, wr):
            return P.op(eng, lambda e: e.tensor_tensor(out=out, in0=in0, in1=in1, op=op), rd=rd, wr=wr)

        def ts(eng, out, in0, s1, s2, op0, op1, rd, wr):
            if op1 is None:
                return P.op(eng, lambda e: e.tensor_scalar(out=out, in0=in0, scalar1=s1, scalar2=None, op0=op0), rd=rd, wr=wr)
            return P.op(eng, lambda e: e.tensor_scalar(out=out, in0=in0, scalar1=s1, scalar2=s2, op0=op0, op1=op1), rd=rd, wr=wr)

        def stt(eng, out, in0, scalar, in1, op0, op1, rd, wr):
            return P.op(eng, lambda e: e.scalar_tensor_tensor(out=out, in0=in0, scalar=scalar, in1=in1, op0=op0, op1=op1), rd=rd, wr=wr)

        def cp(eng, out, in_, rd, wr):
            return P.op(eng, lambda e: e.tensor_copy(out=out, in_=in_), rd=rd, wr=wr)

        xTv = xT.rearrange("(c p) t -> p c t", p=128)
        for c in range(8):
            dma("sp", x[:, c, :], xTv[:, c, :], [], dx[c], "x%d" % c)
        dma("sp", identf[:], ident[:, :], [], [d_ident], "c_ident")
        dma("sp", ng[:], ngT[:, :], [], [d_ng], "c_ng")
        P.op("pool", lambda e: e.memset(ones_bf[:], 1.0), wr=[d_ones])
        P.op("pool", lambda e: e.memset(epsc[:], 1e-6), wr=[d_eps])

        with ExitStack() as ph:
            P.fence()
            cs = sb("cs", [128, 8], F32, ph); d_cs = Dep()
            cb = sb("cb", [128, 8], BF16, ph); d_cb = Dep()
            abT = sb("abT", [128, 96], F32, ph); d_ab = Dep()
            wb = [sb("adaw%d" % i, [128, 8, 512], BF16, ph) for i in range(2)]
            d_wb = [Dep(), Dep()]
            dma("sp", cs[:], cT[:, :], [], [d_cs], "c_cs")
            dma("sp", abT[:], ada_bT[:, :], [], [d_ab], "c_ab")
            act(cb[:], cs[:], AF.Silu, [d_cs], [d_cb])
            it = 0
            for layer in range(2):
                for fc in range(12):
                    s = it % 2
                    src = ada_w[layer, :, fc * 512:(fc + 1) * 512].rearrange("(kc p) f -> p kc f", p=128)
                    dma("pool", wb[s][:], src, [], [d_wb[s]], "adaw%d" % s)
                    for j in range(4):
                        col = layer * 48 + fc * 4 + j
                        for kc in range(8):
                            mm(banks[0][:, col:col + 1], wb[s][:, kc, j * 128:(j + 1) * 128], cb[:, kc:kc + 1],
                               kc == 0, kc == 7, [d_wb[s], d_cb], [dbk[0]])
                    it += 1
            tt("dve", modT[:], banks[0][:, 0:96], abT[:], ALU.add, [dbk[0], d_ab], [d_mod])
            for layer in range(2):
                stt("dve", gsm[:, layer * 8:(layer + 1) * 8], modT[:, layer * 48 + 8:layer * 48 + 16], 1.0,
                    ng[:, layer * 8:(layer + 1) * 8], ALU.add, ALU.mult, [d_mod, d_ng], [d_gs])
                stt("dve", gsm[:, 16 + layer * 8:16 + (layer + 1) * 8], modT[:, layer * 48 + 32:layer * 48 + 40], 1.0,
                    ng[:, 16 + layer * 8:16 + (layer + 1) * 8], ALU.add, ALU.mult, [d_mod, d_ng], [d_gs])
            if "modT" in dbg:
                out_dmas.append(dma("sp", dbg["modT"][:, :], modT[:], [d_mod], [], "dbg_mod"))

        def allx():
            return [d for row in dx for d in row]

        def norm_mod(ph, gs_ap, sh_ap, hT, d_hT, router_layer=None, extra=None):
            with ExitStack() as loc:
                P.fence()
                sqb = [sb("sqb%d" % i, [128, S], BF16, loc) for i in range(2)]
                d_sq = [Dep(), Dep()]
                rstd = sb("rstd", [128, S], F32, loc); d_rstd = [Dep() for _ in range(NTB)]
                tmp = [sb("ntmp%d" % i, [128, S], F32, loc) for i in range(2)]
                d_tmp = [Dep(), Dep()]
                for c in range(8):
                    s = c % 2
                    act(sqb[s][:], x[:, c, :], AF.Square, dx[c], [d_sq[s]])
                    for tb in range(NTB):
                        mm(banks[tb][:, :], ones_bf[:], sqb[s][:, tb * TB:(tb + 1) * TB], c == 0, c == 7,
                           [d_ones, d_sq[s]], [dbk[tb]])
                for tb in range(NTB):
                    act(rstd[:, tb * TB:(tb + 1) * TB], banks[tb][:, :], AF.Sqrt, [dbk[tb], d_eps], [d_rstd[tb]],
                        bias=epsc[:, 0:1], scale=1.0 / D)
                    P.op("dve", lambda e, tb=tb: e.reciprocal(out=rstd[:, tb * TB:(tb + 1) * TB], in_=rstd[:, tb * TB:(tb + 1) * TB]),
                         rd=[d_rstd[tb]], wr=[d_rstd[tb]])
                if router_layer is not None:
                    h32 = [sb("h32_%d" % i, [128, S], F32, loc) for i in range(2)]
                    d_h32 = [Dep(), Dep()]
                    rw32, d_rw = extra
                for c in range(8):
                    s = c % 2
                    tt("dve", tmp[s][:], x[:, c, :], rstd[:], ALU.mult, dx[c] + d_rstd, [d_tmp[s]])
                    if router_layer is None:
                        act(hT[:, c, :], tmp[s][:], AF.Identity, [d_tmp[s], d_gs, d_mod], [d_hT[c]],
                            bias=sh_ap[:, c:c + 1], scale=gs_ap[:, c:c + 1])
                    else:
                        act(h32[s][:], tmp[s][:], AF.Identity, [d_tmp[s], d_gs, d_mod], [d_h32[s]],
                            bias=sh_ap[:, c:c + 1], scale=gs_ap[:, c:c + 1])
                        cp("pool", hT[:, c, :], h32[s][:], [d_h32[s]], [d_hT[c]])
                        for tb in range(NTB):
                            mm(banks[4 + tb][0:NE, :], rw32[:, c * NE:(c + 1) * NE], h32[s][:, tb * TB:(tb + 1) * TB],
                               c == 0, c == 7, [d_rw, d_h32[s]], [dbk[4 + tb]])

        def moe(layer):
            with ExitStack() as ph:
                P.fence()
                hT = sb("hT", [128, 8, S], BF16, ph); d_hT = [Dep() for _ in range(8)]
                rw32 = sb("rw32", [128, 8 * NE], F32, ph); d_rw = Dep()
                rbs = sb("rbs", [128, NE], F32, ph); d_rb = Dep()
                b1s = sb("b1s", [128, NE * 16], F32, ph); d_b1 = Dep()
                b2s = sb("b2s", [NE, D], BF16, ph); d_b2 = Dep()
                gatesT = sb("gatesT", [NE, S], BF16, ph); d_gT = Dep()
                selsb = sb("selsb", [NE, NE * 128], BF16, ph); d_sel = Dep()
                dma("pool", selsb[:], sel[:, :], [], [d_sel], "c_sel")
                dma("sp", rw32[:], rw[layer, :, :], [], [d_rw], "m_rw")
                dma("sp", rbs[:], rb[layer, :, :], [], [d_rb], "m_rb")
                dma("sp", b1s[:], b1T[layer, :, :], [], [d_b1], "m_b1")
                dma("pool", b2s[:], b2[layer, :, :], [], [d_b2], "m_b2")
                b1v = b1s[:].rearrange("p (e t c) -> p e t c", e=NE, t=2)
                ts("dve", b1v[:, :, 1, :], b1v[:, :, 1, :], 1.0, None, ALU.add, None, [d_b1], [d_b1])
                gs_ap = gsm[:, 16 + layer * 8:16 + (layer + 1) * 8]
                sh_ap = modT[:, layer * 48 + 24:layer * 48 + 32]
                gf_ap = modT[:, layer * 48 + 40:layer * 48 + 48]
                norm_mod(ph, gs_ap, sh_ap, hT, d_hT, router_layer=layer, extra=(rw32, d_rw))
                if ("hffn%d" % layer) in dbg:
                    pass
                with ExitStack() as loc:
                    P.fence()
                    lgT = sb("lgT", [NE, S], F32, loc); d_lgT = Dep()
                    lg = sb("lg", [128, 16, NE], F32, loc); d_lg = Dep()
                    m8 = sb("m8", [128, 16, 8], F32, loc); d_m8 = Dep()
                    msk = sb("msk", [128, 16, NE], F32, loc); d_msk = Dep()
                    ex = sb("ex", [128, 16, NE], F32, loc); d_ex = Dep()
                    zz = sb("zz", [128, 16], F32, loc); d_zz = Dep()
                    for tb in range(NTB):
                        act(lgT[:, tb * TB:(tb + 1) * TB], banks[4 + tb][0:NE, :], AF.Copy, [dbk[4 + tb]], [d_lgT])
                    for t in range(16):
                        P.op("pe", lambda e, t=t: e.transpose(banks[0][:, t * NE:(t + 1) * NE], lgT[:, t * 128:(t + 1) * 128], identf[0:NE, 0:NE]),
                             rd=[d_lgT, d_ident], wr=[dbk[0]])
                    tt("dve", lg[:], banks[0][:, :].rearrange("p (t e) -> p t e", e=NE),
                       rbs[:, None, :].to_broadcast([128, 16, NE]), ALU.add, [dbk[0], d_rb], [d_lg])
                    for t in range(16):
                        P.op("dve", lambda e, t=t: e.max(out=m8[:, t, :], in_=lg[:, t, :]), rd=[d_lg], wr=[d_m8])
                    tt("dve", msk[:], lg[:], m8[:, :, 3:4].to_broadcast([128, 16, NE]), ALU.is_ge, [d_lg, d_m8], [d_msk])
                    tt("dve", ex[:], lg[:], m8[:, :, 0:1].to_broadcast([128, 16, NE]), ALU.subtract, [d_lg, d_m8], [d_ex])
                    act(ex[:], ex[:], AF.Exp, [d_ex], [d_ex])
                    tt("dve", ex[:], ex[:], msk[:], ALU.mult, [d_ex, d_msk], [d_ex])
                    P.op("dve", lambda e: e.tensor_reduce(out=zz[:], in_=ex[:], axis=AX.X, op=ALU.add), rd=[d_ex], wr=[d_zz])
                    P.op("dve", lambda e: e.reciprocal(out=zz[:], in_=zz[:]), rd=[d_zz], wr=[d_zz])
                    tt("dve", ex[:], ex[:], zz[:, :, None].to_broadcast([128, 16, NE]), ALU.mult, [d_ex, d_zz], [d_ex])
                    if ("gates%d" % layer) in dbg:
                        out_dmas.append(dma("sp", dbg["gates%d" % layer].rearrange("(t p) e -> p t e", p=128), ex[:], [d_ex], [], "dbg_g"))
                    for t in range(16):
                        tb, o = t // 4, (t % 4) * 128
                        P.op("pe", lambda e, t=t, tb=tb, o=o: e.transpose(banks[4 + tb][0:NE, o:o + 128], ex[:, t, :], identf[:, :]),
                             rd=[d_ex, d_ident], wr=[dbk[4 + tb]])
                    for tb in range(NTB):
                        act(gatesT[:, tb * TB:(tb + 1) * TB], banks[4 + tb][0:NE, :], AF.Copy, [dbk[4 + tb]], [d_gT])
                with ExitStack() as loc:
                    P.fence()
                    actb = sb("actb", [128, 8, S], BF16, loc); d_act = [[Dep() for _ in range(NTB)] for _ in range(8)]
                    NR = 4
                    w1r = [sb("w1r%d" % i, [128, 8, 256], BF16, loc) for i in range(NR)]; d_w1 = [Dep() for _ in range(NR)]
                    w2r = [sb("w2r%d" % i, [128, 8, D], BF16, loc) for i in range(1)]; d_w2 = [Dep()]
                    gbc = [sb("gbc%d" % i, [128, S], BF16, loc) for i in range(2)]; d_gbc = [Dep(), Dep()]
                    NT = 2
                    xg = [sb("xg%d" % i, [128, TB], F32, loc) for i in range(NT)]; d_xg = [Dep() for _ in range(NT)]
                    sg = [sb("sg%d" % i, [128, TB], F32, loc) for i in range(NT)]; d_sg = [Dep() for _ in range(NT)]
                    xl = [sb("xl%d" % i, [128, TB], F32, loc) for i in range(NT)]; d_xl = [Dep() for _ in range(NT)]
                    uu = [sb("uu%d" % i, [128, TB], F32, loc) for i in range(NT)]; d_uu = [Dep() for _ in range(NT)]
                    nexp = cfg.get("nexp", NE)
                    pieces = [(e, j) for e in range(nexp) for j in range(8)]
                    def load_piece(n):
                        e, j = pieces[n]
                        s = n % NR
                        src = w1[layer, e, :, j * 256:(j + 1) * 256].rearrange("(kc p) f -> p kc f", p=128)
                        dma("pool", w1r[s][:], src, [], [d_w1[s]], "w1r%d" % s)
                    def load_w2(e):
                        s = 0
                        src = w2[layer, e, :, :].rearrange("(kc p) f -> p kc f", p=128)
                        dma("pool", w2r[s][:], src, [], [d_w2[s]], "w2r%d" % s)
                    for n in range(min(NR - 1, len(pieces))):
                        load_piece(n)
                    load_w2(0)
                    blk = 0
                    ob = 0
                    for e in range(nexp):
                        gs_ = e % 2
                        for tb in range(NTB):
                            mm(banks[6][:, :], selsb[:, e * 128:(e + 1) * 128], gatesT[:, tb * TB:(tb + 1) * TB], True, True,
                               [d_sel, d_gT], [dbk[6]])
                            act(gbc[gs_][:, tb * TB:(tb + 1) * TB], banks[6][:, :], AF.Copy, [dbk[6]], [d_gbc[gs_]], scale=1.0 / 1.702)
                        for j in range(8):
                            n = e * 8 + j
                            if n + NR - 1 < len(pieces):
                                load_piece(n + NR - 1)
                            s = n % NR
                            for tb in range(NTB):
                                pa, pb = (blk % 2) * 2, (blk % 2) * 2 + 1
                                k = blk % NT
                                for kc in range(8):
                                    mm(banks[pa][:, :], w1r[s][:, kc, 0:128], hT[:, kc, tb * TB:(tb + 1) * TB], kc == 0, kc == 7,
                                       [d_w1[s], d_hT[kc]], [dbk[pa]])
                                for kc in range(8):
                                    mm(banks[pb][:, :], w1r[s][:, kc, 128:256], hT[:, kc, tb * TB:(tb + 1) * TB], kc == 0, kc == 7,
                                       [d_w1[s], d_hT[kc]], [dbk[pb]])
                                ts("dve", xg[k][:], banks[pa][:, :], b1s[:, e * 16 + j:e * 16 + j + 1], 7.0, ALU.add, ALU.min,
                                   [dbk[pa], d_b1], [d_xg[k]])
                                act(sg[k][:], xg[k][:], AF.Silu, [d_xg[k]], [d_sg[k]], scale=1.702)
                                ts("dve", xl[k][:], banks[pb][:, :], b1s[:, e * 16 + 8 + j:e * 16 + 8 + j + 1], 8.0, ALU.add, ALU.min,
                                   [dbk[pb], d_b1], [d_xl[k]])
                                stt("dve", uu[k][:], xl[k][:], -6.0, sg[k][:], ALU.max, ALU.mult, [d_xl[k], d_sg[k]], [d_uu[k]])
                                tt("dve", actb[:, j, tb * TB:(tb + 1) * TB], uu[k][:], gbc[gs_][:, tb * TB:(tb + 1) * TB], ALU.mult,
                                   [d_uu[k], d_gbc[gs_]], [d_act[j][tb]])
                                blk += 1
                        s2 = 0
                        for i in range(8):
                            for tb in range(NTB):
                                bo = 4 + (ob % 2)
                                for j in range(8):
                                    mm(banks[bo][:, :], w2r[s2][:, j, i * 128:(i + 1) * 128], actb[:, j, tb * TB:(tb + 1) * TB],
                                       j == 0, j == 7, [d_w2[s2], d_act[j][tb]], [dbk[bo]])
                                stt("dve", x[:, i, tb * TB:(tb + 1) * TB], banks[bo][:, :], gf_ap[:, i:i + 1], x[:, i, tb * TB:(tb + 1) * TB],
                                    ALU.mult, ALU.add, [dbk[bo], d_mod, dx[i][tb]], [dx[i][tb]])
                                ob += 1
                        if e + 1 < nexp:
                            load_w2(e + 1)
                    for i in range(8):
                        for tb in range(NTB):
                            bo = 4 + (ob % 2)
                            mm(banks[bo][:, :], b2s[:, i * 128:(i + 1) * 128], gatesT[:, tb * TB:(tb + 1) * TB], True, True,
                               [d_b2, d_gT], [dbk[bo]])
                            stt("dve", x[:, i, tb * TB:(tb + 1) * TB], banks[bo][:, :], gf_ap[:, i:i + 1], x[:, i, tb * TB:(tb + 1) * TB],
                                ALU.mult, ALU.add, [dbk[bo], d_mod, dx[i][tb]], [dx[i][tb]])
                            ob += 1


        def attn_mixer(layer):
            LAM_INIT = 0.8 - 0.6 * float(np.exp(-0.3 * layer))
            with ExitStack() as ph:
                P.fence()
                hT = sb("hTa", [128, 8, S], BF16, ph); d_hT = [Dep() for _ in range(8)]
                yT = sb("yTa", [128, 8, S], BF16, ph); d_yT = [Dep() for _ in range(8)]
                norm_mod(ph, gsm[:, layer * 8:(layer + 1) * 8], modT[:, layer * 48:layer * 48 + 8], hT, d_hT)
                gm_ap = modT[:, layer * 48 + 16:layer * 48 + 24]
                with ExitStack() as loc:
                    P.fence()
                    lamb = sb("lamb", [128, 256], F32, loc); d_lam = Dep()
                    lsc = sb("lsc", [128, 8], F32, loc); d_lsc = Dep()
                    ltmp = sb("ltmp", [128, 128], F32, loc)
                    sgc = sb("sgc", [128, 1], F32, loc); d_sgc = Dep()
                    cfar = sb("cfar", [128, 16], F32, loc); d_cfar = Dep()
                    identb = sb("identb", [128, 128], BF16, loc); d_idb = Dep()
                    dma("sp", lamb[:], dr["lamb"][:, :], [], [d_lam], "a_lam")
                    dma("sp", sgc[:], dr["sublnT"][:, :], [], [d_sgc], "a_sg")
                    dma("sp", cfar[:], dr["cfar"][:, :], [], [d_cfar], "a_cf")
                    dma("pool", identb[:], dr["antiident"][:, :], [], [d_idb], "a_aid")
                    ts("dve", sgc[:], sgc[:], 1.0 - LAM_INIT, None, ALU.mult, None, [d_sgc], [d_sgc])
                    tt("dve", ltmp[:, 0:64], lamb[:, 0:64], lamb[:, 64:128], ALU.mult, [d_lam], [d_lsc])
                    tt("dve", ltmp[:, 64:128], lamb[:, 128:192], lamb[:, 192:256], ALU.mult, [d_lam], [d_lsc])
                    P.op("dve", lambda e: e.tensor_reduce(out=lsc[:, 0:2], in_=ltmp[:].rearrange("p (a b) -> p a b", a=2), axis=AX.X, op=ALU.add),
                         rd=[d_lsc], wr=[d_lsc])
                    act(lsc[:, 2:4], lsc[:, 0:2], AF.Exp, [d_lsc], [d_lsc])
                    tt("dve", lsc[:, 4:5], lsc[:, 3:4], lsc[:, 2:3], ALU.subtract, [d_lsc], [d_lsc])
                    ts("dve", lsc[:, 4:5], lsc[:, 4:5], -LAM_INIT, None, ALU.add, None, [d_lsc], [d_lsc])
                    gdr = nc.dram_tensor("gr_scratch", [8, 4096], F32)
                    d_gdr = Dep()
                    with ExitStack() as tbs:
                        P.fence()
                        tab = sb("tab", [32, 8], F32, tbs); d_tab = Dep()
                        oh = sb("oh", [32, 4096], F32, tbs); d_oh = Dep()
                        gsb = sb("gsb", [8, 4096], F32, tbs); d_gsb = Dep()
                        dma("sp", tab[:], dr["reltab"][:, :], [], [d_tab], "a_tab")
                        dma("sp", oh[:], dr["onehot"][:, :], [], [d_oh], "a_oh")
                        for cb_ in range(8):
                            mm(banks[7][0:8, :], tab[:, :], oh[:, cb_ * 512:(cb_ + 1) * 512], True, True, [d_tab, d_oh], [dbk[7]])
                            act(gsb[:, cb_ * 512:(cb_ + 1) * 512], banks[7][0:8, :], AF.Copy, [dbk[7]], [d_gsb])
                        dma("sp", gdr.ap()[:, :], gsb[:], [d_gsb], [d_gdr], "a_gdr")
                        if "gsb" in dbg:
                            out_dmas.append(dma("sp", dbg["gsb"][:, :], gsb[:], [d_gsb], [], "dbg_gsb"))
                    P.fence()
                    wq = sb("wq", [128, 8, 128], BF16, loc); d_wq = Dep()
                    wk = sb("wk", [128, 8, 128], BF16, loc); d_wk = Dep()
                    wv = sb("wv", [128, 8, 128], BF16, loc); d_wv = Dep()
                    qT = sb("qT", [128, S], BF16, loc); d_q = Dep()
                    kT = sb("kT", [128, S], BF16, loc); d_k = Dep()
                    vt = sb("vt", [128, 16, 128], BF16, loc); d_v = Dep()
                    bt = sb("bt", [128, 6, 512], BF16, loc); d_bt = Dep()
                    E = [sb("E%d" % i, [128, 512], BF16, loc) for i in range(3)]; d_E = [Dep() for _ in range(3)]
                    oh_ = sb("ohd", [128, S], F32, loc); d_ohd = [Dep() for _ in range(NTB)]
                    rz = sb("rz", [128, 512], F32, loc); d_rz = Dep()
                    o0 = sb("o0", [128, 512], F32, loc); d_o0 = Dep()
                    o1 = sb("o1", [128, 512], F32, loc); d_o1 = Dep()
                    sq = sb("asq", [128, S], BF16, loc); d_sq = Dep()
                    rs = sb("ars", [128, S], F32, loc); d_rs = [Dep() for _ in range(NTB)]
                    w_in = dr["attn_w_in"]
                    ei = 0
                    sbk = 0
                    for h in range(8):
                        for (wt, dw, c0) in ((wq, d_wq, h * 128), (wk, d_wk, 1024 + h * 128), (wv, d_wv, 2048 + h * 128)):
                            dma("pool", wt[:], w_in[:, c0:c0 + 128].rearrange("(kc p) f -> p kc f", p=128), [], [dw], "a_w%d" % c0)
                        for di in range(6):
                            delta = -128 + 128 * di
                            src = bass.AP(tensor=gdr, offset=h * 4096 + 2047 - delta - 127, ap=[[1, 128], [1, 512]])
                            dma("pool", bt[:, di, :], src, [d_gdr], [d_bt], "a_bt")
                        for tb in range(NTB):
                            for (wt, dw, dst, dd, sc_) in ((wq, d_wq, qT, d_q, 0.125), (wk, d_wk, kT, d_k, 1.0)):
                                bk = sbk % 2; sbk += 1
                                for kc in range(8):
                                    mm(banks[bk][:, :], wt[:, kc, :], hT[:, kc, tb * TB:(tb + 1) * TB], kc == 0, kc == 7, [dw, d_hT[kc]], [dbk[bk]])
                                act(dst[:, tb * TB:(tb + 1) * TB], banks[bk][:, :], AF.Copy, [dbk[bk]], [dd], scale=sc_)
                        for t in range(16):
                            bk = sbk % 2; sbk += 1
                            for kc in range(8):
                                mm(banks[bk][:, 0:128], hT[:, kc, t * 128:(t + 1) * 128], wv[:, kc, :], kc == 0, kc == 7, [d_wv, d_hT[kc]], [dbk[bk]])
                            cp("dve", vt[:, t, :], banks[bk][:, 0:128], [dbk[bk]], [d_v])
                        for qb in range(NTB):
                            for m in range(2):
                                bo, bz = 2 + 2 * m, 3 + 2 * m
                                for kt in range(16):
                                    delta = 128 * kt - 512 * qb
                                    near = -255 < delta < 639
                                    bk = sbk % 2; sbk += 1
                                    mm(banks[bk][:, :], kT[m * 64:(m + 1) * 64, kt * 128:(kt + 1) * 128], qT[m * 64:(m + 1) * 64, qb * TB:(qb + 1) * TB],
                                       True, not near, [d_k, d_q], [dbk[bk]])
                                    if near:
                                        di = (delta + 128) // 128
                                        mm(banks[bk][:, :], identb[:], bt[:, di, :], False, True, [d_idb, d_bt], [dbk[bk]])
                                    e_ = ei % 3; ei += 1
                                    if near:
                                        act(E[e_][:], banks[bk][:, :], AF.Exp, [dbk[bk]], [d_E[e_]])
                                    else:
                                        col = h * 2 + (1 if delta > 0 else 0)
                                        act(E[e_][:], banks[bk][:, :], AF.Exp, [dbk[bk], d_cfar], [d_E[e_]], bias=cfar[:, col:col + 1])
                                    mm(banks[bo][:, :], vt[:, kt, :], E[e_][:], kt == 0, kt == 15, [d_v, d_E[e_]], [dbk[bo]])
                                    mm(banks[bz][:, :], ones_bf[:], E[e_][:], kt == 0, kt == 15, [d_ones, d_E[e_]], [dbk[bz]])
                            P.op("dve", lambda e: e.reciprocal(out=rz[:], in_=banks[3][:, :]), rd=[dbk[3]], wr=[d_rz])
                            tt("dve", o0[:], banks[2][:, :], rz[:], ALU.mult, [dbk[2], d_rz], [d_o0])
                            P.op("dve", lambda e: e.reciprocal(out=rz[:], in_=banks[5][:, :]), rd=[dbk[5]], wr=[d_rz])
                            tt("dve", o1[:], banks[4][:, :], rz[:], ALU.mult, [dbk[4], d_rz], [d_o1])
                            stt("dve", oh_[:, qb * TB:(qb + 1) * TB], o1[:], lsc[:, 4:5], o0[:], ALU.mult, ALU.add, [d_o0, d_o1, d_lsc], [d_ohd[qb]])
                        if "ohd" in dbg and h == 7:
                            out_dmas.append(dma("sp", dbg["ohd"][:, :], oh_[:], d_ohd, [], "dbg_ohd"))
                        if "bt" in dbg and h == 7:
                            out_dmas.append(dma("pool", dbg["bt"][:, :], bt[:].rearrange("p a b -> p (a b)"), [d_bt], [], "dbg_bt"))
                            out_dmas.append(dma("pool", dbg["qT"][:, :], qT[:], [d_q], [], "dbg_qT"))
                            out_dmas.append(dma("pool", dbg["kT"][:, :], kT[:], [d_k], [], "dbg_kT"))
                            out_dmas.append(dma("pool", dbg["vt"][:, :], vt[:].rearrange("p a b -> p (a b)"), [d_v], [], "dbg_vt"))
                        act(sq[:], oh_[:], AF.Square, d_ohd, [d_sq])
                        for tb in range(NTB):
                            bk = 6 + tb % 2
                            mm(banks[bk][:, :], ones_bf[:], sq[:, tb * TB:(tb + 1) * TB], True, True, [d_ones, d_sq], [dbk[bk]])
                            act(rs[:, tb * TB:(tb + 1) * TB], banks[bk][:, :], AF.Sqrt, [dbk[bk], d_eps], [d_rs[tb]], bias=epsc[:, 0:1], scale=1.0 / 128)
                            P.op("dve", lambda e, tb=tb: e.reciprocal(out=rs[:, tb * TB:(tb + 1) * TB], in_=rs[:, tb * TB:(tb + 1) * TB]), rd=[d_rs[tb]], wr=[d_rs[tb]])
                        stt("dve", yT[:, h, :], oh_[:], sgc[:, 0:1], rs[:], ALU.mult, ALU.mult, d_ohd + d_rs + [d_sgc], [d_yT[h]])
                out_proj(ph, dr["attn_w_out"], yT, d_yT, gm_ap)

        def out_proj(ph, w_out, yT, d_yT, gm_ap):
            with ExitStack() as loc:
                P.fence()
                wo = [sb("wo%d" % i, [128, 8, 128], BF16, loc) for i in range(2)]; d_wo = [Dep(), Dep()]
                ob = 0
                for i in range(8):
                    s = i % 2
                    dma("pool", wo[s][:], w_out[:, i * 128:(i + 1) * 128].rearrange("(kc p) f -> p kc f", p=128), [], [d_wo[s]], "wo%d" % s)
                    for tb in range(NTB):
                        bo = ob % 2; ob += 1
                        for kc in range(8):
                            mm(banks[bo][:, :], wo[s][:, kc, :], yT[:, kc, tb * TB:(tb + 1) * TB], kc == 0, kc == 7, [d_wo[s], d_yT[kc]], [dbk[bo]])
                        stt("dve", x[:, i, tb * TB:(tb + 1) * TB], banks[bo][:, :], gm_ap[:, i:i + 1], x[:, i, tb * TB:(tb + 1) * TB],
                            ALU.mult, ALU.add, [dbk[bo], d_mod, dx[i][tb]], [dx[i][tb]])

        class Arr:
            def __init__(self, ap, deps):
                self.ap = ap
                self.deps = deps

        def ab_mixer(layer):
            W_IN = dr["ab_w_in"]
            ydram = nc.dram_tensor("y_scratch", [D, S], BF16)
            ydv = ydram.ap()
            d_yd = Dep()
            gm_ap = modT[:, layer * 48 + 16:layer * 48 + 24]
            with ExitStack() as ph:
                P.fence()
                hT = sb("hTm", [128, 8, S], BF16, ph); d_hT = [Dep() for _ in range(8)]
                norm_mod(ph, gsm[:, layer * 8:(layer + 1) * 8], modT[:, layer * 48:layer * 48 + 8], hT, d_hT)
                with ExitStack() as sc:
                    P.fence()
                    d_big = [Dep(), Dep()]
                    bigs = [(banks[0], d_big[0]), (banks[1], d_big[1])]
                    smalls = [(banks[b][:, 0:128], Dep()) for b in (2, 3, 4, 5)]
                    yregs = [(banks[b][:, 0:128], Dep()) for b in (6, 7)]
                    alld = d_big + [d for _, d in smalls] + [d for _, d in yregs]
                    scr = sb("barscr", [128, 1], F32, sc)
                    P.op("dve", lambda e: e.memset(scr[:], 0.0), rd=list(dbk), wr=alld)
                    cnt = {"big": 0, "small": 0, "y": 0, "ev": 0, "w": 0, "tok": 0, "mat": 0, "bg": 0}

                    def big():
                        cnt["big"] += 1
                        return bigs[cnt["big"] % 2]

                    def small():
                        cnt["small"] += 1
                        return smalls[cnt["small"] % 4]

                    def yreg():
                        cnt["y"] += 1
                        return yregs[cnt["y"] % 2]

                    msk = sb("msk", [128, 1536], F32, sc); d_msk = Dep()
                    dma("sp", msk[:], dr["masks"][:, :], [], [d_msk], "b_msk")
                    IU = msk[:, 0:128]; SU = msk[:, 128:256]; IL = msk[:, 256:384]; SLm = msk[:, 384:512]
                    CM = msk[:, 512:1024].rearrange("p (c k) -> p c k", c=4)
                    IEX = msk[:, 1024:1536].rearrange("p (c k) -> p c k", c=4)
                    ones32 = sb("ones32", [128, 128], F32, sc); d_o32 = Dep()
                    P.op("pool", lambda e: e.memset(ones32[:], 1.0), wr=[d_o32])
                    epsln = sb("epsln", [128, 1], F32, sc); d_epsln = Dep()
                    P.op("pool", lambda e: e.memset(epsln[:], 64e-5), wr=[d_epsln])
                    m32 = sb("m32", [128, S], BF16, sc); d_m32 = Dep()
                    P.op("pool", lambda e: e.memset(m32[:], 1.0), wr=[d_m32])
                    P.op("pool", lambda e: e.memset(m32[:].rearrange("p (c t) -> p c t", t=32)[:, :, 0:1], 0.0), rd=[d_m32], wr=[d_m32])
                    lbT = sb("lbT", [128, 16], F32, sc); d_lb = Dep()
                    lbc = sb("lbc", [128, 8], F32, sc); oml = sb("oml", [128, 8], F32, sc)
                    dma("sp", lbT[:], dr["lbT"][:, :], [], [d_lb], "b_lb")
                    lbv = lbT[:].rearrange("p (d s h) -> p d s h", d=2, s=2)
                    tt("dve", lbc[:].rearrange("p (d h) -> p d h", d=2), lbv[:, :, 0, :], lbv[:, :, 1, :], ALU.subtract, [d_lb], [d_lb])
                    act(lbc[:], lbc[:], AF.Sigmoid, [d_lb], [d_lb])
                    ts("dve", oml[:], lbc[:], -1.0, 1.0, ALU.mult, ALU.add, [d_lb], [d_lb])
                    hng = sb("hng", [128, 1], F32, sc); d_hng = Dep()
                    dma("sp", hng[:], dr["hngT"][:, :], [], [d_hng], "b_hng")
                    rwp = sb("rwp", [64, 88], F32, sc); d_rwp = Dep()
                    rwo = sb("rwo", [64, 88], F32, sc); rwh = sb("rwh", [64, 88], F32, sc)
                    dma("sp", rwp[:], dr["rwp"][:, :], [], [d_rwp], "b_rwp")
                    ts("dve", rwo[:], rwp[:], -1.0, 1.0, ALU.mult, ALU.add, [d_rwp], [d_rwp])
                    ts("dve", rwh[:], rwp[:], 0.5, None, ALU.mult, None, [d_rwp], [d_rwp])
                    rwp2 = sb("rwp2", [128, 3], F32, sc); d_rwp2 = Dep()
                    rwo2 = sb("rwo2", [128, 3], F32, sc); rwh2 = sb("rwh2", [128, 3], F32, sc)
                    dma("sp", rwp2[:], dr["rwp2"][:, :], [], [d_rwp2], "b_rwp2")
                    ts("dve", rwo2[:], rwp2[:], -1.0, 1.0, ALU.mult, ALU.add, [d_rwp2], [d_rwp2])
                    ts("dve", rwh2[:], rwp2[:], 0.5, None, ALU.mult, None, [d_rwp2], [d_rwp2])
                    rw2b = sb("rw2b", [128, 512], BF16, sc); d_rw2 = Dep()
                    ra2b = sb("ra2b", [64, 512], BF16, sc); d_ra2 = Dep()
                    rg2b = sb("rg2b", [128, 512], BF16, sc); d_rg2 = Dep()
                    dma("pool", rw2b[:], dr["rw2"][:, :], [], [d_rw2], "b_rw2")
                    dma("pool", ra2b[:], dr["ra2"][:, :], [], [d_ra2], "b_ra2")
                    dma("pool", rg2b[:], dr["rg2"][:, :], [], [d_rg2], "b_rg2")

                    wbx = [sb("wbx%d" % i, [128, S], F32, sc) for i in range(2)]
                    SLT = [Arr(x[:, i, :], dx[i]) for i in range(8)] + [Arr(wbx[i][:], [Dep()]) for i in range(2)]
                    wbuf = [sb("abwb%d" % i, [128, 8, 128], BF16, sc) for i in range(2)]; d_wbuf = [Dep(), Dep()]
                    WCt = [sb("WC%d" % i, [128, 64], F32, sc) for i in range(2)]; d_WC = [Dep(), Dep()]
                    STt = [sb("ST%d" % i, [128, 128], F32, sc) for i in range(2)]; d_ST = [Dep(), Dep()]
                    NTOK, NMAT, NBG = 10, 22, 5
                    tokt = [(sb("tok%d" % i, [128, 128], F32, sc), Dep()) for i in range(NTOK)]
                    matt = [(sb("mat%d" % i, [128, 128], F32, sc), Dep()) for i in range(NMAT)]
                    bgt = [(sb("bg%d" % i, [128, 512], F32, sc), Dep()) for i in range(NBG)]
                    yo = [sb("yo%d" % i, [128, S], BF16, sc) for i in range(2)]; d_yo = [Dep(), Dep()]
                    tott = sb("tott", [128, 64], F32, sc); d_tott = Dep()

                    def tok():
                        cnt["tok"] += 1
                        return tokt[cnt["tok"] % NTOK]

                    def mat():
                        cnt["mat"] += 1
                        return matt[cnt["mat"] % NMAT]

                    def bg():
                        cnt["bg"] += 1
                        return bgt[cnt["bg"] % NBG]

                    def evac(out, in_, rd, wr, scale=None, eng=None):
                        cnt["ev"] += 1
                        if eng is None:
                            eng = "act" if cnt["ev"] % 2 == 0 else "dve"
                        if eng == "act":
                            return act(out, in_, AF.Copy, rd, wr, scale=(1.0 if scale is None else scale))
                        if scale is None:
                            return cp("dve", out, in_, rd, wr)
                        return ts("dve", out, in_, scale, None, ALU.mult, None, rd, wr)

                    def mms(out, dout, terms, last_stop=True, first_start=True):
                        n = len(terms)
                        for i, (l, r, deps) in enumerate(terms):
                            mm(out, l, r, first_start and i == 0, last_stop and i == n - 1, deps, [dout])

                    def proj(col0, ncols, consume):
                        s = cnt["w"] % 2; cnt["w"] += 1
                        dma("pool", wbuf[s][:, :, 0:ncols], W_IN[:, col0:col0 + ncols].rearrange("(kc p) f -> p kc f", p=128),
                            [], [d_wbuf[s]], "abw%d" % s)
                        for tb in range(NTB):
                            bk, dbk_ = big()
                            for kc in range(8):
                                mm(bk[0:ncols, :], wbuf[s][:, kc, 0:ncols], hT[:, kc, tb * TB:(tb + 1) * TB], kc == 0, kc == 7,
                                   [d_wbuf[s], d_hT[kc]], [dbk_])
                            consume(tb, bk[0:ncols, :], dbk_)

                    def proj_to(col0, ncols, dst, func=AF.Copy, bias=0.0, extra_rd=()):
                        def c(tb, ps, dps):
                            act(dst.ap[0:ncols, tb * TB:(tb + 1) * TB], ps, func, [dps] + list(extra_rd), dst.deps, bias=bias)
                        proj(col0, ncols, c)

                    def shiftmix(raw, tmp, n, omu, hmu, dpar):
                        r = raw.ap; t = tmp.ap
                        tt("pool", t[0:n, 1:S - 1], r[0:n, 0:S - 2], r[0:n, 2:S], ALU.add, raw.deps, tmp.deps)
                        cp("pool", t[0:n, 0:1], r[0:n, 1:2], raw.deps, tmp.deps)
                        cp("pool", t[0:n, S - 1:S], r[0:n, S - 2:S - 1], raw.deps, tmp.deps)
                        ts("dve", r[0:n, :], r[0:n, :], omu, None, ALU.mult, None, raw.deps + [dpar], raw.deps)
                        stt("dve", r[0:n, :], t[0:n, :], hmu, r[0:n, :], ALU.mult, ALU.add, tmp.deps + raw.deps + [dpar], raw.deps)

                    def v3(ap, n):
                        return ap[0:n, :].rearrange("p (c t) -> p c t", t=32)

                    def cumsum(ld, cum, n):
                        P.op("dve", lambda e: e.tensor_tensor_scan(out=cum.ap[0:n, :], data0=m32[0:n, :],
                                                                   data1=ld.ap[0:n, :], initial=0.0, op0=ALU.mult, op1=ALU.add),
                             rd=ld.deps + [d_m32], wr=cum.deps)

                    def dir_arrays(dirn, n, LDa, CUMa, D2, D3, srcR, srcK, srcB, srcKK, wc, dwc):
                        cumsum(LDa, CUMa, n)
                        c3 = v3(CUMa.ap, n)
                        act(wc[0:n, :], c3[:, :, 31], AF.Exp, CUMa.deps, [dwc])
                        cp("dve", tott[0:n, :], c3[:, :, 31], CUMa.deps, [d_tott])
                        delta = srcB is not None
                        if dirn == 0:
                            act(D2.ap[0:n, :], CUMa.ap[0:n, :], AF.Exp, CUMa.deps, D2.deps)
                            tt("dve", D2.ap[0:n, :], D2.ap[0:n, :], srcR.ap[0:n, :], ALU.mult, D2.deps + srcR.deps, D2.deps)
                            act(D3.ap[0:n, :], CUMa.ap[0:n, :], AF.Exp, CUMa.deps, D3.deps, scale=-1.0)
                            if delta:
                                tt("dve", LDa.ap[0:n, :], CUMa.ap[0:n, :], LDa.ap[0:n, :], ALU.subtract, CUMa.deps + LDa.deps, LDa.deps)
                                act(LDa.ap[0:n, :], LDa.ap[0:n, :], AF.Exp, LDa.deps, LDa.deps)
                                tt("pool", LDa.ap[0:n, :], LDa.ap[0:n, :], srcKK.ap[0:n, :], ALU.mult, LDa.deps + srcKK.deps, LDa.deps)
                                tt("pool", CUMa.ap[0:n, :], D3.ap[0:n, :], srcB.ap[0:n, :], ALU.mult, D3.deps + srcB.deps + LDa.deps, CUMa.deps)
                            tt("dve", D3.ap[0:n, :], D3.ap[0:n, :], srcK.ap[0:n, :], ALU.mult, D3.deps + srcK.deps + CUMa.deps, D3.deps)
                            return D2, D3, CUMa, LDa
                        else:
                            tt("dve", c3, c3, tott[0:n, :, None].to_broadcast([n, 64, 32]), ALU.subtract, CUMa.deps + [d_tott], CUMa.deps)
                            if delta:
                                act(D2.ap[0:n, :], CUMa.ap[0:n, :], AF.Exp, CUMa.deps, D2.deps, scale=-1.0)
                                tt("pool", D2.ap[0:n, :], D2.ap[0:n, :], srcKK.ap[0:n, :], ALU.mult, D2.deps + srcKK.deps, D2.deps)
                            tt("dve", LDa.ap[0:n, :], LDa.ap[0:n, :], CUMa.ap[0:n, :], ALU.subtract, LDa.deps + CUMa.deps, LDa.deps)
                            act(D3.ap[0:n, :], LDa.ap[0:n, :], AF.Exp, LDa.deps, D3.deps)
                            tt("dve", D3.ap[0:n, :], D3.ap[0:n, :], srcR.ap[0:n, :], ALU.mult, D3.deps + srcR.deps, D3.deps)
                            act(LDa.ap[0:n, :], LDa.ap[0:n, :], AF.Exp, LDa.deps, LDa.deps, scale=-1.0)
                            if delta:
                                tt("pool", CUMa.ap[0:n, :], LDa.ap[0:n, :], srcB.ap[0:n, :], ALU.mult, LDa.deps + srcB.deps + D2.deps, CUMa.deps)
                            tt("dve", LDa.ap[0:n, :], LDa.ap[0:n, :], srcK.ap[0:n, :], ALU.mult, LDa.deps + srcK.deps + CUMa.deps, LDa.deps)
                            return D3, LDa, CUMa, D2

                    def scan(dk, delta, dirn, rT, kT_, vA, bT, aT, wc, dwc, osum, first):
                        fwd = dirn == 0
                        m_iu = IU if fwd else IL
                        m_su = SU if fwd else SLm
                        m_sl = SLm if fwd else SU
                        idk = identf[0:dk, 0:dk]
                        P.op("pool", lambda e: e.memset(STt[0][:], 0.0), wr=[d_ST[0]])
                        cur = 0
                        tiles = list(range(16)) if fwd else list(range(15, -1, -1))
                        chunks = list(range(4)) if fwd else list(range(3, -1, -1))

                        def tr(arr, n0):
                            reg, dreg = small()
                            P.op("pe", lambda e: e.transpose(reg[:, 0:dk], arr.ap[0:dk, n0:n0 + 128], idk), rd=arr.deps + [d_ident], wr=[dreg])
                            t, d = tok()
                            evac(t[:, 0:dk], reg[:, 0:dk], [dreg], [d])
                            return t, d

                        def mmev(terms, rows, cols, scale=None, mask=None):
                            reg, dreg = small()
                            mms(reg[0:rows, 0:cols], dreg, terms)
                            t, d = mat()
                            if mask is None:
                                evac(t[0:rows, 0:cols], reg[0:rows, 0:cols], [dreg], [d], scale=scale)
                            else:
                                stt("dve", t[0:rows, 0:cols], reg[0:rows, 0:cols], (1.0 if scale is None else scale), mask[0:rows, 0:cols],
                                    ALU.mult, ALU.mult, [dreg, d_msk], [d])
                            return t, d

                        for n in tiles:
                            n0 = n * 128
                            rs_ = rT.ap[0:dk, n0:n0 + 128]; ks_ = kT_.ap[0:dk, n0:n0 + 128]
                            ktok, d_ktok = tr(kT_, n0)
                            vtok, d_vtok = tr(vA, n0)
                            MrkT, d_mrk = mmev([(ks_, rs_, kT_.deps + rT.deps)], 128, 128, mask=m_iu)
                            vexp, d_vexp = bg()
                            tt("pool", vexp[:, 0:4 * dk].rearrange("p (c k) -> p c k", c=4), vtok[:, None, 0:dk].to_broadcast([128, 4, dk]),
                               CM[:, :, 0:dk], ALU.mult, [d_vtok, d_msk], [d_vexp])
                            hreg, d_hreg = big()
                            yr, d_yr = yreg()
                            if delta:
                                as_ = aT.ap[0:dk, n0:n0 + 128]; bs_ = bT.ap[0:dk, n0:n0 + 128]
                                btok, d_btok = tr(bT, n0)
                                atok, d_atok = tr(aT, n0)
                                rtok, d_rtok = tr(rT, n0)
                                N1, d_N1 = mmev([(as_, bs_, aT.deps + bT.deps)], 128, 128, scale=-1.0, mask=m_sl)
                                N1T, d_N1T = mmev([(bs_, as_, aT.deps + bT.deps)], 128, 128, scale=-1.0, mask=m_su)
                                Y, d_Y = mat()
                                tt("pool", Y[:, :], N1T[:, :], identf[:, :], ALU.add, [d_N1T, d_ident], [d_Y])
                                Np, d_Np, NpT, d_NpT = N1, d_N1, N1T, d_N1T
                                for lvl in range(4):
                                    N2, d_N2 = mmev([(NpT[:, :], Np[:, :], [d_Np, d_NpT])], 128, 128)
                                    if lvl < 3:
                                        N2T, d_N2T = mmev([(Np[:, :], NpT[:, :], [d_Np, d_NpT])], 128, 128)
                                    Y, d_Y = mmev([(identf[:, :], Y[:, :], [d_ident, d_Y]), (N2[:, :], Y[:, :], [d_N2, d_Y])], 128, 128)
                                    Np, d_Np, NpT, d_NpT = N2, d_N2, N2T, d_N2T
                                TT, d_TT = Y, d_Y
                                LakT, d_lak = mmev([(ks_, as_, kT_.deps + aT.deps)], 128, 128, mask=m_su)
                                Z, d_Z = mmev([(LakT[:, :], vtok[:, 0:dk], [d_lak, d_vtok])], 128, dk)
                                negP, d_negP = mmev([(TT[:, :], Z[:, 0:dk], [d_TT, d_Z])], 128, dk, scale=-1.0)
                                negTA, d_negTA = mmev([(TT[:, :], atok[:, 0:dk], [d_TT, d_atok])], 128, dk, scale=-1.0)
                                MrbT, d_mrb = mmev([(bs_, rs_, bT.deps + rT.deps)], 128, 128, mask=m_iu)
                                QT, d_QT = mmev([(rtok[:, 0:dk], identf[:, :], [d_rtok, d_ident]),
                                                 (negTA[:, 0:dk], MrbT[:, :], [d_negTA, d_mrb])], dk, 128)
                                pexp, d_pexp = bg()
                                tt("pool", pexp[:, 0:4 * dk].rearrange("p (c k) -> p c k", c=4), negP[:, None, 0:dk].to_broadcast([128, 4, dk]),
                                   CM[:, :, 0:dk], ALU.mult, [d_negP, d_msk], [d_pexp])
                                bexp, d_bexp = bg()
                                tt("pool", bexp[:, 0:4 * dk].rearrange("p (c k) -> p c k", c=4), btok[:, None, 0:dk].to_broadcast([128, 4, dk]),
                                   CM[:, :, 0:dk], ALU.mult, [d_btok, d_msk], [d_bexp])
                                greg, d_greg = big()
                                mms(greg[0:dk, 0:4 * dk], d_greg, [(idk, IEX[0:dk, :, 0:dk], [d_ident, d_msk]),
                                                                  (negTA[:, 0:dk], bexp[:, 0:4 * dk], [d_negTA, d_bexp])])
                                GT, d_GT = bg()
                                evac(GT[0:dk, 0:4 * dk], greg[0:dk, 0:4 * dk], [d_greg], [d_GT])
                                mms(hreg[0:dk, 0:4 * dk], d_hreg, [(ktok[:, 0:dk], vexp[:, 0:4 * dk], [d_ktok, d_vexp]),
                                                                  (btok[:, 0:dk], pexp[:, 0:4 * dk], [d_btok, d_pexp])])
                                mms(yr[0:dk, :], d_yr, [(vtok[:, 0:dk], MrkT[:, :], [d_vtok, d_mrk]),
                                                        (negP[:, 0:dk], MrbT[:, :], [d_negP, d_mrb])], last_stop=False)
                            else:
                                mms(hreg[0:dk, 0:4 * dk], d_hreg, [(ktok[:, 0:dk], vexp[:, 0:4 * dk], [d_ktok, d_vexp])])
                                mms(yr[0:dk, :], d_yr, [(vtok[:, 0:dk], MrkT[:, :], [d_vtok, d_mrk])], last_stop=False)
                            Hs, d_Hs = bg()
                            tt("dve", Hs[0:dk, 0:4 * dk].rearrange("p (c k) -> p c k", c=4), hreg[0:dk, 0:4 * dk].rearrange("p (c k) -> p c k", c=4),
                               wc[0:dk, n * 4:(n + 1) * 4][:, :, None].to_broadcast([dk, 4, dk]), ALU.mult, [d_hreg, dwc], [d_Hs])
                            for ci, c in enumerate(chunks):
                                cc = slice(c * 32, (c + 1) * 32)
                                stc = STt[cur]; dstc = d_ST[cur]
                                stn = STt[1 - cur]; dstn = d_ST[1 - cur]
                                if delta:
                                    mm(yr[0:dk, cc], stc[0:dk, 0:dk], QT[0:dk, cc], False, ci == 3, [dstc, d_QT], [d_yr])
                                    sr, d_sr = small()
                                    mm(sr[0:dk, 0:dk], GT[0:dk, c * dk:(c + 1) * dk], stc[0:dk, 0:dk], True, True, [d_GT, dstc], [d_sr])
                                    stt("dve", stn[0:dk, 0:dk], sr[0:dk, 0:dk], wc[0:dk, n * 4 + c:n * 4 + c + 1], Hs[0:dk, c * dk:(c + 1) * dk],
                                        ALU.mult, ALU.add, [d_sr, dwc, d_Hs], [dstn])
                                else:
                                    mm(yr[0:dk, cc], stc[0:dk, 0:dk], rT.ap[0:dk, n0 + c * 32:n0 + (c + 1) * 32], False, ci == 3,
                                       [dstc] + rT.deps, [d_yr])
                                    stt("dve", stn[0:dk, 0:dk], stc[0:dk, 0:dk], wc[0:dk, n * 4 + c:n * 4 + c + 1], Hs[0:dk, c * dk:(c + 1) * dk],
                                        ALU.mult, ALU.add, [dstc, dwc, d_Hs], [dstn])
                                cur = 1 - cur
                            if first:
                                evac(osum.ap[0:dk, n0:n0 + 128], yr[0:dk, :], [d_yr], osum.deps)
                            else:
                                tt("dve", osum.ap[0:dk, n0:n0 + 128], yr[0:dk, :], osum.ap[0:dk, n0:n0 + 128], ALU.add, [d_yr] + osum.deps, osum.deps)
                        if cur != 0:
                            pass

                    ycount = [0]

                    def y_out(src_ap, n, row0, rd):
                        s = ycount[0] % 2; ycount[0] += 1
                        return s

                    for h in range(cfg.get('hg_heads', 4)):
                        Qs, Vv, FG, OS, D0, D1, D2, D3 = SLT[0], SLT[1], SLT[2], SLT[3], SLT[4], SLT[5], SLT[6], SLT[7]
                        proj_to(h * 128, 128, Qs, AF.Silu)
                        proj_to(1536 + h * 128, 128, Vv, AF.Copy)
                        for dirn in range(cfg.get('dirs', 2)):
                            col = dirn * 4 + h
                            proj_to(512 + dirn * 512 + h * 128, 128, FG, AF.Sigmoid)
                            ts("dve", FG.ap, FG.ap, oml[:, col:col + 1], lbc[:, col:col + 1], ALU.mult, ALU.add, FG.deps + [d_lb], FG.deps)
                            act(D0.ap, FG.ap, AF.Ln, FG.deps, D0.deps)
                            ts("dve", FG.ap, FG.ap, -1.0, 1.0, ALU.mult, ALU.add, FG.deps, FG.deps)
                            w = dirn
                            rT_, kT2, _, _ = dir_arrays(dirn, 128, D0, D1, D2, D3, Qs, FG, None, None, WCt[w], d_WC[w])
                            scan(128, False, dirn, rT_, kT2, Vv, None, None, WCt[w], d_WC[w], OS, dirn == 0)
                        sqa = D0
                        act(sqa.ap, OS.ap, AF.Square, OS.deps, sqa.deps)
                        for tb in range(NTB):
                            bk, dbk_ = big()
                            mm(bk[:, :], ones32[:, :], sqa.ap[:, tb * TB:(tb + 1) * TB], True, True, [d_o32] + sqa.deps, [dbk_])
                            act(D1.ap[:, tb * TB:(tb + 1) * TB], bk[:, :], AF.Sqrt, [dbk_, d_eps], D1.deps, bias=epsc[:, 0:1], scale=1.0 / 128)
                        P.op("dve", lambda e, a=D1.ap: e.reciprocal(out=a, in_=a), rd=D1.deps, wr=D1.deps)
                        proj_to(2048 + h * 128, 128, D2, AF.Silu)
                        stt("dve", D1.ap, OS.ap, hng[:, 0:1], D1.ap, ALU.mult, ALU.mult, OS.deps + D1.deps + [d_hng], D1.deps)
                        s = ycount[0] % 2; ycount[0] += 1
                        tt("dve", yo[s][:, :], D1.ap, D2.ap, ALU.mult, D1.deps + D2.deps, [d_yo[s]])
                        dma("sp", ydv[h * 128:(h + 1) * 128, :], yo[s][:, :], [d_yo[s]], [d_yd], "b_yo%d" % s)

                    BO = 2560
                    WL = sb("WL", [128, S], BF16, sc); d_WL = Dep()
                    AL = sb("AL", [64, S], BF16, sc); d_AL = Dep()
                    GL = sb("GL", [128, S], BF16, sc); d_GL = Dep()
                    T0, T1 = SLT[8], SLT[9]
                    proj_to(BO + 1536, 128, T0)
                    shiftmix(T0, T1, 128, rwo2[:, 0:1], rwh2[:, 0:1], d_rwp2)
                    act(WL[:, :], T0.ap, AF.Tanh, T0.deps, [d_WL])
                    proj_to(BO + 1664, 64, T0)
                    shiftmix(T0, T1, 64, rwo2[0:64, 1:2], rwh2[0:64, 1:2], d_rwp2)
                    cp("dve", AL[:, :], T0.ap[0:64, :], T0.deps, [d_AL])
                    proj_to(BO + 1728, 128, T0)
                    shiftmix(T0, T1, 128, rwo2[:, 2:3], rwh2[:, 2:3], d_rwp2)
                    act(GL[:, :], T0.ap, AF.Sigmoid, T0.deps, [d_GL])
                    for h in range(cfg.get('rw_heads', 8)):
                        R, K, V, Bv, KK, OS, D0, D1, D2, D3 = SLT
                        pc = lambda i: rwp[:, h * 11 + i:h * 11 + i + 1]
                        hs = slice(h * 64, (h + 1) * 64)
                        for (dst, c0, mi) in ((R, 0, 0), (K, 512, 1), (V, 1024, 2)):
                            proj_to(BO + c0 + h * 64, 64, dst)
                            shiftmix(dst, D0, 64, rwo[:, h * 11 + mi:h * 11 + mi + 1], rwh[:, h * 11 + mi:h * 11 + mi + 1], d_rwp)
                        for tb in range(NTB):
                            bk, dbk_ = big()
                            mm(bk[0:64, :], ra2b[:, hs], AL[:, tb * TB:(tb + 1) * TB], True, True, [d_ra2, d_AL], [dbk_])
                            act(Bv.ap[0:64, tb * TB:(tb + 1) * TB], bk[0:64, :], AF.Sigmoid, [dbk_, d_rwp], Bv.deps, bias=pc(5))
                        ts("dve", KK.ap[0:64, :], K.ap[0:64, :], pc(6), None, ALU.mult, None, K.deps + [d_rwp], KK.deps)
                        act(D0.ap[0:64, :], KK.ap[0:64, :], AF.Square, KK.deps, D0.deps)
                        for tb in range(NTB):
                            bk, dbk_ = big()
                            mm(bk[0:64, :], ones32[0:64, 0:64], D0.ap[0:64, tb * TB:(tb + 1) * TB], True, True, [d_o32] + D0.deps, [dbk_])
                            act(D1.ap[0:64, tb * TB:(tb + 1) * TB], bk[0:64, :], AF.Sqrt, [dbk_], D1.deps)
                        ts("dve", D1.ap[0:64, :], D1.ap[0:64, :], 1e-12, None, ALU.max, None, D1.deps, D1.deps)
                        P.op("dve", lambda e, a=D1.ap[0:64, :]: e.reciprocal(out=a, in_=a), rd=D1.deps, wr=D1.deps)
                        tt("dve", KK.ap[0:64, :], KK.ap[0:64, :], D1.ap[0:64, :], ALU.mult, KK.deps + D1.deps, KK.deps)
                        ts("dve", D0.ap[0:64, :], Bv.ap[0:64, :], -1.0, pc(7), ALU.add, ALU.mult, Bv.deps + D0.deps + [d_rwp], D0.deps)
                        stt("dve", K.ap[0:64, :], D0.ap[0:64, :], 1.0, K.ap[0:64, :], ALU.add, ALU.mult, D0.deps + K.deps, K.deps)
                        tt("dve", Bv.ap[0:64, :], Bv.ap[0:64, :], KK.ap[0:64, :], ALU.mult, Bv.deps + KK.deps + D0.deps, Bv.deps)
                        for dirn in range(cfg.get('dirs', 2)):
                            for tb in range(NTB):
                                bk, dbk_ = big()
                                mm(bk[0:64, :], rw2b[dirn * 64:(dirn + 1) * 64, hs], WL[dirn * 64:(dirn + 1) * 64, tb * TB:(tb + 1) * TB], True, True,
                                   [d_rw2, d_WL], [dbk_])
                                act(D0.ap[0:64, tb * TB:(tb + 1) * TB], bk[0:64, :], AF.Sigmoid, [dbk_, d_rwp], D0.deps, bias=pc(3 + dirn))
                            ts("dve", D0.ap[0:64, :], D0.ap[0:64, :], -float(np.exp(-0.5)), None, ALU.mult, None, D0.deps, D0.deps)
                            w = dirn
                            rT_, kT2, bT2, aT2 = dir_arrays(dirn, 64, D0, D1, D2, D3, R, K, Bv, KK, WCt[w], d_WC[w])
                            scan(64, True, dirn, rT_, kT2, V, bT2, aT2, WCt[w], d_WC[w], OS, dirn == 0)
                        Y_ = OS
                        for tb in range(NTB):
                            tsl = slice(tb * TB, (tb + 1) * TB)
                            bk, dbk_ = big()
                            mm(bk[0:64, :], ones32[0:64, 0:64], Y_.ap[0:64, tsl], True, True, [d_o32] + Y_.deps, [dbk_])
                            stt("dve", D0.ap[0:64, tsl], bk[0:64, :], -1.0 / 64, Y_.ap[0:64, tsl], ALU.mult, ALU.add, [dbk_] + Y_.deps + D0.deps, D0.deps)
                        act(D1.ap[0:64, :], D0.ap[0:64, :], AF.Square, D0.deps + D1.deps, D1.deps)
                        for tb in range(NTB):
                            tsl = slice(tb * TB, (tb + 1) * TB)
                            bk, dbk_ = big()
                            mm(bk[0:64, :], ones32[0:64, 0:64], D1.ap[0:64, tsl], True, True, [d_o32] + D1.deps, [dbk_])
                            act(D2.ap[0:64, tsl], bk[0:64, :], AF.Sqrt, [dbk_, d_epsln] + D2.deps, D2.deps, bias=epsln[0:64, 0:1], scale=1.0 / 64)
                        P.op("dve", lambda e, a=D2.ap[0:64, :]: e.reciprocal(out=a, in_=a), rd=D2.deps, wr=D2.deps)
                        tt("dve", D0.ap[0:64, :], D0.ap[0:64, :], D2.ap[0:64, :], ALU.mult, D0.deps + D2.deps, D0.deps)
                        ts("dve", D0.ap[0:64, :], D0.ap[0:64, :], pc(9), pc(10), ALU.mult, ALU.add, D0.deps + [d_rwp], D0.deps)
                        stt("dve", D1.ap[0:64, :], R.ap[0:64, :], pc(8), K.ap[0:64, :], ALU.mult, ALU.mult, R.deps + K.deps + D1.deps + [d_rwp], D1.deps)
                        for tb in range(NTB):
                            tsl = slice(tb * TB, (tb + 1) * TB)
                            bk, dbk_ = big()
                            mm(bk[0:64, :], ones32[0:64, 0:64], D1.ap[0:64, tsl], True, True, [d_o32] + D1.deps, [dbk_])
                            tt("dve", D2.ap[0:64, tsl], bk[0:64, :], V.ap[0:64, tsl], ALU.mult, [dbk_] + V.deps + D2.deps, D2.deps)
                        tt("dve", D0.ap[0:64, :], D0.ap[0:64, :], D2.ap[0:64, :], ALU.add, D0.deps + D2.deps, D0.deps)
                        s = ycount[0] % 2; ycount[0] += 1
                        for tb in range(NTB):
                            tsl = slice(tb * TB, (tb + 1) * TB)
                            bk, dbk_ = big()
                            mm(bk[0:64, :], rg2b[:, hs], GL[:, tsl], True, True, [d_rg2, d_GL], [dbk_])
                            tt("dve", yo[s][0:64, tsl], bk[0:64, :], D0.ap[0:64, tsl], ALU.mult, [dbk_] + D0.deps, [d_yo[s]])
                        dma("sp", ydv[512 + h * 64:512 + (h + 1) * 64, :], yo[s][0:64, :], [d_yo[s]], [d_yd], "b_yo%d" % s)
                    P.op("dve", lambda e: e.memset(scr[:], 0.0), rd=alld, wr=list(dbk))
                xTv2 = xT.rearrange("(c p) t -> p c t", p=128)
                for c in range(8):
                    dma("sp", x[:, c, :], xTv2[:, c, :], [], dx[c], "x%d" % c)
            with ExitStack() as ph:
                P.fence()
                yT = sb("yTm", [128, 8, S], BF16, ph); d_yT = [Dep() for _ in range(8)]
                yv = ydv.rearrange("(c p) t -> p c t", p=128)
                for c in range(8):
                    dma("sp", yT[:, c, :], yv[:, c, :], [d_yd], [d_yT[c]], "b_yl%d" % c)
                if "ydbg" in dbg:
                    yd = dbg["ydbg"].rearrange("(c p) t -> p c t", p=128)
                    for c in range(8):
                        out_dmas.append(dma("pool", yd[:, c, :], yT[:, c, :], [d_yT[c]], [], "dbg_y"))
                out_proj(ph, dr["ab_w_out"], yT, d_yT, gm_ap)


        def dump_x(name):
            if name in dbg:
                v = dbg[name].rearrange("(c p) t -> p c t", p=128)
                for c in range(8):
                    out_dmas.append(dma("sp", v[:, c, :], x[:, c, :], dx[c], [], "dbg_" + name))

        for layer in range(2):
            if layer == 1 and cfg.get("attn", True):
                attn_mixer(layer)
            if layer == 0 and cfg.get("ab", True):
                ab_mixer(layer)
            dump_x("xmix%d" % layer)
            if cfg.get("moe", True):
                moe(layer)
            dump_x("xffn%d" % layer)

        with ExitStack() as ph:
            P.fence()
            sqb = [sb("fsq%d" % i, [128, S], BF16, ph) for i in range(2)]; d_sq = [Dep(), Dep()]
            rstd = sb("frstd", [128, S], F32, ph); d_rstd = [Dep() for _ in range(NTB)]
            ot = [sb("fot%d" % i, [128, S], F32, ph) for i in range(2)]; d_ot = [Dep(), Dep()]
            for c in range(8):
                s = c % 2
                act(sqb[s][:], x[:, c, :], AF.Square, dx[c], [d_sq[s]])
                for tb in range(NTB):
                    mm(banks[tb][:, :], ones_bf[:], sqb[s][:, tb * TB:(tb + 1) * TB], c == 0, c == 7, [d_ones, d_sq[s]], [dbk[tb]])
            for tb in range(NTB):
                act(rstd[:, tb * TB:(tb + 1) * TB], banks[tb][:, :], AF.Sqrt, [dbk[tb], d_eps], [d_rstd[tb]], bias=epsc[:, 0:1], scale=1.0 / D)
                P.op("dve", lambda e, tb=tb: e.reciprocal(out=rstd[:, tb * TB:(tb + 1) * TB], in_=rstd[:, tb * TB:(tb + 1) * TB]),
                     rd=[d_rstd[tb]], wr=[d_rstd[tb]])
            ov = outT.rearrange("(c p) t -> p c t", p=128)
            for c in range(8):
                s = c % 2
                stt("dve", ot[s][:], x[:, c, :], ng[:, 32 + c:33 + c], rstd[:], ALU.mult, ALU.mult, dx[c] + d_rstd + [d_ng], [d_ot[s]])
                out_dmas.append(dma("sp", ov[:, c, :], ot[s][:], [d_ot[s]], [], "out%d" % s))
        P.emit(final_waits=out_dmas)
    return nc, P


def _bucket_onehot():
    jp = np.arange(4096)
    rel = 2047 - jp
    nb = 16
    max_exact = 8
    bucket = np.where(rel > 0, nb, 0)
    n = np.abs(rel)
    nf = np.maximum(n, 1).astype(np.float32)
    large = max_exact + (np.log(nf / max_exact) / np.float32(np.log(128 / max_exact)) * (nb - max_exact)).astype(np.int32)
    large = np.minimum(large, nb - 1)
    bucket = bucket + np.where(n < max_exact, n, large)
    oh = np.zeros((32, 4096), np.float32)
    oh[bucket, jp] = 1.0
    return oh


def host_inputs(inp, cfg={}):
    f32 = np.float32
    def colT(v, n):
        return np.ascontiguousarray(np.asarray(v, f32).reshape(n, 128).T)
    shared = {}
    shared["ada_w"] = np.ascontiguousarray(inp["ada_w"], f32)
    shared["ada_bT"] = np.concatenate([colT(inp["ada_b"][l], 48) for l in range(2)], axis=1)
    shared["ngT"] = np.concatenate([colT(inp["norm_mix_g"][0], 8), colT(inp["norm_mix_g"][1], 8),
                                    colT(inp["norm_ffn_g"][0], 8), colT(inp["norm_ffn_g"][1], 8),
                                    colT(inp["final_norm_g"], 8)], axis=1)
    rwl = []
    for l in range(2):
        r = np.asarray(inp["router_w"][l], f32).reshape(8, 128, NE).transpose(1, 0, 2).reshape(128, 8 * NE)
        rwl.append(r)
    shared["rw"] = np.ascontiguousarray(np.stack(rwl))
    shared["rb"] = np.ascontiguousarray(np.broadcast_to(np.asarray(inp["router_b"], f32)[:, None, :], (2, 128, NE)))
    w1 = np.asarray(inp["moe_w1"], f32)
    w1p = np.empty_like(w1)
    w1v = w1p.reshape(2, NE, D, 8, 256)
    w1v[..., 0:128] = w1[..., 0::2].reshape(2, NE, D, 8, 128)
    w1v[..., 128:256] = w1[..., 1::2].reshape(2, NE, D, 8, 128)
    shared["w1"] = np.ascontiguousarray(w1p[:, :cfg.get("nexp", NE)])
    b1 = np.asarray(inp["moe_b1"], f32)
    b1g = b1[..., 0::2].reshape(2, NE, 8, 128)
    b1l = b1[..., 1::2].reshape(2, NE, 8, 128)
    b1c = np.concatenate([b1g, b1l], axis=2)
    shared["b1T"] = np.ascontiguousarray(b1c.transpose(0, 3, 1, 2).reshape(2, 128, NE * 16))
    shared["w2"] = np.ascontiguousarray(np.asarray(inp["moe_w2"], f32)[:, :cfg.get("nexp", NE)])
    shared["b2"] = np.ascontiguousarray(inp["moe_b2"], f32)
    shared["ident"] = np.eye(128, dtype=f32)
    selm = np.zeros((NE, NE, 128), f32)
    for e in range(NE):
        selm[e, e, :] = 1.0
    shared["sel"] = selm.reshape(NE, NE * 128)
    shared["attn_w_in"] = np.ascontiguousarray(inp["attn_w_in"][0], f32)
    shared["attn_w_out"] = np.ascontiguousarray(inp["attn_w_out"][0], f32)
    shared["lamb"] = np.ascontiguousarray(np.broadcast_to(np.asarray(inp["attn_lambda"][0], f32).reshape(1, 256), (128, 256)))
    shared["sublnT"] = np.ascontiguousarray(np.asarray(inp["attn_subln_g"][0], f32).reshape(128, 1))
    tabv = np.asarray(inp["rel_bias_table"], f32)
    shared["reltab"] = np.ascontiguousarray(tabv)
    cf = np.stack([tabv[15, :], tabv[31, :]], axis=1).reshape(1, 16)
    shared["cfar"] = np.ascontiguousarray(np.broadcast_to(cf, (128, 16)))
    shared["onehot"] = _bucket_onehot()
    shared["antiident"] = np.ascontiguousarray(np.eye(128, dtype=f32)[::-1])
    shared["ab_w_in"] = np.ascontiguousarray(inp["ab_w_in"][0], f32)
    shared["ab_w_out"] = np.ascontiguousarray(inp["ab_w_out"][0], f32)
    lb = np.asarray(inp["hgrn_lb"], f32)
    shared["lbT"] = np.ascontiguousarray(lb.reshape(2, 2, 4, 128).transpose(3, 0, 1, 2).reshape(128, 16))
    shared["hngT"] = np.ascontiguousarray(np.asarray(inp["hgrn_norm_g"][0], f32).reshape(128, 1))
    mu = np.asarray(inp["rwkv_mu"][0], f32)
    items = [mu[0:512], mu[512:1024], mu[1024:1536], inp["rwkv_w0"][0, 0], inp["rwkv_w0"][0, 1], inp["rwkv_a0"][0],
             inp["rwkv_k_k"][0], inp["rwkv_k_a"][0], inp["rwkv_r_k"][0], inp["rwkv_ln_g"][0], inp["rwkv_ln_b"][0]]
    it = np.stack([np.asarray(v, f32).reshape(8, 64) for v in items])
    shared["rwp"] = np.ascontiguousarray(it.transpose(2, 1, 0).reshape(64, 88))
    r2 = np.zeros((128, 3), f32)
    r2[:, 0] = mu[1536:1664]; r2[0:64, 1] = mu[1664:1728]; r2[:, 2] = mu[1728:1856]
    shared["rwp2"] = r2
    shared["rw2"] = np.ascontiguousarray(np.asarray(inp["rwkv_w2"][0], f32).reshape(128, 512))
    shared["ra2"] = np.ascontiguousarray(inp["rwkv_a2"][0], f32)
    shared["rg2"] = np.ascontiguousarray(inp["rwkv_g2"][0], f32)
    idx = np.arange(128)
    same = (idx[:, None] // 32) == (idx[None, :] // 32)
    IU = (same & (idx[:, None] <= idx[None, :])).astype(f32)
    SU = (same & (idx[:, None] < idx[None, :])).astype(f32)
    CMm = np.zeros((128, 4, 128), f32)
    for c_ in range(4):
        CMm[c_ * 32:(c_ + 1) * 32, c_, :] = 1.0
    IEX = np.broadcast_to(np.eye(128, dtype=f32)[:, None, :], (128, 4, 128))
    shared["masks"] = np.ascontiguousarray(np.concatenate([IU, SU, IU.T, SU.T, CMm.reshape(128, 512), IEX.reshape(128, 512)], axis=1))
    maps = []
    for b in range(8):
        m = dict(shared)
        m["xT"] = np.ascontiguousarray(np.asarray(inp["x"][b], f32).T)
        m["cT"] = colT(inp["c"][b], 8)
        maps.append(m)
    return maps


from concourse.bass_utils import run_bass_kernel_spmd


def kernel(**inputs):
    cfg = {}
    nc, P = build(cfg)
    maps = host_inputs(inputs, cfg)
    res = run_bass_kernel_spmd(nc, maps, core_ids=list(range(8)))
    out = np.stack([np.ascontiguousarray(np.asarray(res.results[b]["outT"]).T) for b in range(8)])
    return out.astype(np.float32)
```

```python
import numpy as np
import concourse.bass as bass
import concourse.mybir as mybir

F32 = mybir.dt.float32
BF16 = mybir.dt.bfloat16
ALU = mybir.AluOpType
AF = mybir.ActivationFunctionType
AX = mybir.AxisListType

ENGS = ("pe", "dve", "act", "pool", "sp")
SEM_CAP = 16000


class Dep:
    __slots__ = ("name", "w", "r")

    def __init__(self, name=""):
        self.name = name
        self.w = None
        self.r = []


class Op:
    __slots__ = ("eng", "fn", "deps", "sig", "ev", "isdma", "dkey")

    def __init__(self, eng, fn, isdma, dkey):
        self.eng = eng
        self.fn = fn
        self.deps = []
        self.sig = False
        self.ev = None
        self.isdma = isdma
        self.dkey = dkey


class Prog:
    def __init__(self, nc):
        self.nc = nc
        self.ops = {e: [] for e in ENGS}
        self.dma_cnt = {}
        self.fence_ops = []
        self.need = {}

    def fence(self):
        f = []
        for e in ENGS:
            for o in reversed(self.ops[e]):
                if not o.isdma:
                    f.append(o)
                    break
        last = {}
        for e in ENGS:
            for o in self.ops[e]:
                if o.isdma:
                    last[o.dkey] = o
        f.extend(last.values())
        self.fence_ops = f
        self.need = {e: True for e in ENGS}

    def op(self, eng, fn, rd=(), wr=(), dma=None):
        o = Op(eng, fn, dma is not None, dma)
        deps = []
        for d in rd:
            if d.w is not None:
                deps.append(d.w)
        for d in wr:
            if d.w is not None:
                deps.append(d.w)
            deps.extend(d.r)
        seen = set()
        for y in deps:
            if id(y) in seen or y is o:
                continue
            seen.add(id(y))
            if (not y.isdma) and (not o.isdma) and y.eng == eng == "pe":
                continue
            o.deps.append(y)
            y.sig = True
        if self.need.get(eng):
            self.need[eng] = False
            for y in self.fence_ops:
                if id(y) in seen or y is o:
                    continue
                seen.add(id(y))
                o.deps.append(y)
                y.sig = True
        for d in rd:
            d.r.append(o)
        for d in wr:
            d.w = o
            d.r = []
        if o.isdma:
            o.sig = True
        self.ops[eng].append(o)
        return o

    def emit(self, final_waits=()):
        nc = self.nc
        from contextlib import ExitStack
        nsig = {}
        for e in ENGS:
            c = 0
            for o in self.ops[e]:
                if o.isdma:
                    k = o.dkey
                    self.dma_cnt[k] = self.dma_cnt.get(k, 0) + 16
                    o.ev = ("dma:" + str(k), self.dma_cnt[k])
                elif o.sig:
                    o.ev = ("%s:%d" % (e, c // SEM_CAP), c % SEM_CAP + 1)
                    c += 1
            nsig[e] = c
        semnames = set()
        for e in ENGS:
            for o in self.ops[e]:
                if o.ev is not None:
                    semnames.add(o.ev[0])
        semnames = sorted(semnames)
        self.n_sems = len(semnames)
        self.semnames = semnames
        with ExitStack() as st:
            sems = {}
            for i, n in enumerate(semnames):
                sems[n] = st.enter_context(nc.semaphore("s%d" % i))
            block = st.enter_context(nc.Block())
            prog = self

            def run(ename, eng):
                waited = {}
                for o in prog.ops[ename]:
                    for y in o.deps:
                        s, v = y.ev
                        if waited.get(s, 0) < v:
                            eng.wait_ge(sems[s], v)
                            waited[s] = v
                    ins = o.fn(eng)
                    if o.ev is not None:
                        ins.then_inc(sems[o.ev[0]], 16 if o.isdma else 1)
                if ename == "sp":
                    for y in final_waits:
                        s, v = y.ev
                        if waited.get(s, 0) < v:
                            eng.wait_ge(sems[s], v)
                            waited[s] = v

            @block.tensor
            def _(eng):
                run("pe", eng)

            @block.vector
            def _(eng):
                run("dve", eng)

            @block.scalar
            def _(eng):
                run("act", eng)

            @block.gpsimd
            def _(eng):
                run("pool", eng)

            @block.sync
            def _(eng):
                run("sp", eng)


from contextlib import ExitStack
import numpy as np

S = 2048
D = 1024
NE = 32
TB = 512
NTB = S // TB


class Ctx:
    pass


def build(cfg):
    nc = bass.Bass("TRN2", target_bir_lowering=False)
    P = Prog(nc)
    dr = {}

    def din(name, shape, dt=F32):
        dr[name] = nc.dram_tensor(name, list(shape), dt, kind="ExternalInput").ap()
        return dr[name]

    xT = din("xT", [D, S])
    cT = din("cT", [128, 8])
    ada_w = din("ada_w", [2, D, 6 * D])
    ada_bT = din("ada_bT", [128, 96])
    ngT = din("ngT", [128, 40])
    rw = din("rw", [2, 128, 8 * NE])
    rb = din("rb", [2, 128, NE])
    w1 = din("w1", [2, cfg.get("nexp", NE), D, 2 * D])
    b1T = din("b1T", [2, 128, NE * 16])
    w2 = din("w2", [2, cfg.get("nexp", NE), D, D])
    b2 = din("b2", [2, NE, D])
    ident = din("ident", [128, 128])
    sel = din("sel", [NE, NE * 128])
    din("attn_w_in", [D, 3 * D])
    din("attn_w_out", [D, D])
    din("lamb", [128, 256])
    din("sublnT", [128, 1])
    din("cfar", [128, 16])
    din("reltab", [32, 8])
    din("onehot", [32, 4096])
    din("antiident", [128, 128])
    din("ab_w_in", [D, 4416])
    din("ab_w_out", [D, D])
    din("lbT", [128, 16])
    din("hngT", [128, 1])
    din("rwp", [64, 88])
    din("rwp2", [128, 3])
    din("rw2", [128, 512])
    din("ra2", [64, 512])
    din("rg2", [128, 512])
    din("masks", [128, 1536])
    outT = nc.dram_tensor("outT", [D, S], F32, kind="ExternalOutput").ap()
    dbg = {}
    for name, shape in cfg.get("dbg", {}).items():
        dbg[name] = nc.dram_tensor("dbg_" + name, list(shape), F32, kind="ExternalOutput").ap()

    out_dmas = []
    es = ExitStack()
    with es:
        uid = [0]

        def sb(name, shape, dt=F32, stack=es):
            uid[0] += 1
            return stack.enter_context(nc.sbuf_tensor("%s_%d" % (name, uid[0]), list(shape), dt))

        x = sb("x", [128, 8, S]); dx = [[Dep() for _ in range(NTB)] for _ in range(8)]
        banks = [es.enter_context(nc.psum_tensor("bank%d" % i, [128, 512], F32)) for i in range(8)]
        dbk = [Dep("bank%d" % i) for i in range(8)]
        ones_bf = sb("ones_bf", [128, 128], BF16); d_ones = Dep()
        identf = sb("identf", [128, 128]); d_ident = Dep()
        modT = sb("modT", [128, 96]); d_mod = Dep()
        ng = sb("ng", [128, 40]); d_ng = Dep()
        gsm = sb("gsm", [128, 40]); d_gs = Dep()
        epsc = sb("epsc", [128, 1]); d_eps = Dep()

        def dma(q, out, in_, rd, wr, key):
            return P.op(q, lambda e: e.dma_start(out=out, in_=in_), rd=rd, wr=wr, dma=key)

        def mm(out, lhsT, rhs, start, stop, rd, wr):
            return P.op("pe", lambda e: e.matmul(out, lhsT=lhsT, rhs=rhs, start=start, stop=stop), rd=rd, wr=wr)

        def act(out, in_, func, rd, wr, bias=0.0, scale=1.0):
            return P.op("act", lambda e: e.activation(out=out, in_=in_, func=func, bias=bias, scale=scale), rd=rd, wr=wr)

        def tt(eng, out, in0, in1, op, rd, wr):
            return P.op(eng, lambda e: e.tensor_tensor(out=out, in0=in0, in1=in1, op=op), rd=rd, wr=wr)

        def ts(eng, out, in0, s1, s2, op0, op1, rd, wr):
            if op1 is None:
                return P.op(eng, lambda e: e.tensor_scalar(out=out, in0=in0, scalar1=s1, scalar2=None, op0=op0), rd=rd, wr=wr)
            return P.op(eng, lambda e: e.tensor_scalar(out=out, in0=in0, scalar1=s1, scalar2=s2, op0=op0, op1=op1), rd=rd, wr=wr)

        def stt(eng, out, in0, scalar, in1, op0, op1, rd, wr):
            return P.op(eng, lambda e: e.scalar_tensor_tensor(out=out, in0=in0, scalar=scalar, in1=in1, op0=op0, op1=op1), rd=rd, wr=wr)

        def cp(eng, out, in_, rd, wr):
            return P.op(eng, lambda e: e.tensor_copy(out=out, in_=in_), rd=rd, wr=wr)

        xTv = xT.rearrange("(c p) t -> p c t", p=128)
        for c in range(8):
            dma("sp", x[:, c, :], xTv[:, c, :], [], dx[c], "x%d" % c)
        dma("sp", identf[:], ident[:, :], [], [d_ident], "c_ident")
        dma("sp", ng[:], ngT[:, :], [], [d_ng], "c_ng")
        P.op("pool", lambda e: e.memset(ones_bf[:], 1.0), wr=[d_ones])
        P.op("pool", lambda e: e.memset(epsc[:], 1e-6), wr=[d_eps])

        with ExitStack() as ph:
            P.fence()
            cs = sb("cs", [128, 8], F32, ph); d_cs = Dep()
            cb = sb("cb", [128, 8], BF16, ph); d_cb = Dep()
            abT = sb("abT", [128, 96], F32, ph); d_ab = Dep()
            wb = [sb("adaw%d" % i, [128, 8, 512], BF16, ph) for i in range(2)]
            d_wb = [Dep(), Dep()]
            dma("sp", cs[:], cT[:, :], [], [d_cs], "c_cs")
            dma("sp", abT[:], ada_bT[:, :], [], [d_ab], "c_ab")
            act(cb[:], cs[:], AF.Silu, [d_cs], [d_cb])
            it = 0
            for layer in range(2):
                for fc in range(12):
                    s = it % 2
                    src = ada_w[layer, :, fc * 512:(fc + 1) * 512].rearrange("(kc p) f -> p kc f", p=128)
                    dma("pool", wb[s][:], src, [], [d_wb[s]], "adaw%d" % s)
                    for j in range(4):
                        col = layer * 48 + fc * 4 + j
                        for kc in range(8):
                            mm(banks[0][:, col:col + 1], wb[s][:, kc, j * 128:(j + 1) * 128], cb[:, kc:kc + 1],
                               kc == 0, kc == 7, [d_wb[s], d_cb], [dbk[0]])
                    it += 1
            tt("dve", modT[:], banks[0][:, 0:96], abT[:], ALU.add, [dbk[0], d_ab], [d_mod])
            for layer in range(2):
                stt("dve", gsm[:, layer * 8:(layer + 1) * 8], modT[:, layer * 48 + 8:layer * 48 + 16], 1.0,
                    ng[:, layer * 8:(layer + 1) * 8], ALU.add, ALU.mult, [d_mod, d_ng], [d_gs])
                stt("dve", gsm[:, 16 + layer * 8:16 + (layer + 1) * 8], modT[:, layer * 48 + 32:layer * 48 + 40], 1.0,
                    ng[:, 16 + layer * 8:16 + (layer + 1) * 8], ALU.add, ALU.mult, [d_mod, d_ng], [d_gs])
            if "modT" in dbg:
                out_dmas.append(dma("sp", dbg["modT"][:, :], modT[:], [d_mod], [], "dbg_mod"))

        def allx():
            return [d for row in dx for d in row]

        def norm_mod(ph, gs_ap, sh_ap, hT, d_hT, router_layer=None, extra=None):
            with ExitStack() as loc:
                P.fence()
                sqb = [sb("sqb%d" % i, [128, S], BF16, loc) for i in range(2)]
                d_sq = [Dep(), Dep()]
                rstd = sb("rstd", [128, S], F32, loc); d_rstd = [Dep() for _ in range(NTB)]
                tmp = [sb("ntmp%d" % i, [128, S], F32, loc) for i in range(2)]
                d_tmp = [Dep(), Dep()]
                for c in range(8):
                    s = c % 2
                    act(sqb[s][:], x[:, c, :], AF.Square, dx[c], [d_sq[s]])
                    for tb in range(NTB):
                        mm(banks[tb][:, :], ones_bf[:], sqb[s][:, tb * TB:(tb + 1) * TB], c == 0, c == 7,
                           [d_ones, d_sq[s]], [dbk[tb]])
                for tb in range(NTB):
                    act(rstd[:, tb * TB:(tb + 1) * TB], banks[tb][:, :], AF.Sqrt, [dbk[tb], d_eps], [d_rstd[tb]],
                        bias=epsc[:, 0:1], scale=1.0 / D)
                    P.op("dve", lambda e, tb=tb: e.reciprocal(out=rstd[:, tb * TB:(tb + 1) * TB], in_=rstd[:, tb * TB:(tb + 1) * TB]),
                         rd=[d_rstd[tb]], wr=[d_rstd[tb]])
                if router_layer is not None:
                    h32 = [sb("h32_%d" % i, [128, S], F32, loc) for i in range(2)]
                    d_h32 = [Dep(), Dep()]
                    rw32, d_rw = extra
                for c in range(8):
                    s = c % 2
                    tt("dve", tmp[s][:], x[:, c, :], rstd[:], ALU.mult, dx[c] + d_rstd, [d_tmp[s]])
                    if router_layer is None:
                        act(hT[:, c, :], tmp[s][:], AF.Identity, [d_tmp[s], d_gs, d_mod], [d_hT[c]],
                            bias=sh_ap[:, c:c + 1], scale=gs_ap[:, c:c + 1])
                    else:
                        act(h32[s][:], tmp[s][:], AF.Identity, [d_tmp[s], d_gs, d_mod], [d_h32[s]],
                            bias=sh_ap[:, c:c + 1], scale=gs_ap[:, c:c + 1])
                        cp("pool", hT[:, c, :], h32[s][:], [d_h32[s]], [d_hT[c]])
                        for tb in range(NTB):
                            mm(banks[4 + tb][0:NE, :], rw32[:, c * NE:(c + 1) * NE], h32[s][:, tb * TB:(tb + 1) * TB],
                               c == 0, c == 7, [d_rw, d_h32[s]], [dbk[4 + tb]])

        def moe(layer):
            with ExitStack() as ph:
                P.fence()
                hT = sb("hT", [128, 8, S], BF16, ph); d_hT = [Dep() for _ in range(8)]
                rw32 = sb("rw32", [128, 8 * NE], F32, ph); d_rw = Dep()
                rbs = sb("rbs", [128, NE], F32, ph); d_rb = Dep()
                b1s = sb("b1s", [128, NE * 16], F32, ph); d_b1 = Dep()
                b2s = sb("b2s", [NE, D], BF16, ph); d_b2 = Dep()
                gatesT = sb("gatesT", [NE, S], BF16, ph); d_gT = Dep()
                selsb = sb("selsb", [NE, NE * 128], BF16, ph); d_sel = Dep()
                dma("pool", selsb[:], sel[:, :], [], [d_sel], "c_sel")
                dma("sp", rw32[:], rw[layer, :, :], [], [d_rw], "m_rw")
                dma("sp", rbs[:], rb[layer, :, :], [], [d_rb], "m_rb")
                dma("sp", b1s[:], b1T[layer, :, :], [], [d_b1], "m_b1")
                dma("pool", b2s[:], b2[layer, :, :], [], [d_b2], "m_b2")
                b1v = b1s[:].rearrange("p (e t c) -> p e t c", e=NE, t=2)
                ts("dve", b1v[:, :, 1, :], b1v[:, :, 1, :], 1.0, None, ALU.add, None, [d_b1], [d_b1])
                gs_ap = gsm[:, 16 + layer * 8:16 + (layer + 1) * 8]
                sh_ap = modT[:, layer * 48 + 24:layer * 48 + 32]
                gf_ap = modT[:, layer * 48 + 40:layer * 48 + 48]
                norm_mod(ph, gs_ap, sh_ap, hT, d_hT, router_layer=layer, extra=(rw32, d_rw))
                if ("hffn%d" % layer) in dbg:
                    pass
                with ExitStack() as loc:
                    P.fence()
                    lgT = sb("lgT", [NE, S], F32, loc); d_lgT = Dep()
                    lg = sb("lg", [128, 16, NE], F32, loc); d_lg = Dep()
                    m8 = sb("m8", [128, 16, 8], F32, loc); d_m8 = Dep()
                    msk = sb("msk", [128, 16, NE], F32, loc); d_msk = Dep()
                    ex = sb("ex", [128, 16, NE], F32, loc); d_ex = Dep()
                    zz = sb("zz", [128, 16], F32, loc); d_zz = Dep()
                    for tb in range(NTB):
                        act(lgT[:, tb * TB:(tb + 1) * TB], banks[4 + tb][0:NE, :], AF.Copy, [dbk[4 + tb]], [d_lgT])
                    for t in range(16):
                        P.op("pe", lambda e, t=t: e.transpose(banks[0][:, t * NE:(t + 1) * NE], lgT[:, t * 128:(t + 1) * 128], identf[0:NE, 0:NE]),
                             rd=[d_lgT, d_ident], wr=[dbk[0]])
                    tt("dve", lg[:], banks[0][:, :].rearrange("p (t e) -> p t e", e=NE),
                       rbs[:, None, :].to_broadcast([128, 16, NE]), ALU.add, [dbk[0], d_rb], [d_lg])
                    for t in range(16):
                        P.op("dve", lambda e, t=t: e.max(out=m8[:, t, :], in_=lg[:, t, :]), rd=[d_lg], wr=[d_m8])
                    tt("dve", msk[:], lg[:], m8[:, :, 3:4].to_broadcast([128, 16, NE]), ALU.is_ge, [d_lg, d_m8], [d_msk])
                    tt("dve", ex[:], lg[:], m8[:, :, 0:1].to_broadcast([128, 16, NE]), ALU.subtract, [d_lg, d_m8], [d_ex])
                    act(ex[:], ex[:], AF.Exp, [d_ex], [d_ex])
                    tt("dve", ex[:], ex[:], msk[:], ALU.mult, [d_ex, d_msk], [d_ex])
                    P.op("dve", lambda e: e.tensor_reduce(out=zz[:], in_=ex[:], axis=AX.X, op=ALU.add), rd=[d_ex], wr=[d_zz])
                    P.op("dve", lambda e: e.reciprocal(out=zz[:], in_=zz[:]), rd=[d_zz], wr=[d_zz])
                    tt("dve", ex[:], ex[:], zz[:, :, None].to_broadcast([128, 16, NE]), ALU.mult, [d_ex, d_zz], [d_ex])
                    if ("gates%d" % layer) in dbg:
                        out_dmas.append(dma("sp", dbg["gates%d" % layer].rearrange("(t p) e -> p t e", p=128), ex[:], [d_ex], [], "dbg_g"))
                    for t in range(16):
                        tb, o = t // 4, (t % 4) * 128
                        P.op("pe", lambda e, t=t, tb=tb, o=o: e.transpose(banks[4 + tb][0:NE, o:o + 128], ex[:, t, :], identf[:, :]),
                             rd=[d_ex, d_ident], wr=[dbk[4 + tb]])
                    for tb in range(NTB):
                        act(gatesT[:, tb * TB:(tb + 1) * TB], banks[4 + tb][0:NE, :], AF.Copy, [dbk[4 + tb]], [d_gT])
                with ExitStack() as loc:
                    P.fence()
                    actb = sb("actb", [128, 8, S], BF16, loc); d_act = [[Dep() for _ in range(NTB)] for _ in range(8)]
                    NR = 4
                    w1r = [sb("w1r%d" % i, [128, 8, 256], BF16, loc) for i in range(NR)]; d_w1 = [Dep() for _ in range(NR)]
                    w2r = [sb("w2r%d" % i, [128, 8, D], BF16, loc) for i in range(1)]; d_w2 = [Dep()]
                    gbc = [sb("gbc%d" % i, [128, S], BF16, loc) for i in range(2)]; d_gbc = [Dep(), Dep()]
                    NT = 2
                    xg = [sb("xg%d" % i, [128, TB], F32, loc) for i in range(NT)]; d_xg = [Dep() for _ in range(NT)]
                    sg = [sb("sg%d" % i, [128, TB], F32, loc) for i in range(NT)]; d_sg = [Dep() for _ in range(NT)]
                    xl = [sb("xl%d" % i, [128, TB], F32, loc) for i in range(NT)]; d_xl = [Dep() for _ in range(NT)]
                    uu = [sb("uu%d" % i, [128, TB], F32, loc) for i in range(NT)]; d_uu = [Dep() for _ in range(NT)]
                    nexp = cfg.get("nexp", NE)
                    pieces = [(e, j) for e in range(nexp) for j in range(8)]
                    def load_piece(n):
                        e, j = pieces[n]
                        s = n % NR
                        src = w1[layer, e, :, j * 256:(j + 1) * 256].rearrange("(kc p) f -> p kc f", p=128)
                        dma("pool", w1r[s][:], src, [], [d_w1[s]], "w1r%d" % s)
                    def load_w2(e):
                        s = 0
                        src = w2[layer, e, :, :].rearrange("(kc p) f -> p kc f", p=128)
                        dma("pool", w2r[s][:], src, [], [d_w2[s]], "w2r%d" % s)
                    for n in range(min(NR - 1, len(pieces))):
                        load_piece(n)
                    load_w2(0)
                    blk = 0
                    ob = 0
                    for e in range(nexp):
                        gs_ = e % 2
                        for tb in range(NTB):
                            mm(banks[6][:, :], selsb[:, e * 128:(e + 1) * 128], gatesT[:, tb * TB:(tb + 1) * TB], True, True,
                               [d_sel, d_gT], [dbk[6]])
                            act(gbc[gs_][:, tb * TB:(tb + 1) * TB], banks[6][:, :], AF.Copy, [dbk[6]], [d_gbc[gs_]], scale=1.0 / 1.702)
                        for j in range(8):
                            n = e * 8 + j
                            if n + NR - 1 < len(pieces):
                                load_piece(n + NR - 1)
                            s = n % NR
                            for tb in range(NTB):
                                pa, pb = (blk % 2) * 2, (blk % 2) * 2 + 1
                                k = blk % NT
                                for kc in range(8):
                                    mm(banks[pa][:, :], w1r[s][:, kc, 0:128], hT[:, kc, tb * TB:(tb + 1) * TB], kc == 0, kc == 7,
                                       [d_w1[s], d_hT[kc]], [dbk[pa]])
                                for kc in range(8):
                                    mm(banks[pb][:, :], w1r[s][:, kc, 128:256], hT[:, kc, tb * TB:(tb + 1) * TB], kc == 0, kc == 7,
                                       [d_w1[s], d_hT[kc]], [dbk[pb]])
                                ts("dve", xg[k][:], banks[pa][:, :], b1s[:, e * 16 + j:e * 16 + j + 1], 7.0, ALU.add, ALU.min,
                                   [dbk[pa], d_b1], [d_xg[k]])
                                act(sg[k][:], xg[k][:], AF.Silu, [d_xg[k]], [d_sg[k]], scale=1.702)
                                ts("dve", xl[k][:], banks[pb][:, :], b1s[:, e * 16 + 8 + j:e * 16 + 8 + j + 1], 8.0, ALU.add, ALU.min,
                                   [dbk[pb], d_b1], [d_xl[k]])
                                stt("dve", uu[k][:], xl[k][:], -6.0, sg[k][:], ALU.max, ALU.mult, [d_xl[k], d_sg[k]], [d_uu[k]])
                                tt("dve", actb[:, j, tb * TB:(tb + 1) * TB], uu[k][:], gbc[gs_][:, tb * TB:(tb + 1) * TB], ALU.mult,
                                   [d_uu[k], d_gbc[gs_]], [d_act[j][tb]])
                                blk += 1
                        s2 = 0
                        for i in range(8):
                            for tb in range(NTB):
                                bo = 4 + (ob % 2)
                                for j in range(8):
                                    mm(banks[bo][:, :], w2r[s2][:, j, i * 128:(i + 1) * 128], actb[:, j, tb * TB:(tb + 1) * TB],
                                       j == 0, j == 7, [d_w2[s2], d_act[j][tb]], [dbk[bo]])
                                stt("dve", x[:, i, tb * TB:(tb + 1) * TB], banks[bo][:, :], gf_ap[:, i:i + 1], x[:, i, tb * TB:(tb + 1) * TB],
                                    ALU.mult, ALU.add, [dbk[bo], d_mod, dx[i][tb]], [dx[i][tb]])
                                ob += 1
                        if e + 1 < nexp:
                            load_w2(e + 1)
                    for i in range(8):
                        for tb in range(NTB):
                            bo = 4 + (ob % 2)
                            mm(banks[bo][:, :], b2s[:, i * 128:(i + 1) * 128], gatesT[:, tb * TB:(tb + 1) * TB], True, True,
                               [d_b2, d_gT], [dbk[bo]])
                            stt("dve", x[:, i, tb * TB:(tb + 1) * TB], banks[bo][:, :], gf_ap[:, i:i + 1], x[:, i, tb * TB:(tb + 1) * TB],
                                ALU.mult, ALU.add, [dbk[bo], d_mod, dx[i][tb]], [dx[i][tb]])
                            ob += 1


        def attn_mixer(layer):
            LAM_INIT = 0.8 - 0.6 * float(np.exp(-0.3 * layer))
            with ExitStack() as ph:
                P.fence()
                hT = sb("hTa", [128, 8, S], BF16, ph); d_hT = [Dep() for _ in range(8)]
                yT = sb("yTa", [128, 8, S], BF16, ph); d_yT = [Dep() for _ in range(8)]
                norm_mod(ph, gsm[:, layer * 8:(layer + 1) * 8], modT[:, layer * 48:layer * 48 + 8], hT, d_hT)
                gm_ap = modT[:, layer * 48 + 16:layer * 48 + 24]
                with ExitStack() as loc:
                    P.fence()
                    lamb = sb("lamb", [128, 256], F32, loc); d_lam = Dep()
                    lsc = sb("lsc", [128, 8], F32, loc); d_lsc = Dep()
                    ltmp = sb("ltmp", [128, 128], F32, loc)
                    sgc = sb("sgc", [128, 1], F32, loc); d_sgc = Dep()
                    cfar = sb("cfar", [128, 16], F32, loc); d_cfar = Dep()
                    identb = sb("identb", [128, 128], BF16, loc); d_idb = Dep()
                    dma("sp", lamb[:], dr["lamb"][:, :], [], [d_lam], "a_lam")
                    dma("sp", sgc[:], dr["sublnT"][:, :], [], [d_sgc], "a_sg")
                    dma("sp", cfar[:], dr["cfar"][:, :], [], [d_cfar], "a_cf")
                    dma("pool", identb[:], dr["antiident"][:, :], [], [d_idb], "a_aid")
                    ts("dve", sgc[:], sgc[:], 1.0 - LAM_INIT, None, ALU.mult, None, [d_sgc], [d_sgc])
                    tt("dve", ltmp[:, 0:64], lamb[:, 0:64], lamb[:, 64:128], ALU.mult, [d_lam], [d_lsc])
                    tt("dve", ltmp[:, 64:128], lamb[:, 128:192], lamb[:, 192:256], ALU.mult, [d_lam], [d_lsc])
                    P.op("dve", lambda e: e.tensor_reduce(out=lsc[:, 0:2], in_=ltmp[:].rearrange("p (a b) -> p a b", a=2), axis=AX.X, op=ALU.add),
                         rd=[d_lsc], wr=[d_lsc])
                    act(lsc[:, 2:4], lsc[:, 0:2], AF.Exp, [d_lsc], [d_lsc])
                    tt("dve", lsc[:, 4:5], lsc[:, 3:4], lsc[:, 2:3], ALU.subtract, [d_lsc], [d_lsc])
                    ts("dve", lsc[:, 4:5], lsc[:, 4:5], -LAM_INIT, None, ALU.add, None, [d_lsc], [d_lsc])
                    gdr = nc.dram_tensor("gr_scratch", [8, 4096], F32)
                    d_gdr = Dep()
                    with ExitStack() as tbs:
                        P.fence()
                        tab = sb("tab", [32, 8], F32, tbs); d_tab = Dep()
                        oh = sb("oh", [32, 4096], F32, tbs); d_oh = Dep()
                        gsb = sb("gsb", [8, 4096], F32, tbs); d_gsb = Dep()
                        dma("sp", tab[:], dr["reltab"][:, :], [], [d_tab], "a_tab")
                        dma("sp", oh[:], dr["onehot"][:, :], [], [d_oh], "a_oh")
                        for cb_ in range(8):
                            mm(banks[7][0:8, :], tab[:, :], oh[:, cb_ * 512:(cb_ + 1) * 512], True, True, [d_tab, d_oh], [dbk[7]])
                            act(gsb[:, cb_ * 512:(cb_ + 1) * 512], banks[7][0:8, :], AF.Copy, [dbk[7]], [d_gsb])
                        dma("sp", gdr.ap()[:, :], gsb[:], [d_gsb], [d_gdr], "a_gdr")
                        if "gsb" in dbg:
                            out_dmas.append(dma("sp", dbg["gsb"][:, :], gsb[:], [d_gsb], [], "dbg_gsb"))
                    P.fence()
                    wq = sb("wq", [128, 8, 128], BF16, loc); d_wq = Dep()
                    wk = sb("wk", [128, 8, 128], BF16, loc); d_wk = Dep()
                    wv = sb("wv", [128, 8, 128], BF16, loc); d_wv = Dep()
                    qT = sb("qT", [128, S], BF16, loc); d_q = Dep()
                    kT = sb("kT", [128, S], BF16, loc); d_k = Dep()
                    vt = sb("vt", [128, 16, 128], BF16, loc); d_v = Dep()
                    bt = sb("bt", [128, 6, 512], BF16, loc); d_bt = Dep()
                    E = [sb("E%d" % i, [128, 512], BF16, loc) for i in range(3)]; d_E = [Dep() for _ in range(3)]
                    oh_ = sb("ohd", [128, S], F32, loc); d_ohd = [Dep() for _ in range(NTB)]
                    rz = sb("rz", [128, 512], F32, loc); d_rz = Dep()
                    o0 = sb("o0", [128, 512], F32, loc); d_o0 = Dep()
                    o1 = sb("o1", [128, 512], F32, loc); d_o1 = Dep()
                    sq = sb("asq", [128, S], BF16, loc); d_sq = Dep()
                    rs = sb("ars", [128, S], F32, loc); d_rs = [Dep() for _ in range(NTB)]
                    w_in = dr["attn_w_in"]
                    ei = 0
                    sbk = 0
                    for h in range(8):
                        for (wt, dw, c0) in ((wq, d_wq, h * 128), (wk, d_wk, 1024 + h * 128), (wv, d_wv, 2048 + h * 128)):
                            dma("pool", wt[:], w_in[:, c0:c0 + 128].rearrange("(kc p) f -> p kc f", p=128), [], [dw], "a_w%d" % c0)
                        for di in range(6):
                            delta = -128 + 128 * di
                            src = bass.AP(tensor=gdr, offset=h * 4096 + 2047 - delta - 127, ap=[[1, 128], [1, 512]])
                            dma("pool", bt[:, di, :], src, [d_gdr], [d_bt], "a_bt")
                        for tb in range(NTB):
                            for (wt, dw, dst, dd, sc_) in ((wq, d_wq, qT, d_q, 0.125), (wk, d_wk, kT, d_k, 1.0)):
                                bk = sbk % 2; sbk += 1
                                for kc in range(8):
                                    mm(banks[bk][:, :], wt[:, kc, :], hT[:, kc, tb * TB:(tb + 1) * TB], kc == 0, kc == 7, [dw, d_hT[kc]], [dbk[bk]])
                                act(dst[:, tb * TB:(tb + 1) * TB], banks[bk][:, :], AF.Copy, [dbk[bk]], [dd], scale=sc_)
                        for t in range(16):
                            bk = sbk % 2; sbk += 1
                            for kc in range(8):
                                mm(banks[bk][:, 0:128], hT[:, kc, t * 128:(t + 1) * 128], wv[:, kc, :], kc == 0, kc == 7, [d_wv, d_hT[kc]], [dbk[bk]])
                            cp("dve", vt[:, t, :], banks[bk][:, 0:128], [dbk[bk]], [d_v])
                        for qb in range(NTB):
                            for m in range(2):
                                bo, bz = 2 + 2 * m, 3 + 2 * m
                                for kt in range(16):
                                    delta = 128 * kt - 512 * qb
                                    near = -255 < delta < 639
                                    bk = sbk % 2; sbk += 1
                                    mm(banks[bk][:, :], kT[m * 64:(m + 1) * 64, kt * 128:(kt + 1) * 128], qT[m * 64:(m + 1) * 64, qb * TB:(qb + 1) * TB],
                                       True, not near, [d_k, d_q], [dbk[bk]])
                                    if near:
                                        di = (delta + 128) // 128
                                        mm(banks[bk][:, :], identb[:], bt[:, di, :], False, True, [d_idb, d_bt], [dbk[bk]])
                                    e_ = ei % 3; ei += 1
                                    if near:
                                        act(E[e_][:], banks[bk][:, :], AF.Exp, [dbk[bk]], [d_E[e_]])
                                    else:
                                        col = h * 2 + (1 if delta > 0 else 0)
                                        act(E[e_][:], banks[bk][:, :], AF.Exp, [dbk[bk], d_cfar], [d_E[e_]], bias=cfar[:, col:col + 1])
                                    mm(banks[bo][:, :], vt[:, kt, :], E[e_][:], kt == 0, kt == 15, [d_v, d_E[e_]], [dbk[bo]])
                                    mm(banks[bz][:, :], ones_bf[:], E[e_][:], kt == 0, kt == 15, [d_ones, d_E[e_]], [dbk[bz]])
                            P.op("dve", lambda e: e.reciprocal(out=rz[:], in_=banks[3][:, :]), rd=[dbk[3]], wr=[d_rz])
                            tt("dve", o0[:], banks[2][:, :], rz[:], ALU.mult, [dbk[2], d_rz], [d_o0])
                            P.op("dve", lambda e: e.reciprocal(out=rz[:], in_=banks[5][:, :]), rd=[dbk[5]], wr=[d_rz])
                            tt("dve", o1[:], banks[4][:, :], rz[:], ALU.mult, [dbk[4], d_rz], [d_o1])
                            stt("dve", oh_[:, qb * TB:(qb + 1) * TB], o1[:], lsc[:, 4:5], o0[:], ALU.mult, ALU.add, [d_o0, d_o1, d_lsc], [d_ohd[qb]])
                        if "ohd" in dbg and h == 7:
                            out_dmas.append(dma("sp", dbg["ohd"][:, :], oh_[:], d_ohd, [], "dbg_ohd"))
                        if "bt" in dbg and h == 7:
                            out_dmas.append(dma("pool", dbg["bt"][:, :], bt[:].rearrange("p a b -> p (a b)"), [d_bt], [], "dbg_bt"))
                            out_dmas.append(dma("pool", dbg["qT"][:, :], qT[:], [d_q], [], "dbg_qT"))
                            out_dmas.append(dma("pool", dbg["kT"][:, :], kT[:], [d_k], [], "dbg_kT"))
                            out_dmas.append(dma("pool", dbg["vt"][:, :], vt[:].rearrange("p a b -> p (a b)"), [d_v], [], "dbg_vt"))
                        act(sq[:], oh_[:], AF.Square, d_ohd, [d_sq])
                        for tb in range(NTB):
                            bk = 6 + tb % 2
                            mm(banks[bk][:, :], ones_bf[:], sq[:, tb * TB:(tb + 1) * TB], True, True, [d_ones, d_sq], [dbk[bk]])
                            act(rs[:, tb * TB:(tb + 1) * TB], banks[bk][:, :], AF.Sqrt, [dbk[bk], d_eps], [d_rs[tb]], bias=epsc[:, 0:1], scale=1.0 / 128)
                            P.op("dve", lambda e, tb=tb: e.reciprocal(out=rs[:, tb * TB:(tb + 1) * TB], in_=rs[:, tb * TB:(tb + 1) * TB]), rd=[d_rs[tb]], wr=[d_rs[tb]])
                        stt("dve", yT[:, h, :], oh_[:], sgc[:, 0:1], rs[:], ALU.mult, ALU.mult, d_ohd + d_rs + [d_sgc], [d_yT[h]])
                out_proj(ph, dr["attn_w_out"], yT, d_yT, gm_ap)

        def out_proj(ph, w_out, yT, d_yT, gm_ap):
            with ExitStack() as loc:
                P.fence()
                wo = [sb("wo%d" % i, [128, 8, 128], BF16, loc) for i in range(2)]; d_wo = [Dep(), Dep()]
                ob = 0
                for i in range(8):
                    s = i % 2
                    dma("pool", wo[s][:], w_out[:, i * 128:(i + 1) * 128].rearrange("(kc p) f -> p kc f", p=128), [], [d_wo[s]], "wo%d" % s)
                    for tb in range(NTB):
                        bo = ob % 2; ob += 1
                        for kc in range(8):
                            mm(banks[bo][:, :], wo[s][:, kc, :], yT[:, kc, tb * TB:(tb + 1) * TB], kc == 0, kc == 7, [d_wo[s], d_yT[kc]], [dbk[bo]])
                        stt("dve", x[:, i, tb * TB:(tb + 1) * TB], banks[bo][:, :], gm_ap[:, i:i + 1], x[:, i, tb * TB:(tb + 1) * TB],
                            ALU.mult, ALU.add, [dbk[bo], d_mod, dx[i][tb]], [dx[i][tb]])

        class Arr:
            def __init__(self, ap, deps):
                self.ap = ap
                self.deps = deps

        def ab_mixer(layer):
            W_IN = dr["ab_w_in"]
            ydram = nc.dram_tensor("y_scratch", [D, S], BF16)
            ydv = ydram.ap()
            d_yd = Dep()
            gm_ap = modT[:, layer * 48 + 16:layer * 48 + 24]
            with ExitStack() as ph:
                P.fence()
                hT = sb("hTm", [128, 8, S], BF16, ph); d_hT = [Dep() for _ in range(8)]
                norm_mod(ph, gsm[:, layer * 8:(layer + 1) * 8], modT[:, layer * 48:layer * 48 + 8], hT, d_hT)
                with ExitStack() as sc:
                    P.fence()
                    d_big = [Dep(), Dep()]
                    bigs = [(banks[0], d_big[0]), (banks[1], d_big[1])]
                    smalls = [(banks[b][:, 0:128], Dep()) for b in (2, 3, 4, 5)]
                    yregs = [(banks[b][:, 0:128], Dep()) for b in (6, 7)]
                    alld = d_big + [d for _, d in smalls] + [d for _, d in yregs]
                    scr = sb("barscr", [128, 1], F32, sc)
                    P.op("dve", lambda e: e.memset(scr[:], 0.0), rd=list(dbk), wr=alld)
                    cnt = {"big": 0, "small": 0, "y": 0, "ev": 0, "w": 0, "tok": 0, "mat": 0, "bg": 0}

                    def big():
                        cnt["big"] += 1
                        return bigs[cnt["big"] % 2]

                    def small():
                        cnt["small"] += 1
                        return smalls[cnt["small"] % 4]

                    def yreg():
                        cnt["y"] += 1
                        return yregs[cnt["y"] % 2]

                    msk = sb("msk", [128, 1536], F32, sc); d_msk = Dep()
                    dma("sp", msk[:], dr["masks"][:, :], [], [d_msk], "b_msk")
                    IU = msk[:, 0:128]; SU = msk[:, 128:256]; IL = msk[:, 256:384]; SLm = msk[:, 384:512]
                    CM = msk[:, 512:1024].rearrange("p (c k) -> p c k", c=4)
                    IEX = msk[:, 1024:1536].rearrange("p (c k) -> p c k", c=4)
                    ones32 = sb("ones32", [128, 128], F32, sc); d_o32 = Dep()
                    P.op("pool", lambda e: e.memset(ones32[:], 1.0), wr=[d_o32])
                    epsln = sb("epsln", [128, 1], F32, sc); d_epsln = Dep()
                    P.op("pool", lambda e: e.memset(epsln[:], 64e-5), wr=[d_epsln])
                    m32 = sb("m32", [128, S], BF16, sc); d_m32 = Dep()
                    P.op("pool", lambda e: e.memset(m32[:], 1.0), wr=[d_m32])
                    P.op("pool", lambda e: e.memset(m32[:].rearrange("p (c t) -> p c t", t=32)[:, :, 0:1], 0.0), rd=[d_m32], wr=[d_m32])
                    lbT = sb("lbT", [128, 16], F32, sc); d_lb = Dep()
                    lbc = sb("lbc", [128, 8], F32, sc); oml = sb("oml", [128, 8], F32, sc)
                    dma("sp", lbT[:], dr["lbT"][:, :], [], [d_lb], "b_lb")
                    lbv = lbT[:].rearrange("p (d s h) -> p d s h", d=2, s=2)
                    tt("dve", lbc[:].rearrange("p (d h) -> p d h", d=2), lbv[:, :, 0, :], lbv[:, :, 1, :], ALU.subtract, [d_lb], [d_lb])
                    act(lbc[:], lbc[:], AF.Sigmoid, [d_lb], [d_lb])
                    ts("dve", oml[:], lbc[:], -1.0, 1.0, ALU.mult, ALU.add, [d_lb], [d_lb])
                    hng = sb("hng", [128, 1], F32, sc); d_hng = Dep()
                    dma("sp", hng[:], dr["hngT"][:, :], [], [d_hng], "b_hng")
                    rwp = sb("rwp", [64, 88], F32, sc); d_rwp = Dep()
                    rwo = sb("rwo", [64, 88], F32, sc); rwh = sb("rwh", [64, 88], F32, sc)
                    dma("sp", rwp[:], dr["rwp"][:, :], [], [d_rwp], "b_rwp")
                    ts("dve", rwo[:], rwp[:], -1.0, 1.0, ALU.mult, ALU.add, [d_rwp], [d_rwp])
                    ts("dve", rwh[:], rwp[:], 0.5, None, ALU.mult, None, [d_rwp], [d_rwp])
                    rwp2 = sb("rwp2", [128, 3], F32, sc); d_rwp2 = Dep()
                    rwo2 = sb("rwo2", [128, 3], F32, sc); rwh2 = sb("rwh2", [128, 3], F32, sc)
                    dma("sp", rwp2[:], dr["rwp2"][:, :], [], [d_rwp2], "b_rwp2")
                    ts("dve", rwo2[:], rwp2[:], -1.0, 1.0, ALU.mult, ALU.add, [d_rwp2], [d_rwp2])
                    ts("dve", rwh2[:], rwp2[:], 0.5, None, ALU.mult, None, [d_rwp2], [d_rwp2])
                    rw2b = sb("rw2b", [128, 512], BF16, sc); d_rw2 = Dep()
                    ra2b = sb("ra2b", [64, 512], BF16, sc); d_ra2 = Dep()
                    rg2b = sb("rg2b", [128, 512], BF16, sc); d_rg2 = Dep()
                    dma("pool", rw2b[:], dr["rw2"][:, :], [], [d_rw2], "b_rw2")
                    dma("pool", ra2b[:], dr["ra2"][:, :], [], [d_ra2], "b_ra2")
                    dma("pool", rg2b[:], dr["rg2"][:, :], [], [d_rg2], "b_rg2")

                    wbx = [sb("wbx%d" % i, [128, S], F32, sc) for i in range(2)]
                    SLT = [Arr(x[:, i, :], dx[i]) for i in range(8)] + [Arr(wbx[i][:], [Dep()]) for i in range(2)]
                    wbuf = [sb("abwb%d" % i, [128, 8, 128], BF16, sc) for i in range(2)]; d_wbuf = [Dep(), Dep()]
                    WCt = [sb("WC%d" % i, [128, 64], F32, sc) for i in range(2)]; d_WC = [Dep(), Dep()]
                    STt = [sb("ST%d" % i, [128, 128], F32, sc) for i in range(2)]; d_ST = [Dep(), Dep()]
                    NTOK, NMAT, NBG = 10, 44, 10
                    tokt = [(sb("tok%d" % i, [128, 128], F32, sc), Dep()) for i in range(NTOK)]
                    matt = [(sb("mat%d" % i, [128, 128], F32, sc), Dep()) for i in range(NMAT)]
                    bgt = [(sb("bg%d" % i, [128, 512], F32, sc), Dep()) for i in range(NBG)]
                    yo = [sb("yo%d" % i, [128, S], BF16, sc) for i in range(2)]; d_yo = [Dep(), Dep()]
                    tott = sb("tott", [128, 64], F32, sc); d_tott = Dep()

                    def tok():
                        cnt["tok"] += 1
                        return tokt[cnt["tok"] % NTOK]

                    def mat():
                        cnt["mat"] += 1
                        return matt[cnt["mat"] % NMAT]

                    def bg():
                        cnt["bg"] += 1
                        return bgt[cnt["bg"] % NBG]

                    def evac(out, in_, rd, wr, scale=None, eng=None):
                        cnt["ev"] += 1
                        if eng is None:
                            eng = "act" if cnt["ev"] % 2 == 0 else "dve"
                        if eng == "act":
                            return act(out, in_, AF.Copy, rd, wr, scale=(1.0 if scale is None else scale))
                        if scale is None:
                            return cp("dve", out, in_, rd, wr)
                        return ts("dve", out, in_, scale, None, ALU.mult, None, rd, wr)

                    def mms(out, dout, terms, last_stop=True, first_start=True):
                        n = len(terms)
                        for i, (l, r, deps) in enumerate(terms):
                            mm(out, l, r, first_start and i == 0, last_stop and i == n - 1, deps, [dout])

                    def proj(col0, ncols, consume):
                        s = cnt["w"] % 2; cnt["w"] += 1
                        dma("pool", wbuf[s][:, :, 0:ncols], W_IN[:, col0:col0 + ncols].rearrange("(kc p) f -> p kc f", p=128),
                            [], [d_wbuf[s]], "abw%d" % s)
                        for tb in range(NTB):
                            bk, dbk_ = big()
                            for kc in range(8):
                                mm(bk[0:ncols, :], wbuf[s][:, kc, 0:ncols], hT[:, kc, tb * TB:(tb + 1) * TB], kc == 0, kc == 7,
                                   [d_wbuf[s], d_hT[kc]], [dbk_])
                            consume(tb, bk[0:ncols, :], dbk_)

                    def proj_to(col0, ncols, dst, func=AF.Copy, bias=0.0, extra_rd=()):
                        def c(tb, ps, dps):
                            act(dst.ap[0:ncols, tb * TB:(tb + 1) * TB], ps, func, [dps] + list(extra_rd), dst.deps, bias=bias)
                        proj(col0, ncols, c)

                    def shiftmix(raw, tmp, n, omu, hmu, dpar):
                        r = raw.ap; t = tmp.ap
                        tt("pool", t[0:n, 1:S - 1], r[0:n, 0:S - 2], r[0:n, 2:S], ALU.add, raw.deps, tmp.deps)
                        cp("pool", t[0:n, 0:1], r[0:n, 1:2], raw.deps, tmp.deps)
                        cp("pool", t[0:n, S - 1:S], r[0:n, S - 2:S - 1], raw.deps, tmp.deps)
                        ts("dve", r[0:n, :], r[0:n, :], omu, None, ALU.mult, None, raw.deps + [dpar], raw.deps)
                        stt("dve", r[0:n, :], t[0:n, :], hmu, r[0:n, :], ALU.mult, ALU.add, tmp.deps + raw.deps + [dpar], raw.deps)

                    def v3(ap, n):
                        return ap[0:n, :].rearrange("p (c t) -> p c t", t=32)

                    def cumsum(ld, cum, n):
                        P.op("dve", lambda e: e.tensor_tensor_scan(out=cum.ap[0:n, :], data0=m32[0:n, :],
                                                                   data1=ld.ap[0:n, :], initial=0.0, op0=ALU.mult, op1=ALU.add),
                             rd=ld.deps + [d_m32], wr=cum.deps)

                    def dir_arrays(dirn, n, LDa, CUMa, D2, D3, srcR, srcK, srcB, srcKK, wc, dwc):
                        cumsum(LDa, CUMa, n)
                        c3 = v3(CUMa.ap, n)
                        act(wc[0:n, :], c3[:, :, 31], AF.Exp, CUMa.deps, [dwc])
                        cp("dve", tott[0:n, :], c3[:, :, 31], CUMa.deps, [d_tott])
                        delta = srcB is not None
                        if dirn == 0:
                            act(D2.ap[0:n, :], CUMa.ap[0:n, :], AF.Exp, CUMa.deps, D2.deps)
                            tt("dve", D2.ap[0:n, :], D2.ap[0:n, :], srcR.ap[0:n, :], ALU.mult, D2.deps + srcR.deps, D2.deps)
                            act(D3.ap[0:n, :], CUMa.ap[0:n, :], AF.Exp, CUMa.deps, D3.deps, scale=-1.0)
                            if delta:
                                tt("dve", LDa.ap[0:n, :], CUMa.ap[0:n, :], LDa.ap[0:n, :], ALU.subtract, CUMa.deps + LDa.deps, LDa.deps)
                                act(LDa.ap[0:n, :], LDa.ap[0:n, :], AF.Exp, LDa.deps, LDa.deps)
                                tt("pool", LDa.ap[0:n, :], LDa.ap[0:n, :], srcKK.ap[0:n, :], ALU.mult, LDa.deps + srcKK.deps, LDa.deps)
                                tt("pool", CUMa.ap[0:n, :], D3.ap[0:n, :], srcB.ap[0:n, :], ALU.mult, D3.deps + srcB.deps + LDa.deps, CUMa.deps)
                            tt("dve", D3.ap[0:n, :], D3.ap[0:n, :], srcK.ap[0:n, :], ALU.mult, D3.deps + srcK.deps + CUMa.deps, D3.deps)
                            return D2, D3, CUMa, LDa
                        else:
                            tt("dve", c3, c3, tott[0:n, :, None].to_broadcast([n, 64, 32]), ALU.subtract, CUMa.deps + [d_tott], CUMa.deps)
                            if delta:
                                act(D2.ap[0:n, :], CUMa.ap[0:n, :], AF.Exp, CUMa.deps, D2.deps, scale=-1.0)
                                tt("pool", D2.ap[0:n, :], D2.ap[0:n, :], srcKK.ap[0:n, :], ALU.mult, D2.deps + srcKK.deps, D2.deps)
                            tt("dve", LDa.ap[0:n, :], LDa.ap[0:n, :], CUMa.ap[0:n, :], ALU.subtract, LDa.deps + CUMa.deps, LDa.deps)
                            act(D3.ap[0:n, :], LDa.ap[0:n, :], AF.Exp, LDa.deps, D3.deps)
                            tt("dve", D3.ap[0:n, :], D3.ap[0:n, :], srcR.ap[0:n, :], ALU.mult, D3.deps + srcR.deps, D3.deps)
                            act(LDa.ap[0:n, :], LDa.ap[0:n, :], AF.Exp, LDa.deps, LDa.deps, scale=-1.0)
                            if delta:
                                tt("pool", CUMa.ap[0:n, :], LDa.ap[0:n, :], srcB.ap[0:n, :], ALU.mult, LDa.deps + srcB.deps + D2.deps, CUMa.deps)
                            tt("dve", LDa.ap[0:n, :], LDa.ap[0:n, :], srcK.ap[0:n, :], ALU.mult, LDa.deps + srcK.deps + CUMa.deps, LDa.deps)
                            return D3, LDa, CUMa, D2

                    def scan(dk, delta, dirn, rT, kT_, vA, bT, aT, wc, dwc, osum, first):
                        fwd = dirn == 0
                        m_iu = IU if fwd else IL
                        m_su = SU if fwd else SLm
                        m_sl = SLm if fwd else SU
                        idk = identf[0:dk, 0:dk]
                        P.op("pool", lambda e: e.memset(STt[0][:], 0.0), wr=[d_ST[0]])
                        st = {"cur": 0, "seqdone": True}
                        tiles = list(range(16)) if fwd else list(range(15, -1, -1))
                        chunks = list(range(4)) if fwd else list(range(3, -1, -1))
                        res = {}

                        def tr(arr, n0, role, par):
                            reg, dreg = small()
                            P.op("pe", lambda e: e.transpose(reg[:, 0:dk], arr.ap[0:dk, n0:n0 + 128], idk), rd=arr.deps + [d_ident], wr=[dreg])
                            t, d = tokt[role * 2 + par]
                            evac(t[:, 0:dk], reg[:, 0:dk], [dreg], [d])
                            return t, d

                        def mmev(terms, rows, cols, role, par, scale=None, mask=None):
                            reg, dreg = small()
                            mms(reg[0:rows, 0:cols], dreg, terms)
                            t, d = matt[role * 2 + par]
                            if mask is None:
                                evac(t[0:rows, 0:cols], reg[0:rows, 0:cols], [dreg], [d], scale=scale)
                            else:
                                stt("dve", t[0:rows, 0:cols], reg[0:rows, 0:cols], (1.0 if scale is None else scale), mask[0:rows, 0:cols],
                                    ALU.mult, ALU.mult, [dreg, d_msk], [d])
                            return t, d

                        def prep(n, par):
                            n0 = n * 128
                            rs_ = rT.ap[0:dk, n0:n0 + 128]; ks_ = kT_.ap[0:dk, n0:n0 + 128]
                            ktok, d_ktok = tr(kT_, n0, 0, par)
                            vtok, d_vtok = tr(vA, n0, 1, par)
                            yield
                            MrkT, d_mrk = mmev([(ks_, rs_, kT_.deps + rT.deps)], 128, 128, 0, par, mask=m_iu)
                            vexp, d_vexp = bgt[0 * 2 + par]
                            tt("pool", vexp[:, 0:4 * dk].rearrange("p (c k) -> p c k", c=4), vtok[:, None, 0:dk].to_broadcast([128, 4, dk]),
                               CM[:, :, 0:dk], ALU.mult, [d_vtok, d_msk], [d_vexp])
                            yield
                            QT = d_QT = GT = d_GT = None
                            if delta:
                                as_ = aT.ap[0:dk, n0:n0 + 128]; bs_ = bT.ap[0:dk, n0:n0 + 128]
                                btok, d_btok = tr(bT, n0, 2, par)
                                atok, d_atok = tr(aT, n0, 3, par)
                                rtok, d_rtok = tr(rT, n0, 4, par)
                                yield
                                N1, d_N1 = mmev([(as_, bs_, aT.deps + bT.deps)], 128, 128, 1, par, scale=-1.0, mask=m_sl)
                                N1T, d_N1T = mmev([(bs_, as_, aT.deps + bT.deps)], 128, 128, 2, par, scale=-1.0, mask=m_su)
                                yield
                                Y, d_Y = matt[3 * 2 + par]
                                tt("pool", Y[:, :], N1T[:, :], identf[:, :], ALU.add, [d_N1T, d_ident], [d_Y])
                                Np, d_Np, NpT, d_NpT = N1, d_N1, N1T, d_N1T
                                for lvl in range(4):
                                    N2, d_N2 = mmev([(NpT[:, :], Np[:, :], [d_Np, d_NpT])], 128, 128, 4 + 3 * lvl, par)
                                    if lvl < 3:
                                        N2T, d_N2T = mmev([(Np[:, :], NpT[:, :], [d_Np, d_NpT])], 128, 128, 5 + 3 * lvl, par)
                                    yield
                                    Y, d_Y = mmev([(identf[:, :], Y[:, :], [d_ident, d_Y]), (N2[:, :], Y[:, :], [d_N2, d_Y])], 128, 128, 6 + 3 * lvl, par)
                                    yield
                                    Np, d_Np, NpT, d_NpT = N2, d_N2, N2T, d_N2T
                                TT, d_TT = Y, d_Y
                                LakT, d_lak = mmev([(ks_, as_, kT_.deps + aT.deps)], 128, 128, 16, par, mask=m_su)
                                MrbT, d_mrb = mmev([(bs_, rs_, bT.deps + rT.deps)], 128, 128, 20, par, mask=m_iu)
                                yield
                                Z, d_Z = mmev([(LakT[:, :], vtok[:, 0:dk], [d_lak, d_vtok])], 128, dk, 17, par)
                                negTA, d_negTA = mmev([(TT[:, :], atok[:, 0:dk], [d_TT, d_atok])], 128, dk, 19, par, scale=-1.0)
                                bexp, d_bexp = bgt[2 * 2 + par]
                                tt("pool", bexp[:, 0:4 * dk].rearrange("p (c k) -> p c k", c=4), btok[:, None, 0:dk].to_broadcast([128, 4, dk]),
                                   CM[:, :, 0:dk], ALU.mult, [d_btok, d_msk], [d_bexp])
                                yield
                                while not st["seqdone"]:
                                    yield
                                negP, d_negP = mmev([(TT[:, :], Z[:, 0:dk], [d_TT, d_Z])], 128, dk, 18, par, scale=-1.0)
                                QT, d_QT = mmev([(rtok[:, 0:dk], identf[:, :], [d_rtok, d_ident]),
                                                 (negTA[:, 0:dk], MrbT[:, :], [d_negTA, d_mrb])], dk, 128, 21, par)
                                yield
                                pexp, d_pexp = bgt[1 * 2 + par]
                                tt("pool", pexp[:, 0:4 * dk].rearrange("p (c k) -> p c k", c=4), negP[:, None, 0:dk].to_broadcast([128, 4, dk]),
                                   CM[:, :, 0:dk], ALU.mult, [d_negP, d_msk], [d_pexp])
                                greg, d_greg = big()
                                mms(greg[0:dk, 0:4 * dk], d_greg, [(idk, IEX[0:dk, :, 0:dk], [d_ident, d_msk]),
                                                                  (negTA[:, 0:dk], bexp[:, 0:4 * dk], [d_negTA, d_bexp])])
                                GT, d_GT = bgt[3 * 2 + par]
                                evac(GT[0:dk, 0:4 * dk], greg[0:dk, 0:4 * dk], [d_greg], [d_GT])
                                yield
                                hreg, d_hreg = big()
                                mms(hreg[0:dk, 0:4 * dk], d_hreg, [(ktok[:, 0:dk], vexp[:, 0:4 * dk], [d_ktok, d_vexp]),
                                                                  (btok[:, 0:dk], pexp[:, 0:4 * dk], [d_btok, d_pexp])])
                            else:
                                while not st["seqdone"]:
                                    yield
                                hreg, d_hreg = big()
                                mms(hreg[0:dk, 0:4 * dk], d_hreg, [(ktok[:, 0:dk], vexp[:, 0:4 * dk], [d_ktok, d_vexp])])
                            Hs, d_Hs = bgt[4 * 2 + par]
                            tt("dve", Hs[0:dk, 0:4 * dk].rearrange("p (c k) -> p c k", c=4), hreg[0:dk, 0:4 * dk].rearrange("p (c k) -> p c k", c=4),
                               wc[0:dk, n * 4:(n + 1) * 4][:, :, None].to_broadcast([dk, 4, dk]), ALU.mult, [d_hreg, dwc], [d_Hs])
                            yield
                            while not st["seqdone"]:
                                yield
                            yr, d_yr = yregs[par]
                            if delta:
                                mms(yr[0:dk, :], d_yr, [(vtok[:, 0:dk], MrkT[:, :], [d_vtok, d_mrk]),
                                                        (negP[:, 0:dk], MrbT[:, :], [d_negP, d_mrb])], last_stop=False)
                            else:
                                mms(yr[0:dk, :], d_yr, [(vtok[:, 0:dk], MrkT[:, :], [d_vtok, d_mrk])], last_stop=False)
                            res[n] = (QT, d_QT, GT, d_GT, Hs, d_Hs, yr, d_yr)

                        def seqs(ns):
                            for n in ns:
                                n0 = n * 128
                                QT, d_QT, GT, d_GT, Hs, d_Hs, yr, d_yr = res.pop(n)
                                for ci, c in enumerate(chunks):
                                    cc = slice(c * 32, (c + 1) * 32)
                                    cur = st["cur"]
                                    stc = STt[cur]; dstc = d_ST[cur]
                                    stn = STt[1 - cur]; dstn = d_ST[1 - cur]
                                    if delta:
                                        mm(yr[0:dk, cc], stc[0:dk, 0:dk], QT[0:dk, cc], False, ci == 3, [dstc, d_QT], [d_yr])
                                        sr, d_sr = small()
                                        mm(sr[0:dk, 0:dk], GT[0:dk, c * dk:(c + 1) * dk], stc[0:dk, 0:dk], True, True, [d_GT, dstc], [d_sr])
                                        stt("dve", stn[0:dk, 0:dk], sr[0:dk, 0:dk], wc[0:dk, n * 4 + c:n * 4 + c + 1], Hs[0:dk, c * dk:(c + 1) * dk],
                                            ALU.mult, ALU.add, [d_sr, dwc, d_Hs], [dstn])
                                    else:
                                        mm(yr[0:dk, cc], stc[0:dk, 0:dk], rT.ap[0:dk, n0 + c * 32:n0 + (c + 1) * 32], False, ci == 3,
                                           [dstc] + rT.deps, [d_yr])
                                        stt("dve", stn[0:dk, 0:dk], stc[0:dk, 0:dk], wc[0:dk, n * 4 + c:n * 4 + c + 1], Hs[0:dk, c * dk:(c + 1) * dk],
                                            ALU.mult, ALU.add, [dstc, dwc, d_Hs], [dstn])
                                    st["cur"] = 1 - cur
                                    yield
                                if first:
                                    evac(osum.ap[0:dk, n0:n0 + 128], yr[0:dk, :], [d_yr], osum.deps)
                                else:
                                    tt("dve", osum.ap[0:dk, n0:n0 + 128], yr[0:dk, :], osum.ap[0:dk, n0:n0 + 128], ALU.add, [d_yr] + osum.deps, osum.deps)
                                yield
                            st["seqdone"] = True

                        pairs = [tiles[i:i + 2] for i in range(0, 16, 2)]
                        prev = None
                        for pr in pairs + [None]:
                            gens = []
                            if prev is not None:
                                st["seqdone"] = False
                                gens.append(seqs(prev))
                            if pr is not None:
                                gens += [prep(n, i) for i, n in enumerate(pr)]
                            while gens:
                                for g in list(gens):
                                    try:
                                        next(g)
                                    except StopIteration:
                                        gens.remove(g)
                            prev = pr

                    ycount = [0]

                    def y_out(src_ap, n, row0, rd):
                        s = ycount[0] % 2; ycount[0] += 1
                        return s

                    for h in range(cfg.get('hg_heads', 4)):
                        Qs, Vv, FG, OS, D0, D1, D2, D3 = SLT[0], SLT[1], SLT[2], SLT[3], SLT[4], SLT[5], SLT[6], SLT[7]
                        proj_to(h * 128, 128, Qs, AF.Silu)
                        proj_to(1536 + h * 128, 128, Vv, AF.Copy)
                        for dirn in range(cfg.get('dirs', 2)):
                            col = dirn * 4 + h
                            proj_to(512 + dirn * 512 + h * 128, 128, FG, AF.Sigmoid)
                            ts("dve", FG.ap, FG.ap, oml[:, col:col + 1], lbc[:, col:col + 1], ALU.mult, ALU.add, FG.deps + [d_lb], FG.deps)
                            act(D0.ap, FG.ap, AF.Ln, FG.deps, D0.deps)
                            ts("dve", FG.ap, FG.ap, -1.0, 1.0, ALU.mult, ALU.add, FG.deps, FG.deps)
                            w = dirn
                            rT_, kT2, _, _ = dir_arrays(dirn, 128, D0, D1, D2, D3, Qs, FG, None, None, WCt[w], d_WC[w])
                            scan(128, False, dirn, rT_, kT2, Vv, None, None, WCt[w], d_WC[w], OS, dirn == 0)
                        sqa = D0
                        act(sqa.ap, OS.ap, AF.Square, OS.deps, sqa.deps)
                        for tb in range(NTB):
                            bk, dbk_ = big()
                            mm(bk[:, :], ones32[:, :], sqa.ap[:, tb * TB:(tb + 1) * TB], True, True, [d_o32] + sqa.deps, [dbk_])
                            act(D1.ap[:, tb * TB:(tb + 1) * TB], bk[:, :], AF.Sqrt, [dbk_, d_eps], D1.deps, bias=epsc[:, 0:1], scale=1.0 / 128)
                        P.op("dve", lambda e, a=D1.ap: e.reciprocal(out=a, in_=a), rd=D1.deps, wr=D1.deps)
                        proj_to(2048 + h * 128, 128, D2, AF.Silu)
                        stt("dve", D1.ap, OS.ap, hng[:, 0:1], D1.ap, ALU.mult, ALU.mult, OS.deps + D1.deps + [d_hng], D1.deps)
                        s = ycount[0] % 2; ycount[0] += 1
                        tt("dve", yo[s][:, :], D1.ap, D2.ap, ALU.mult, D1.deps + D2.deps, [d_yo[s]])
                        dma("sp", ydv[h * 128:(h + 1) * 128, :], yo[s][:, :], [d_yo[s]], [d_yd], "b_yo%d" % s)

                    BO = 2560
                    WL = sb("WL", [128, S], BF16, sc); d_WL = Dep()
                    AL = sb("AL", [64, S], BF16, sc); d_AL = Dep()
                    GL = sb("GL", [128, S], BF16, sc); d_GL = Dep()
                    T0, T1 = SLT[8], SLT[9]
                    proj_to(BO + 1536, 128, T0)
                    shiftmix(T0, T1, 128, rwo2[:, 0:1], rwh2[:, 0:1], d_rwp2)
                    act(WL[:, :], T0.ap, AF.Tanh, T0.deps, [d_WL])
                    proj_to(BO + 1664, 64, T0)
                    shiftmix(T0, T1, 64, rwo2[0:64, 1:2], rwh2[0:64, 1:2], d_rwp2)
                    cp("dve", AL[:, :], T0.ap[0:64, :], T0.deps, [d_AL])
                    proj_to(BO + 1728, 128, T0)
                    shiftmix(T0, T1, 128, rwo2[:, 2:3], rwh2[:, 2:3], d_rwp2)
                    act(GL[:, :], T0.ap, AF.Sigmoid, T0.deps, [d_GL])
                    for h in range(cfg.get('rw_heads', 8)):
                        R, K, V, Bv, KK, OS, D0, D1, D2, D3 = SLT
                        pc = lambda i: rwp[:, h * 11 + i:h * 11 + i + 1]
                        hs = slice(h * 64, (h + 1) * 64)
                        for (dst, c0, mi) in ((R, 0, 0), (K, 512, 1), (V, 1024, 2)):
                            proj_to(BO + c0 + h * 64, 64, dst)
                            shiftmix(dst, D0, 64, rwo[:, h * 11 + mi:h * 11 + mi + 1], rwh[:, h * 11 + mi:h * 11 + mi + 1], d_rwp)
                        for tb in range(NTB):
                            bk, dbk_ = big()
                            mm(bk[0:64, :], ra2b[:, hs], AL[:, tb * TB:(tb + 1) * TB], True, True, [d_ra2, d_AL], [dbk_])
                            act(Bv.ap[0:64, tb * TB:(tb + 1) * TB], bk[0:64, :], AF.Sigmoid, [dbk_, d_rwp], Bv.deps, bias=pc(5))
                        ts("dve", KK.ap[0:64, :], K.ap[0:64, :], pc(6), None, ALU.mult, None, K.deps + [d_rwp], KK.deps)
                        act(D0.ap[0:64, :], KK.ap[0:64, :], AF.Square, KK.deps, D0.deps)
                        for tb in range(NTB):
                            bk, dbk_ = big()
                            mm(bk[0:64, :], ones32[0:64, 0:64], D0.ap[0:64, tb * TB:(tb + 1) * TB], True, True, [d_o32] + D0.deps, [dbk_])
                            act(D1.ap[0:64, tb * TB:(tb + 1) * TB], bk[0:64, :], AF.Sqrt, [dbk_], D1.deps)
                        ts("dve", D1.ap[0:64, :], D1.ap[0:64, :], 1e-12, None, ALU.max, None, D1.deps, D1.deps)
                        P.op("dve", lambda e, a=D1.ap[0:64, :]: e.reciprocal(out=a, in_=a), rd=D1.deps, wr=D1.deps)
                        tt("dve", KK.ap[0:64, :], KK.ap[0:64, :], D1.ap[0:64, :], ALU.mult, KK.deps + D1.deps, KK.deps)
                        ts("dve", D0.ap[0:64, :], Bv.ap[0:64, :], -1.0, pc(7), ALU.add, ALU.mult, Bv.deps + D0.deps + [d_rwp], D0.deps)
                        stt("dve", K.ap[0:64, :], D0.ap[0:64, :], 1.0, K.ap[0:64, :], ALU.add, ALU.mult, D0.deps + K.deps, K.deps)
                        tt("dve", Bv.ap[0:64, :], Bv.ap[0:64, :], KK.ap[0:64, :], ALU.mult, Bv.deps + KK.deps + D0.deps, Bv.deps)
                        for dirn in range(cfg.get('dirs', 2)):
                            for tb in range(NTB):
                                bk, dbk_ = big()
                                mm(bk[0:64, :], rw2b[dirn * 64:(dirn + 1) * 64, hs], WL[dirn * 64:(dirn + 1) * 64, tb * TB:(tb + 1) * TB], True, True,
                                   [d_rw2, d_WL], [dbk_])
                                act(D0.ap[0:64, tb * TB:(tb + 1) * TB], bk[0:64, :], AF.Sigmoid, [dbk_, d_rwp], D0.deps, bias=pc(3 + dirn))
                            ts("dve", D0.ap[0:64, :], D0.ap[0:64, :], -float(np.exp(-0.5)), None, ALU.mult, None, D0.deps, D0.deps)
                            w = dirn
                            rT_, kT2, bT2, aT2 = dir_arrays(dirn, 64, D0, D1, D2, D3, R, K, Bv, KK, WCt[w], d_WC[w])
                            scan(64, True, dirn, rT_, kT2, V, bT2, aT2, WCt[w], d_WC[w], OS, dirn == 0)
                        Y_ = OS
                        for tb in range(NTB):
                            tsl = slice(tb * TB, (tb + 1) * TB)
                            bk, dbk_ = big()
                            mm(bk[0:64, :], ones32[0:64, 0:64], Y_.ap[0:64, tsl], True, True, [d_o32] + Y_.deps, [dbk_])
                            stt("dve", D0.ap[0:64, tsl], bk[0:64, :], -1.0 / 64, Y_.ap[0:64, tsl], ALU.mult, ALU.add, [dbk_] + Y_.deps + D0.deps, D0.deps)
                        act(D1.ap[0:64, :], D0.ap[0:64, :], AF.Square, D0.deps + D1.deps, D1.deps)
                        for tb in range(NTB):
                            tsl = slice(tb * TB, (tb + 1) * TB)
                            bk, dbk_ = big()
                            mm(bk[0:64, :], ones32[0:64, 0:64], D1.ap[0:64, tsl], True, True, [d_o32] + D1.deps, [dbk_])
                            act(D2.ap[0:64, tsl], bk[0:64, :], AF.Sqrt, [dbk_, d_epsln] + D2.deps, D2.deps, bias=epsln[0:64, 0:1], scale=1.0 / 64)
                        P.op("dve", lambda e, a=D2.ap[0:64, :]: e.reciprocal(out=a, in_=a), rd=D2.deps, wr=D2.deps)
                        tt("dve", D0.ap[0:64, :], D0.ap[0:64, :], D2.ap[0:64, :], ALU.mult, D0.deps + D2.deps, D0.deps)
                        ts("dve", D0.ap[0:64, :], D0.ap[0:64, :], pc(9), pc(10), ALU.mult, ALU.add, D0.deps + [d_rwp], D0.deps)
                        stt("dve", D1.ap[0:64, :], R.ap[0:64, :], pc(8), K.ap[0:64, :], ALU.mult, ALU.mult, R.deps + K.deps + D1.deps + [d_rwp], D1.deps)
                        for tb in range(NTB):
                            tsl = slice(tb * TB, (tb + 1) * TB)
                            bk, dbk_ = big()
                            mm(bk[0:64, :], ones32[0:64, 0:64], D1.ap[0:64, tsl], True, True, [d_o32] + D1.deps, [dbk_])
                            tt("dve", D2.ap[0:64, tsl], bk[0:64, :], V.ap[0:64, tsl], ALU.mult, [dbk_] + V.deps + D2.deps, D2.deps)
                        tt("dve", D0.ap[0:64, :], D0.ap[0:64, :], D2.ap[0:64, :], ALU.add, D0.deps + D2.deps, D0.deps)
                        s = ycount[0] % 2; ycount[0] += 1
                        for tb in range(NTB):
                            tsl = slice(tb * TB, (tb + 1) * TB)
                            bk, dbk_ = big()
                            mm(bk[0:64, :], rg2b[:, hs], GL[:, tsl], True, True, [d_rg2, d_GL], [dbk_])
                            tt("dve", yo[s][0:64, tsl], bk[0:64, :], D0.ap[0:64, tsl], ALU.mult, [dbk_] + D0.deps, [d_yo[s]])
                        dma("sp", ydv[512 + h * 64:512 + (h + 1) * 64, :], yo[s][0:64, :], [d_yo[s]], [d_yd], "b_yo%d" % s)
                    P.op("dve", lambda e: e.memset(scr[:], 0.0), rd=alld, wr=list(dbk))
                xTv2 = xT.rearrange("(c p) t -> p c t", p=128)
                for c in range(8):
                    dma("sp", x[:, c, :], xTv2[:, c, :], [], dx[c], "x%d" % c)
            with ExitStack() as ph:
                P.fence()
                yT = sb("yTm", [128, 8, S], BF16, ph); d_yT = [Dep() for _ in range(8)]
                yv = ydv.rearrange("(c p) t -> p c t", p=128)
                for c in range(8):
                    dma("sp", yT[:, c, :], yv[:, c, :], [d_yd], [d_yT[c]], "b_yl%d" % c)
                if "ydbg" in dbg:
                    yd = dbg["ydbg"].rearrange("(c p) t -> p c t", p=128)
                    for c in range(8):
                        out_dmas.append(dma("pool", yd[:, c, :], yT[:, c, :], [d_yT[c]], [], "dbg_y"))
                out_proj(ph, dr["ab_w_out"], yT, d_yT, gm_ap)


        def dump_x(name):
            if name in dbg:
                v = dbg[name].rearrange("(c p) t -> p c t", p=128)
                for c in range(8):
                    out_dmas.append(dma("sp", v[:, c, :], x[:, c, :], dx[c], [], "dbg_" + name))

        for layer in range(2):
            if layer == 1 and cfg.get("attn", True):
                attn_mixer(layer)
            if layer == 0 and cfg.get("ab", True):
                ab_mixer(layer)
            dump_x("xmix%d" % layer)
            if cfg.get("moe", True):
                moe(layer)
            dump_x("xffn%d" % layer)

        with ExitStack() as ph:
            P.fence()
            sqb = [sb("fsq%d" % i, [128, S], BF16, ph) for i in range(2)]; d_sq = [Dep(), Dep()]
            rstd = sb("frstd", [128, S], F32, ph); d_rstd = [Dep() for _ in range(NTB)]
            ot = [sb("fot%d" % i, [128, S], F32, ph) for i in range(2)]; d_ot = [Dep(), Dep()]
            for c in range(8):
                s = c % 2
                act(sqb[s][:], x[:, c, :], AF.Square, dx[c], [d_sq[s]])
                for tb in range(NTB):
                    mm(banks[tb][:, :], ones_bf[:], sqb[s][:, tb * TB:(tb + 1) * TB], c == 0, c == 7, [d_ones, d_sq[s]], [dbk[tb]])
            for tb in range(NTB):
                act(rstd[:, tb * TB:(tb + 1) * TB], banks[tb][:, :], AF.Sqrt, [dbk[tb], d_eps], [d_rstd[tb]], bias=epsc[:, 0:1], scale=1.0 / D)
                P.op("dve", lambda e, tb=tb: e.reciprocal(out=rstd[:, tb * TB:(tb + 1) * TB], in_=rstd[:, tb * TB:(tb + 1) * TB]),
                     rd=[d_rstd[tb]], wr=[d_rstd[tb]])
            ov = outT.rearrange("(c p) t -> p c t", p=128)
            for c in range(8):
                s = c % 2
                stt("dve", ot[s][:], x[:, c, :], ng[:, 32 + c:33 + c], rstd[:], ALU.mult, ALU.mult, dx[c] + d_rstd + [d_ng], [d_ot[s]])
                out_dmas.append(dma("sp", ov[:, c, :], ot[s][:], [d_ot[s]], [], "out%d" % s))
        P.emit(final_waits=out_dmas)
    return nc, P


def _bucket_onehot():
    jp = np.arange(4096)
    rel = 2047 - jp
    nb = 16
    max_exact = 8
    bucket = np.where(rel > 0, nb, 0)
    n = np.abs(rel)
    nf = np.maximum(n, 1).astype(np.float32)
    large = max_exact + (np.log(nf / max_exact) / np.float32(np.log(128 / max_exact)) * (nb - max_exact)).astype(np.int32)
    large = np.minimum(large, nb - 1)
    bucket = bucket + np.where(n < max_exact, n, large)
    oh = np.zeros((32, 4096), np.float32)
    oh[bucket, jp] = 1.0
    return oh


def host_inputs(inp, cfg={}):
    f32 = np.float32
    def colT(v, n):
        return np.ascontiguousarray(np.asarray(v, f32).reshape(n, 128).T)
    shared = {}
    shared["ada_w"] = np.ascontiguousarray(inp["ada_w"], f32)
    shared["ada_bT"] = np.concatenate([colT(inp["ada_b"][l], 48) for l in range(2)], axis=1)
    shared["ngT"] = np.concatenate([colT(inp["norm_mix_g"][0], 8), colT(inp["norm_mix_g"][1], 8),
                                    colT(inp["norm_ffn_g"][0], 8), colT(inp["norm_ffn_g"][1], 8),
                                    colT(inp["final_norm_g"], 8)], axis=1)
    rwl = []
    for l in range(2):
        r = np.asarray(inp["router_w"][l], f32).reshape(8, 128, NE).transpose(1, 0, 2).reshape(128, 8 * NE)
        rwl.append(r)
    shared["rw"] = np.ascontiguousarray(np.stack(rwl))
    shared["rb"] = np.ascontiguousarray(np.broadcast_to(np.asarray(inp["router_b"], f32)[:, None, :], (2, 128, NE)))
    w1 = np.asarray(inp["moe_w1"], f32)
    w1p = np.empty_like(w1)
    w1v = w1p.reshape(2, NE, D, 8, 256)
    w1v[..., 0:128] = w1[..., 0::2].reshape(2, NE, D, 8, 128)
    w1v[..., 128:256] = w1[..., 1::2].reshape(2, NE, D, 8, 128)
    shared["w1"] = np.ascontiguousarray(w1p[:, :cfg.get("nexp", NE)])
    b1 = np.asarray(inp["moe_b1"], f32)
    b1g = b1[..., 0::2].reshape(2, NE, 8, 128)
    b1l = b1[..., 1::2].reshape(2, NE, 8, 128)
    b1c = np.concatenate([b1g, b1l], axis=2)
    shared["b1T"] = np.ascontiguousarray(b1c.transpose(0, 3, 1, 2).reshape(2, 128, NE * 16))
    shared["w2"] = np.ascontiguousarray(np.asarray(inp["moe_w2"], f32)[:, :cfg.get("nexp", NE)])
    shared["b2"] = np.ascontiguousarray(inp["moe_b2"], f32)
    shared["ident"] = np.eye(128, dtype=f32)
    selm = np.zeros((NE, NE, 128), f32)
    for e in range(NE):
        selm[e, e, :] = 1.0
    shared["sel"] = selm.reshape(NE, NE * 128)
    shared["attn_w_in"] = np.ascontiguousarray(inp["attn_w_in"][0], f32)
    shared["attn_w_out"] = np.ascontiguousarray(inp["attn_w_out"][0], f32)
    shared["lamb"] = np.ascontiguousarray(np.broadcast_to(np.asarray(inp["attn_lambda"][0], f32).reshape(1, 256), (128, 256)))
    shared["sublnT"] = np.ascontiguousarray(np.asarray(inp["attn_subln_g"][0], f32).reshape(128, 1))
    tabv = np.asarray(inp["rel_bias_table"], f32)
    shared["reltab"] = np.ascontiguousarray(tabv)
    cf = np.stack([tabv[15, :], tabv[31, :]], axis=1).reshape(1, 16)
    shared["cfar"] = np.ascontiguousarray(np.broadcast_to(cf, (128, 16)))
    shared["onehot"] = _bucket_onehot()
    shared["antiident"] = np.ascontiguousarray(np.eye(128, dtype=f32)[::-1])
    shared["ab_w_in"] = np.ascontiguousarray(inp["ab_w_in"][0], f32)
    shared["ab_w_out"] = np.ascontiguousarray(inp["ab_w_out"][0], f32)
    lb = np.asarray(inp["hgrn_lb"], f32)
    shared["lbT"] = np.ascontiguousarray(lb.reshape(2, 2, 4, 128).transpose(3, 0, 1, 2).reshape(128, 16))
    shared["hngT"] = np.ascontiguousarray(np.asarray(inp["hgrn_norm_g"][0], f32).reshape(128, 1))
    mu = np.asarray(inp["rwkv_mu"][0], f32)
    items = [mu[0:512], mu[512:1024], mu[1024:1536], inp["rwkv_w0"][0, 0], inp["rwkv_w0"][0, 1], inp["rwkv_a0"][0],
             inp["rwkv_k_k"][0], inp["rwkv_k_a"][0], inp["rwkv_r_k"][0], inp["rwkv_ln_g"][0], inp["rwkv_ln_b"][0]]
    it = np.stack([np.asarray(v, f32).reshape(8, 64) for v in items])
    shared["rwp"] = np.ascontiguousarray(it.transpose(2, 1, 0).reshape(64, 88))
    r2 = np.zeros((128, 3), f32)
    r2[:, 0] = mu[1536:1664]; r2[0:64, 1] = mu[1664:1728]; r2[:, 2] = mu[1728:1856]
    shared["rwp2"] = r2
    shared["rw2"] = np.ascontiguousarray(np.asarray(inp["rwkv_w2"][0], f32).reshape(128, 512))
    shared["ra2"] = np.ascontiguousarray(inp["rwkv_a2"][0], f32)
    shared["rg2"] = np.ascontiguousarray(inp["rwkv_g2"][0], f32)
    idx = np.arange(128)
    same = (idx[:, None] // 32) == (idx[None, :] // 32)
    IU = (same & (idx[:, None] <= idx[None, :])).astype(f32)
    SU = (same & (idx[:, None] < idx[None, :])).astype(f32)
    CMm = np.zeros((128, 4, 128), f32)
    for c_ in range(4):
        CMm[c_ * 32:(c_ + 1) * 32, c_, :] = 1.0
    IEX = np.broadcast_to(np.eye(128, dtype=f32)[:, None, :], (128, 4, 128))
    shared["masks"] = np.ascontiguousarray(np.concatenate([IU, SU, IU.T, SU.T, CMm.reshape(128, 512), IEX.reshape(128, 512)], axis=1))
    maps = []
    for b in range(8):
        m = dict(shared)
        m["xT"] = np.ascontiguousarray(np.asarray(inp["x"][b], f32).T)
        m["cT"] = colT(inp["c"][b], 8)
        maps.append(m)
    return maps


from concourse.bass_utils import run_bass_kernel_spmd


def kernel(**inputs):
    cfg = {}
    nc, P = build(cfg)
    maps = host_inputs(inputs, cfg)
    res = run_bass_kernel_spmd(nc, maps, core_ids=list(range(8)))
    out = np.stack([np.ascontiguousarray(np.asarray(res.results[b]["outT"]).T) for b in range(8)])
    return out.astype(np.float32)
```

```python
import numpy as np
import concourse.bass as bass
import concourse.mybir as mybir

F32 = mybir.dt.float32
BF16 = mybir.dt.bfloat16
ALU = mybir.AluOpType
AF = mybir.ActivationFunctionType
AX = mybir.AxisListType

ENGS = ("pe", "dve", "act", "pool", "sp")
SEM_CAP = 16000


class Dep:
    __slots__ = ("name", "w", "r")

    def __init__(self, name=""):
        self.name = name
        self.w = None
        self.r = []


class Op:
    __slots__ = ("eng", "fn", "deps", "sig", "ev", "isdma", "dkey")

    def __init__(self, eng, fn, isdma, dkey):
        self.eng = eng
        self.fn = fn
        self.deps = []
        self.sig = False
        self.ev = None
        self.isdma = isdma
        self.dkey = dkey


class Prog:
    def __init__(self, nc):
        self.nc = nc
        self.ops = {e: [] for e in ENGS}
        self.dma_cnt = {}
        self.fence_ops = []
        self.need = {}

    def fence(self):
        f = []
        for e in ENGS:
            for o in reversed(self.ops[e]):
                if not o.isdma:
                    f.append(o)
                    break
        last = {}
        for e in ENGS:
            for o in self.ops[e]:
                if o.isdma:
                    last[o.dkey] = o
        f.extend(last.values())
        self.fence_ops = f
        self.need = {e: True for e in ENGS}

    def op(self, eng, fn, rd=(), wr=(), dma=None):
        o = Op(eng, fn, dma is not None, dma)
        deps = []
        for d in rd:
            if d.w is not None:
                deps.append(d.w)
        for d in wr:
            if d.w is not None:
                deps.append(d.w)
            deps.extend(d.r)
        seen = set()
        for y in deps:
            if id(y) in seen or y is o:
                continue
            seen.add(id(y))
            if (not y.isdma) and (not o.isdma) and y.eng == eng == "pe":
                continue
            o.deps.append(y)
            y.sig = True
        if self.need.get(eng):
            self.need[eng] = False
            for y in self.fence_ops:
                if id(y) in seen or y is o:
                    continue
                seen.add(id(y))
                o.deps.append(y)
                y.sig = True
        for d in rd:
            d.r.append(o)
        for d in wr:
            d.w = o
            d.r = []
        if o.isdma:
            o.sig = True
        self.ops[eng].append(o)
        return o

    def emit(self, final_waits=()):
        nc = self.nc
        from contextlib import ExitStack
        nsig = {}
        for e in ENGS:
            c = 0
            for o in self.ops[e]:
                if o.isdma:
                    k = o.dkey
                    self.dma_cnt[k] = self.dma_cnt.get(k, 0) + 16
                    o.ev = ("dma:" + str(k), self.dma_cnt[k])
                elif o.sig:
                    o.ev = ("%s:%d" % (e, c // SEM_CAP), c % SEM_CAP + 1)
                    c += 1
            nsig[e] = c
        semnames = set()
        for e in ENGS:
            for o in self.ops[e]:
                if o.ev is not None:
                    semnames.add(o.ev[0])
        semnames = sorted(semnames)
        self.n_sems = len(semnames)
        self.semnames = semnames
        with ExitStack() as st:
            sems = {}
            for i, n in enumerate(semnames):
                sems[n] = st.enter_context(nc.semaphore("s%d" % i))
            block = st.enter_context(nc.Block())
            prog = self

            def run(ename, eng):
                waited = {}
                for o in prog.ops[ename]:
                    for y in o.deps:
                        s, v = y.ev
                        if waited.get(s, 0) < v:
                            eng.wait_ge(sems[s], v)
                            waited[s] = v
                    ins = o.fn(eng)
                    if o.ev is not None:
                        ins.then_inc(sems[o.ev[0]], 16 if o.isdma else 1)
                if ename == "sp":
                    for y in final_waits:
                        s, v = y.ev
                        if waited.get(s, 0) < v:
                            eng.wait_ge(sems[s], v)
                            waited[s] = v

            @block.tensor
            def _(eng):
                run("pe", eng)

            @block.vector
            def _(eng):
                run("dve", eng)

            @block.scalar
            def _(eng):
                run("act", eng)

            @block.gpsimd
            def _(eng):
                run("pool", eng)

            @block.sync
            def _(eng):
                run("sp", eng)


from contextlib import ExitStack
import numpy as np

S = 2048
D = 1024
NE = 32
TB = 512
NTB = S // TB


class Ctx:
    pass


def build(cfg):
    nc = bass.Bass("TRN2", target_bir_lowering=False)
    P = Prog(nc)
    dr = {}

    def din(name, shape, dt=F32):
        dr[name] = nc.dram_tensor(name, list(shape), dt, kind="ExternalInput").ap()
        return dr[name]

    xT = din("xT", [D, S])
    cT = din("cT", [128, 8])
    ada_w = din("ada_w", [2, D, 6 * D])
    ada_bT = din("ada_bT", [128, 96])
    ngT = din("ngT", [128, 40])
    rw = din("rw", [2, 128, 8 * NE])
    rb = din("rb", [2, 128, NE])
    w1 = din("w1", [2, cfg.get("nexp", NE), D, 2 * D])
    b1T = din("b1T", [2, 128, NE * 16])
    w2 = din("w2", [2, cfg.get("nexp", NE), D, D])
    b2 = din("b2", [2, NE, D])
    ident = din("ident", [128, 128])
    sel = din("sel", [NE, NE * 128])
    din("attn_w_in", [D, 3 * D])
    din("attn_w_out", [D, D])
    din("lamb", [128, 256])
    din("sublnT", [128, 1])
    din("cfar", [128, 16])
    din("reltab", [32, 8])
    din("onehot", [32, 4096])
    din("antiident", [128, 128])
    din("ab_w_in", [D, 4416])
    din("ab_w_out", [D, D])
    din("lbT", [128, 16])
    din("hngT", [128, 1])
    din("rwp", [64, 88])
    din("rwp2", [128, 3])
    din("rw2", [128, 512])
    din("ra2", [64, 512])
    din("rg2", [128, 512])
    din("masks", [128, 1536])
    outT = nc.dram_tensor("outT", [D, S], F32, kind="ExternalOutput").ap()
    dbg = {}
    for name, shape in cfg.get("dbg", {}).items():
        dbg[name] = nc.dram_tensor("dbg_" + name, list(shape), F32, kind="ExternalOutput").ap()

    out_dmas = []
    es = ExitStack()
    with es:
        uid = [0]

        def sb(name, shape, dt=F32, stack=es):
            uid[0] += 1
            return stack.enter_context(nc.sbuf_tensor("%s_%d" % (name, uid[0]), list(shape), dt))

        x = sb("x", [128, 8, S]); dx = [[Dep() for _ in range(NTB)] for _ in range(8)]
        banks = [es.enter_context(nc.psum_tensor("bank%d" % i, [128, 512], F32)) for i in range(8)]
        dbk = [Dep("bank%d" % i) for i in range(8)]
        ones_bf = sb("ones_bf", [128, 128], BF16); d_ones = Dep()
        identf = sb("identf", [128, 128]); d_ident = Dep()
        modT = sb("modT", [128, 96]); d_mod = Dep()
        ng = sb("ng", [128, 40]); d_ng = Dep()
        gsm = sb("gsm", [128, 40]); d_gs = Dep()
        epsc = sb("epsc", [128, 1]); d_eps = Dep()

        def dma(q, out, in_, rd, wr, key):
            return P.op(q, lambda e: e.dma_start(out=out, in_=in_), rd=rd, wr=wr, dma=key)

        def mm(out, lhsT, rhs, start, stop, rd, wr):
            return P.op("pe", lambda e: e.matmul(out, lhsT=lhsT, rhs=rhs, start=start, stop=stop), rd=rd, wr=wr)

        def act(out, in_, func, rd, wr, bias=0.0, scale=1.0):
            return P.op("act", lambda e: e.activation(out=out, in_=in_, func=func, bias=bias, scale=scale), rd=rd, wr=wr)

        def tt(eng, out, in0, in1, op, rd, wr):
            return P.op(eng, lambda e: e.tensor_tensor(out=out, in0=in0, in1=in1, op=op), rd=rd, wr=wr)

        def ts(eng, out, in0, s1, s2, op0, op1, rd, wr):
            if op1 is None:
                return P.op(eng, lambda e: e.tensor_scalar(out=out, in0=in0, scalar1=s1, scalar2=None, op0=op0), rd=rd, wr=wr)
            return P.op(eng, lambda e: e.tensor_scalar(out=out, in0=in0, scalar1=s1, scalar2=s2, op0=op0, op1=op1), rd=rd, wr=wr)

        def stt(eng, out, in0, scalar, in1, op0, op1, rd, wr):
            return P.op(eng, lambda e: e.scalar_tensor_tensor(out=out, in0=in0, scalar=scalar, in1=in1, op0=op0, op1=op1), rd=rd, wr=wr)

        def cp(eng, out, in_, rd, wr):
            return P.op(eng, lambda e: e.tensor_copy(out=out, in_=in_), rd=rd, wr=wr)

        xTv = xT.rearrange("(c p) t -> p c t", p=128)
        for c in range(8):
            dma("sp", x[:, c, :], xTv[:, c, :], [], dx[c], "x%d" % c)
        dma("sp", identf[:], ident[:, :], [], [d_ident], "c_ident")
        dma("sp", ng[:], ngT[:, :], [], [d_ng], "c_ng")
        P.op("pool", lambda e: e.memset(ones_bf[:], 1.0), wr=[d_ones])
        P.op("pool", lambda e: e.memset(epsc[:], 1e-6), wr=[d_eps])

        with ExitStack() as ph:
            P.fence()
            cs = sb("cs", [128, 8], F32, ph); d_cs = Dep()
            cb = sb("cb", [128, 8], BF16, ph); d_cb = Dep()
            abT = sb("abT", [128, 96], F32, ph); d_ab = Dep()
            wb = [sb("adaw%d" % i, [128, 8, 512], BF16, ph) for i in range(2)]
            d_wb = [Dep(), Dep()]
            dma("sp", cs[:], cT[:, :], [], [d_cs], "c_cs")
            dma("sp", abT[:], ada_bT[:, :], [], [d_ab], "c_ab")
            act(cb[:], cs[:], AF.Silu, [d_cs], [d_cb])
            it = 0
            for layer in range(2):
                for fc in range(12):
                    s = it % 2
                    src = ada_w[layer, :, fc * 512:(fc + 1) * 512].rearrange("(kc p) f -> p kc f", p=128)
                    dma("pool", wb[s][:], src, [], [d_wb[s]], "adaw%d" % s)
                    for j in range(4):
                        col = layer * 48 + fc * 4 + j
                        for kc in range(8):
                            mm(banks[0][:, col:col + 1], wb[s][:, kc, j * 128:(j + 1) * 128], cb[:, kc:kc + 1],
                               kc == 0, kc == 7, [d_wb[s], d_cb], [dbk[0]])
                    it += 1
            tt("dve", modT[:], banks[0][:, 0:96], abT[:], ALU.add, [dbk[0], d_ab], [d_mod])
            for layer in range(2):
                stt("dve", gsm[:, layer * 8:(layer + 1) * 8], modT[:, layer * 48 + 8:layer * 48 + 16], 1.0,
                    ng[:, layer * 8:(layer + 1) * 8], ALU.add, ALU.mult, [d_mod, d_ng], [d_gs])
                stt("dve", gsm[:, 16 + layer * 8:16 + (layer + 1) * 8], modT[:, layer * 48 + 32:layer * 48 + 40], 1.0,
                    ng[:, 16 + layer * 8:16 + (layer + 1) * 8], ALU.add, ALU.mult, [d_mod, d_ng], [d_gs])
            if "modT" in dbg:
                out_dmas.append(dma("sp", dbg["modT"][:, :], modT[:], [d_mod], [], "dbg_mod"))

        def allx():
            return [d for row in dx for d in row]

        def norm_mod(ph, gs_ap, sh_ap, hT, d_hT, router_layer=None, extra=None):
            with ExitStack() as loc:
                P.fence()
                sqb = [sb("sqb%d" % i, [128, S], BF16, loc) for i in range(2)]
                d_sq = [Dep(), Dep()]
                rstd = sb("rstd", [128, S], F32, loc); d_rstd = [Dep() for _ in range(NTB)]
                tmp = [sb("ntmp%d" % i, [128, S], F32, loc) for i in range(2)]
                d_tmp = [Dep(), Dep()]
                for c in range(8):
                    s = c % 2
                    act(sqb[s][:], x[:, c, :], AF.Square, dx[c], [d_sq[s]])
                    for tb in range(NTB):
                        mm(banks[tb][:, :], ones_bf[:], sqb[s][:, tb * TB:(tb + 1) * TB], c == 0, c == 7,
                           [d_ones, d_sq[s]], [dbk[tb]])
                for tb in range(NTB):
                    act(rstd[:, tb * TB:(tb + 1) * TB], banks[tb][:, :], AF.Sqrt, [dbk[tb], d_eps], [d_rstd[tb]],
                        bias=epsc[:, 0:1], scale=1.0 / D)
                    P.op("dve", lambda e, tb=tb: e.reciprocal(out=rstd[:, tb * TB:(tb + 1) * TB], in_=rstd[:, tb * TB:(tb + 1) * TB]),
                         rd=[d_rstd[tb]], wr=[d_rstd[tb]])
                if router_layer is not None:
                    h32 = [sb("h32_%d" % i, [128, S], F32, loc) for i in range(2)]
                    d_h32 = [Dep(), Dep()]
                    rw32, d_rw = extra
                for c in range(8):
                    s = c % 2
                    tt("dve", tmp[s][:], x[:, c, :], rstd[:], ALU.mult, dx[c] + d_rstd, [d_tmp[s]])
                    if router_layer is None:
                        act(hT[:, c, :], tmp[s][:], AF.Identity, [d_tmp[s], d_gs, d_mod], [d_hT[c]],
                            bias=sh_ap[:, c:c + 1], scale=gs_ap[:, c:c + 1])
                    else:
                        act(h32[s][:], tmp[s][:], AF.Identity, [d_tmp[s], d_gs, d_mod], [d_h32[s]],
                            bias=sh_ap[:, c:c + 1], scale=gs_ap[:, c:c + 1])
                        cp("pool", hT[:, c, :], h32[s][:], [d_h32[s]], [d_hT[c]])
                        for tb in range(NTB):
                            mm(banks[4 + tb][0:NE, :], rw32[:, c * NE:(c + 1) * NE], h32[s][:, tb * TB:(tb + 1) * TB],
                               c == 0, c == 7, [d_rw, d_h32[s]], [dbk[4 + tb]])

        def moe(layer):
            with ExitStack() as ph:
                P.fence()
                hT = sb("hT", [128, 8, S], BF16, ph); d_hT = [Dep() for _ in range(8)]
                rw32 = sb("rw32", [128, 8 * NE], F32, ph); d_rw = Dep()
                rbs = sb("rbs", [128, NE], F32, ph); d_rb = Dep()
                b1s = sb("b1s", [128, NE * 16], F32, ph); d_b1 = Dep()
                b2s = sb("b2s", [NE, D], BF16, ph); d_b2 = Dep()
                gatesT = sb("gatesT", [NE, S], BF16, ph); d_gT = Dep()
                selsb = sb("selsb", [NE, NE * 128], BF16, ph); d_sel = Dep()
                dma("pool", selsb[:], sel[:, :], [], [d_sel], "c_sel")
                dma("sp", rw32[:], rw[layer, :, :], [], [d_rw], "m_rw")
                dma("sp", rbs[:], rb[layer, :, :], [], [d_rb], "m_rb")
                dma("sp", b1s[:], b1T[layer, :, :], [], [d_b1], "m_b1")
                dma("pool", b2s[:], b2[layer, :, :], [], [d_b2], "m_b2")
                b1v = b1s[:].rearrange("p (e t c) -> p e t c", e=NE, t=2)
                ts("dve", b1v[:, :, 1, :], b1v[:, :, 1, :], 1.0, None, ALU.add, None, [d_b1], [d_b1])
                gs_ap = gsm[:, 16 + layer * 8:16 + (layer + 1) * 8]
                sh_ap = modT[:, layer * 48 + 24:layer * 48 + 32]
                gf_ap = modT[:, layer * 48 + 40:layer * 48 + 48]
                norm_mod(ph, gs_ap, sh_ap, hT, d_hT, router_layer=layer, extra=(rw32, d_rw))
                if ("hffn%d" % layer) in dbg:
                    pass
                with ExitStack() as loc:
                    P.fence()
                    lgT = sb("lgT", [NE, S], F32, loc); d_lgT = Dep()
                    lg = sb("lg", [128, 16, NE], F32, loc); d_lg = Dep()
                    m8 = sb("m8", [128, 16, 8], F32, loc); d_m8 = Dep()
                    msk = sb("msk", [128, 16, NE], F32, loc); d_msk = Dep()
                    ex = sb("ex", [128, 16, NE], F32, loc); d_ex = Dep()
                    zz = sb("zz", [128, 16], F32, loc); d_zz = Dep()
                    for tb in range(NTB):
                        act(lgT[:, tb * TB:(tb + 1) * TB], banks[4 + tb][0:NE, :], AF.Copy, [dbk[4 + tb]], [d_lgT])
                    for t in range(16):
                        P.op("pe", lambda e, t=t: e.transpose(banks[0][:, t * NE:(t + 1) * NE], lgT[:, t * 128:(t + 1) * 128], identf[0:NE, 0:NE]),
                             rd=[d_lgT, d_ident], wr=[dbk[0]])
                    tt("dve", lg[:], banks[0][:, :].rearrange("p (t e) -> p t e", e=NE),
                       rbs[:, None, :].to_broadcast([128, 16, NE]), ALU.add, [dbk[0], d_rb], [d_lg])
                    for t in range(16):
                        P.op("dve", lambda e, t=t: e.max(out=m8[:, t, :], in_=lg[:, t, :]), rd=[d_lg], wr=[d_m8])
                    tt("dve", msk[:], lg[:], m8[:, :, 3:4].to_broadcast([128, 16, NE]), ALU.is_ge, [d_lg, d_m8], [d_msk])
                    tt("dve", ex[:], lg[:], m8[:, :, 0:1].to_broadcast([128, 16, NE]), ALU.subtract, [d_lg, d_m8], [d_ex])
                    act(ex[:], ex[:], AF.Exp, [d_ex], [d_ex])
                    tt("dve", ex[:], ex[:], msk[:], ALU.mult, [d_ex, d_msk], [d_ex])
                    P.op("dve", lambda e: e.tensor_reduce(out=zz[:], in_=ex[:], axis=AX.X, op=ALU.add), rd=[d_ex], wr=[d_zz])
                    P.op("dve", lambda e: e.reciprocal(out=zz[:], in_=zz[:]), rd=[d_zz], wr=[d_zz])
                    tt("dve", ex[:], ex[:], zz[:, :, None].to_broadcast([128, 16, NE]), ALU.mult, [d_ex, d_zz], [d_ex])
                    if ("gates%d" % layer) in dbg:
                        out_dmas.append(dma("sp", dbg["gates%d" % layer].rearrange("(t p) e -> p t e", p=128), ex[:], [d_ex], [], "dbg_g"))
                    for t in range(16):
                        tb, o = t // 4, (t % 4) * 128
                        P.op("pe", lambda e, t=t, tb=tb, o=o: e.transpose(banks[4 + tb][0:NE, o:o + 128], ex[:, t, :], identf[:, :]),
                             rd=[d_ex, d_ident], wr=[dbk[4 + tb]])
                    for tb in range(NTB):
                        act(gatesT[:, tb * TB:(tb + 1) * TB], banks[4 + tb][0:NE, :], AF.Copy, [dbk[4 + tb]], [d_gT])
                with ExitStack() as loc:
                    P.fence()
                    actb = sb("actb", [128, 8, S], BF16, loc); d_act = [[Dep() for _ in range(NTB)] for _ in range(8)]
                    NR = 4
                    w1r = [sb("w1r%d" % i, [128, 8, 256], BF16, loc) for i in range(NR)]; d_w1 = [Dep() for _ in range(NR)]
                    w2r = [sb("w2r%d" % i, [128, 8, D], BF16, loc) for i in range(1)]; d_w2 = [Dep()]
                    gbc = [sb("gbc%d" % i, [128, S], BF16, loc) for i in range(2)]; d_gbc = [Dep(), Dep()]
                    NT = 2
                    xg = [sb("xg%d" % i, [128, TB], F32, loc) for i in range(NT)]; d_xg = [Dep() for _ in range(NT)]
                    sg = [sb("sg%d" % i, [128, TB], F32, loc) for i in range(NT)]; d_sg = [Dep() for _ in range(NT)]
                    xl = [sb("xl%d" % i, [128, TB], F32, loc) for i in range(NT)]; d_xl = [Dep() for _ in range(NT)]
                    uu = [sb("uu%d" % i, [128, TB], F32, loc) for i in range(NT)]; d_uu = [Dep() for _ in range(NT)]
                    nexp = cfg.get("nexp", NE)
                    pieces = [(e, j) for e in range(nexp) for j in range(8)]
                    def load_piece(n):
                        e, j = pieces[n]
                        s = n % NR
                        src = w1[layer, e, :, j * 256:(j + 1) * 256].rearrange("(kc p) f -> p kc f", p=128)
                        dma("pool", w1r[s][:], src, [], [d_w1[s]], "w1r%d" % s)
                    def load_w2(e):
                        s = 0
                        src = w2[layer, e, :, :].rearrange("(kc p) f -> p kc f", p=128)
                        dma("pool", w2r[s][:], src, [], [d_w2[s]], "w2r%d" % s)
                    for n in range(min(NR - 1, len(pieces))):
                        load_piece(n)
                    load_w2(0)
                    blk = 0
                    ob = 0
                    for e in range(nexp):
                        gs_ = e % 2
                        for tb in range(NTB):
                            mm(banks[6][:, :], selsb[:, e * 128:(e + 1) * 128], gatesT[:, tb * TB:(tb + 1) * TB], True, True,
                               [d_sel, d_gT], [dbk[6]])
                            act(gbc[gs_][:, tb * TB:(tb + 1) * TB], banks[6][:, :], AF.Copy, [dbk[6]], [d_gbc[gs_]], scale=1.0 / 1.702)
                        for j in range(8):
                            n = e * 8 + j
                            if n + NR - 1 < len(pieces):
                                load_piece(n + NR - 1)
                            s = n % NR
                            for tb in range(NTB):
                                pa, pb = (blk % 2) * 2, (blk % 2) * 2 + 1
                                k = blk % NT
                                for kc in range(8):
                                    mm(banks[pa][:, :], w1r[s][:, kc, 0:128], hT[:, kc, tb * TB:(tb + 1) * TB], kc == 0, kc == 7,
                                       [d_w1[s], d_hT[kc]], [dbk[pa]])
                                for kc in range(8):
                                    mm(banks[pb][:, :], w1r[s][:, kc, 128:256], hT[:, kc, tb * TB:(tb + 1) * TB], kc == 0, kc == 7,
                                       [d_w1[s], d_hT[kc]], [dbk[pb]])
                                ts("dve", xg[k][:], banks[pa][:, :], b1s[:, e * 16 + j:e * 16 + j + 1], 7.0, ALU.add, ALU.min,
                                   [dbk[pa], d_b1], [d_xg[k]])
                                act(sg[k][:], xg[k][:], AF.Silu, [d_xg[k]], [d_sg[k]], scale=1.702)
                                ts("dve", xl[k][:], banks[pb][:, :], b1s[:, e * 16 + 8 + j:e * 16 + 8 + j + 1], 8.0, ALU.add, ALU.min,
                                   [dbk[pb], d_b1], [d_xl[k]])
                                stt("dve", uu[k][:], xl[k][:], -6.0, sg[k][:], ALU.max, ALU.mult, [d_xl[k], d_sg[k]], [d_uu[k]])
                                tt("dve", actb[:, j, tb * TB:(tb + 1) * TB], uu[k][:], gbc[gs_][:, tb * TB:(tb + 1) * TB], ALU.mult,
                                   [d_uu[k], d_gbc[gs_]], [d_act[j][tb]])
                                blk += 1
                        s2 = 0
                        for i in range(8):
                            for tb in range(NTB):
                                bo = 4 + (ob % 2)
                                for j in range(8):
                                    mm(banks[bo][:, :], w2r[s2][:, j, i * 128:(i + 1) * 128], actb[:, j, tb * TB:(tb + 1) * TB],
                                       j == 0, j == 7, [d_w2[s2], d_act[j][tb]], [dbk[bo]])
                                stt("dve", x[:, i, tb * TB:(tb + 1) * TB], banks[bo][:, :], gf_ap[:, i:i + 1], x[:, i, tb * TB:(tb + 1) * TB],
                                    ALU.mult, ALU.add, [dbk[bo], d_mod, dx[i][tb]], [dx[i][tb]])
                                ob += 1
                        if e + 1 < nexp:
                            load_w2(e + 1)
                    for i in range(8):
                        for tb in range(NTB):
                            bo = 4 + (ob % 2)
                            mm(banks[bo][:, :], b2s[:, i * 128:(i + 1) * 128], gatesT[:, tb * TB:(tb + 1) * TB], True, True,
                               [d_b2, d_gT], [dbk[bo]])
                            stt("dve", x[:, i, tb * TB:(tb + 1) * TB], banks[bo][:, :], gf_ap[:, i:i + 1], x[:, i, tb * TB:(tb + 1) * TB],
                                ALU.mult, ALU.add, [dbk[bo], d_mod, dx[i][tb]], [dx[i][tb]])
                            ob += 1


        def attn_mixer(layer):
            LAM_INIT = 0.8 - 0.6 * float(np.exp(-0.3 * layer))
            with ExitStack() as ph:
                P.fence()
                hT = sb("hTa", [128, 8, S], BF16, ph); d_hT = [Dep() for _ in range(8)]
                yT = sb("yTa", [128, 8, S], BF16, ph); d_yT = [Dep() for _ in range(8)]
                norm_mod(ph, gsm[:, layer * 8:(layer + 1) * 8], modT[:, layer * 48:layer * 48 + 8], hT, d_hT)
                gm_ap = modT[:, layer * 48 + 16:layer * 48 + 24]
                with ExitStack() as loc:
                    P.fence()
                    lamb = sb("lamb", [128, 256], F32, loc); d_lam = Dep()
                    lsc = sb("lsc", [128, 8], F32, loc); d_lsc = Dep()
                    ltmp = sb("ltmp", [128, 128], F32, loc)
                    sgc = sb("sgc", [128, 1], F32, loc); d_sgc = Dep()
                    cfar = sb("cfar", [128, 16], F32, loc); d_cfar = Dep()
                    identb = sb("identb", [128, 128], BF16, loc); d_idb = Dep()
                    dma("sp", lamb[:], dr["lamb"][:, :], [], [d_lam], "a_lam")
                    dma("sp", sgc[:], dr["sublnT"][:, :], [], [d_sgc], "a_sg")
                    dma("sp", cfar[:], dr["cfar"][:, :], [], [d_cfar], "a_cf")
                    dma("pool", identb[:], dr["antiident"][:, :], [], [d_idb], "a_aid")
                    ts("dve", sgc[:], sgc[:], 1.0 - LAM_INIT, None, ALU.mult, None, [d_sgc], [d_sgc])
                    tt("dve", ltmp[:, 0:64], lamb[:, 0:64], lamb[:, 64:128], ALU.mult, [d_lam], [d_lsc])
                    tt("dve", ltmp[:, 64:128], lamb[:, 128:192], lamb[:, 192:256], ALU.mult, [d_lam], [d_lsc])
                    P.op("dve", lambda e: e.tensor_reduce(out=lsc[:, 0:2], in_=ltmp[:].rearrange("p (a b) -> p a b", a=2), axis=AX.X, op=ALU.add),
                         rd=[d_lsc], wr=[d_lsc])
                    act(lsc[:, 2:4], lsc[:, 0:2], AF.Exp, [d_lsc], [d_lsc])
                    tt("dve", lsc[:, 4:5], lsc[:, 3:4], lsc[:, 2:3], ALU.subtract, [d_lsc], [d_lsc])
                    ts("dve", lsc[:, 4:5], lsc[:, 4:5], -LAM_INIT, None, ALU.add, None, [d_lsc], [d_lsc])
                    gdr = nc.dram_tensor("gr_scratch", [8, 4096], F32)
                    d_gdr = Dep()
                    with ExitStack() as tbs:
                        P.fence()
                        tab = sb("tab", [32, 8], F32, tbs); d_tab = Dep()
                        oh = sb("oh", [32, 4096], F32, tbs); d_oh = Dep()
                        gsb = sb("gsb", [8, 4096], F32, tbs); d_gsb = Dep()
                        dma("sp", tab[:], dr["reltab"][:, :], [], [d_tab], "a_tab")
                        dma("sp", oh[:], dr["onehot"][:, :], [], [d_oh], "a_oh")
                        for cb_ in range(8):
                            mm(banks[7][0:8, :], tab[:, :], oh[:, cb_ * 512:(cb_ + 1) * 512], True, True, [d_tab, d_oh], [dbk[7]])
                            act(gsb[:, cb_ * 512:(cb_ + 1) * 512], banks[7][0:8, :], AF.Copy, [dbk[7]], [d_gsb])
                        dma("sp", gdr.ap()[:, :], gsb[:], [d_gsb], [d_gdr], "a_gdr")
                        if "gsb" in dbg:
                            out_dmas.append(dma("sp", dbg["gsb"][:, :], gsb[:], [d_gsb], [], "dbg_gsb"))
                    P.fence()
                    wq = sb("wq", [128, 8, 128], BF16, loc); d_wq = Dep()
                    wk = sb("wk", [128, 8, 128], BF16, loc); d_wk = Dep()
                    wv = sb("wv", [128, 8, 128], BF16, loc); d_wv = Dep()
                    qT = sb("qT", [128, S], BF16, loc); d_q = Dep()
                    kT = sb("kT", [128, S], BF16, loc); d_k = Dep()
                    vt = sb("vt", [128, 16, 128], BF16, loc); d_v = Dep()
                    bt = sb("bt", [128, 6, 512], BF16, loc); d_bt = Dep()
                    E = [sb("E%d" % i, [128, 512], BF16, loc) for i in range(3)]; d_E = [Dep() for _ in range(3)]
                    oh_ = sb("ohd", [128, S], F32, loc); d_ohd = [Dep() for _ in range(NTB)]
                    rz = sb("rz", [128, 512], F32, loc); d_rz = Dep()
                    o0 = sb("o0", [128, 512], F32, loc); d_o0 = Dep()
                    o1 = sb("o1", [128, 512], F32, loc); d_o1 = Dep()
                    sq = sb("asq", [128, S], BF16, loc); d_sq = Dep()
                    rs = sb("ars", [128, S], F32, loc); d_rs = [Dep() for _ in range(NTB)]
                    w_in = dr["attn_w_in"]
                    ei = 0
                    sbk = 0
                    for h in range(8):
                        for (wt, dw, c0) in ((wq, d_wq, h * 128), (wk, d_wk, 1024 + h * 128), (wv, d_wv, 2048 + h * 128)):
                            dma("pool", wt[:], w_in[:, c0:c0 + 128].rearrange("(kc p) f -> p kc f", p=128), [], [dw], "a_w%d" % c0)
                        for di in range(6):
                            delta = -128 + 128 * di
                            src = bass.AP(tensor=gdr, offset=h * 4096 + 2047 - delta - 127, ap=[[1, 128], [1, 512]])
                            dma("pool", bt[:, di, :], src, [d_gdr], [d_bt], "a_bt")
                        for tb in range(NTB):
                            for (wt, dw, dst, dd, sc_) in ((wq, d_wq, qT, d_q, 0.125), (wk, d_wk, kT, d_k, 1.0)):
                                bk = sbk % 2; sbk += 1
                                for kc in range(8):
                                    mm(banks[bk][:, :], wt[:, kc, :], hT[:, kc, tb * TB:(tb + 1) * TB], kc == 0, kc == 7, [dw, d_hT[kc]], [dbk[bk]])
                                act(dst[:, tb * TB:(tb + 1) * TB], banks[bk][:, :], AF.Copy, [dbk[bk]], [dd], scale=sc_)
                        for t in range(16):
                            bk = sbk % 2; sbk += 1
                            for kc in range(8):
                                mm(banks[bk][:, 0:128], hT[:, kc, t * 128:(t + 1) * 128], wv[:, kc, :], kc == 0, kc == 7, [d_wv, d_hT[kc]], [dbk[bk]])
                            cp("dve", vt[:, t, :], banks[bk][:, 0:128], [dbk[bk]], [d_v])
                        for qb in range(NTB):
                            for m in range(2):
                                bo, bz = 2 + 2 * m, 3 + 2 * m
                                for kt in range(16):
                                    delta = 128 * kt - 512 * qb
                                    near = -255 < delta < 639
                                    bk = sbk % 2; sbk += 1
                                    mm(banks[bk][:, :], kT[m * 64:(m + 1) * 64, kt * 128:(kt + 1) * 128], qT[m * 64:(m + 1) * 64, qb * TB:(qb + 1) * TB],
                                       True, not near, [d_k, d_q], [dbk[bk]])
                                    if near:
                                        di = (delta + 128) // 128
                                        mm(banks[bk][:, :], identb[:], bt[:, di, :], False, True, [d_idb, d_bt], [dbk[bk]])
                                    e_ = ei % 3; ei += 1
                                    if near:
                                        act(E[e_][:], banks[bk][:, :], AF.Exp, [dbk[bk]], [d_E[e_]])
                                    else:
                                        col = h * 2 + (1 if delta > 0 else 0)
                                        act(E[e_][:], banks[bk][:, :], AF.Exp, [dbk[bk], d_cfar], [d_E[e_]], bias=cfar[:, col:col + 1])
                                    mm(banks[bo][:, :], vt[:, kt, :], E[e_][:], kt == 0, kt == 15, [d_v, d_E[e_]], [dbk[bo]])
                                    mm(banks[bz][:, :], ones_bf[:], E[e_][:], kt == 0, kt == 15, [d_ones, d_E[e_]], [dbk[bz]])
                            P.op("dve", lambda e: e.reciprocal(out=rz[:], in_=banks[3][:, :]), rd=[dbk[3]], wr=[d_rz])
                            tt("dve", o0[:], banks[2][:, :], rz[:], ALU.mult, [dbk[2], d_rz], [d_o0])
                            P.op("dve", lambda e: e.reciprocal(out=rz[:], in_=banks[5][:, :]), rd=[dbk[5]], wr=[d_rz])
                            tt("dve", o1[:], banks[4][:, :], rz[:], ALU.mult, [dbk[4], d_rz], [d_o1])
                            stt("dve", oh_[:, qb * TB:(qb + 1) * TB], o1[:], lsc[:, 4:5], o0[:], ALU.mult, ALU.add, [d_o0, d_o1, d_lsc], [d_ohd[qb]])
                        if "ohd" in dbg and h == 7:
                            out_dmas.append(dma("sp", dbg["ohd"][:, :], oh_[:], d_ohd, [], "dbg_ohd"))
                        if "bt" in dbg and h == 7:
                            out_dmas.append(dma("pool", dbg["bt"][:, :], bt[:].rearrange("p a b -> p (a b)"), [d_bt], [], "dbg_bt"))
                            out_dmas.append(dma("pool", dbg["qT"][:, :], qT[:], [d_q], [], "dbg_qT"))
                            out_dmas.append(dma("pool", dbg["kT"][:, :], kT[:], [d_k], [], "dbg_kT"))
                            out_dmas.append(dma("pool", dbg["vt"][:, :], vt[:].rearrange("p a b -> p (a b)"), [d_v], [], "dbg_vt"))
                        act(sq[:], oh_[:], AF.Square, d_ohd, [d_sq])
                        for tb in range(NTB):
                            bk = 6 + tb % 2
                            mm(banks[bk][:, :], ones_bf[:], sq[:, tb * TB:(tb + 1) * TB], True, True, [d_ones, d_sq], [dbk[bk]])
                            act(rs[:, tb * TB:(tb + 1) * TB], banks[bk][:, :], AF.Sqrt, [dbk[bk], d_eps], [d_rs[tb]], bias=epsc[:, 0:1], scale=1.0 / 128)
                            P.op("dve", lambda e, tb=tb: e.reciprocal(out=rs[:, tb * TB:(tb + 1) * TB], in_=rs[:, tb * TB:(tb + 1) * TB]), rd=[d_rs[tb]], wr=[d_rs[tb]])
                        stt("dve", yT[:, h, :], oh_[:], sgc[:, 0:1], rs[:], ALU.mult, ALU.mult, d_ohd + d_rs + [d_sgc], [d_yT[h]])
                out_proj(ph, dr["attn_w_out"], yT, d_yT, gm_ap)

        def out_proj(ph, w_out, yT, d_yT, gm_ap):
            with ExitStack() as loc:
                P.fence()
                wo = [sb("wo%d" % i, [128, 8, 128], BF16, loc) for i in range(2)]; d_wo = [Dep(), Dep()]
                ob = 0
                for i in range(8):
                    s = i % 2
                    dma("pool", wo[s][:], w_out[:, i * 128:(i + 1) * 128].rearrange("(kc p) f -> p kc f", p=128), [], [d_wo[s]], "wo%d" % s)
                    for tb in range(NTB):
                        bo = ob % 2; ob += 1
                        for kc in range(8):
                            mm(banks[bo][:, :], wo[s][:, kc, :], yT[:, kc, tb * TB:(tb + 1) * TB], kc == 0, kc == 7, [d_wo[s], d_yT[kc]], [dbk[bo]])
                        stt("dve", x[:, i, tb * TB:(tb + 1) * TB], banks[bo][:, :], gm_ap[:, i:i + 1], x[:, i, tb * TB:(tb + 1) * TB],
                            ALU.mult, ALU.add, [dbk[bo], d_mod, dx[i][tb]], [dx[i][tb]])

        class Arr:
            def __init__(self, ap, deps):
                self.ap = ap
                self.deps = deps

        def ab_mixer(layer):
            W_IN = dr["ab_w_in"]
            ydram = nc.dram_tensor("y_scratch", [D, S], BF16)
            ydv = ydram.ap()
            d_yd = Dep()
            gm_ap = modT[:, layer * 48 + 16:layer * 48 + 24]
            with ExitStack() as ph:
                P.fence()
                hT = sb("hTm", [128, 8, S], BF16, ph); d_hT = [Dep() for _ in range(8)]
                norm_mod(ph, gsm[:, layer * 8:(layer + 1) * 8], modT[:, layer * 48:layer * 48 + 8], hT, d_hT)
                with ExitStack() as sc:
                    P.fence()
                    d_big = [Dep(), Dep()]
                    bigs = [(banks[0], d_big[0]), (banks[1], d_big[1])]
                    smalls = [(banks[b][:, 0:128], Dep()) for b in (2, 3, 4, 5)]
                    yregs = [(banks[b][:, 0:128], Dep()) for b in (6, 7)]
                    alld = d_big + [d for _, d in smalls] + [d for _, d in yregs]
                    scr = sb("barscr", [128, 1], F32, sc)
                    P.op("dve", lambda e: e.memset(scr[:], 0.0), rd=list(dbk), wr=alld)
                    cnt = {"big": 0, "small": 0, "y": 0, "ev": 0, "w": 0, "tok": 0, "mat": 0, "bg": 0}

                    def big():
                        cnt["big"] += 1
                        return bigs[cnt["big"] % 2]

                    def small():
                        cnt["small"] += 1
                        return smalls[cnt["small"] % 4]

                    def yreg():
                        cnt["y"] += 1
                        return yregs[cnt["y"] % 2]

                    msk = sb("msk", [128, 1536], F32, sc); d_msk = Dep()
                    dma("sp", msk[:], dr["masks"][:, :], [], [d_msk], "b_msk")
                    IU = msk[:, 0:128]; SU = msk[:, 128:256]; IL = msk[:, 256:384]; SLm = msk[:, 384:512]
                    CM = msk[:, 512:1024].rearrange("p (c k) -> p c k", c=4)
                    IEX = msk[:, 1024:1536].rearrange("p (c k) -> p c k", c=4)
                    ones32 = sb("ones32", [128, 128], F32, sc); d_o32 = Dep()
                    P.op("pool", lambda e: e.memset(ones32[:], 1.0), wr=[d_o32])
                    epsln = sb("epsln", [128, 1], F32, sc); d_epsln = Dep()
                    P.op("pool", lambda e: e.memset(epsln[:], 64e-5), wr=[d_epsln])
                    m32 = sb("m32", [128, S], BF16, sc); d_m32 = Dep()
                    P.op("pool", lambda e: e.memset(m32[:], 1.0), wr=[d_m32])
                    P.op("pool", lambda e: e.memset(m32[:].rearrange("p (c t) -> p c t", t=32)[:, :, 0:1], 0.0), rd=[d_m32], wr=[d_m32])
                    lbT = sb("lbT", [128, 16], F32, sc); d_lb = Dep()
                    lbc = sb("lbc", [128, 8], F32, sc); oml = sb("oml", [128, 8], F32, sc)
                    dma("sp", lbT[:], dr["lbT"][:, :], [], [d_lb], "b_lb")
                    lbv = lbT[:].rearrange("p (d s h) -> p d s h", d=2, s=2)
                    tt("dve", lbc[:].rearrange("p (d h) -> p d h", d=2), lbv[:, :, 0, :], lbv[:, :, 1, :], ALU.subtract, [d_lb], [d_lb])
                    act(lbc[:], lbc[:], AF.Sigmoid, [d_lb], [d_lb])
                    ts("dve", oml[:], lbc[:], -1.0, 1.0, ALU.mult, ALU.add, [d_lb], [d_lb])
                    hng = sb("hng", [128, 1], F32, sc); d_hng = Dep()
                    dma("sp", hng[:], dr["hngT"][:, :], [], [d_hng], "b_hng")
                    rwp = sb("rwp", [64, 88], F32, sc); d_rwp = Dep()
                    rwo = sb("rwo", [64, 88], F32, sc); rwh = sb("rwh", [64, 88], F32, sc)
                    dma("sp", rwp[:], dr["rwp"][:, :], [], [d_rwp], "b_rwp")
                    ts("dve", rwo[:], rwp[:], -1.0, 1.0, ALU.mult, ALU.add, [d_rwp], [d_rwp])
                    ts("dve", rwh[:], rwp[:], 0.5, None, ALU.mult, None, [d_rwp], [d_rwp])
                    rwp2 = sb("rwp2", [128, 3], F32, sc); d_rwp2 = Dep()
                    rwo2 = sb("rwo2", [128, 3], F32, sc); rwh2 = sb("rwh2", [128, 3], F32, sc)
                    dma("sp", rwp2[:], dr["rwp2"][:, :], [], [d_rwp2], "b_rwp2")
                    ts("dve", rwo2[:], rwp2[:], -1.0, 1.0, ALU.mult, ALU.add, [d_rwp2], [d_rwp2])
                    ts("dve", rwh2[:], rwp2[:], 0.5, None, ALU.mult, None, [d_rwp2], [d_rwp2])
                    rw2b = sb("rw2b", [128, 512], BF16, sc); d_rw2 = Dep()
                    ra2b = sb("ra2b", [64, 512], BF16, sc); d_ra2 = Dep()
                    rg2b = sb("rg2b", [128, 512], BF16, sc); d_rg2 = Dep()
                    dma("pool", rw2b[:], dr["rw2"][:, :], [], [d_rw2], "b_rw2")
                    dma("pool", ra2b[:], dr["ra2"][:, :], [], [d_ra2], "b_ra2")
                    dma("pool", rg2b[:], dr["rg2"][:, :], [], [d_rg2], "b_rg2")

                    wbx = [sb("wbx%d" % i, [128, S], F32, sc) for i in range(2)]
                    SLT = [Arr(x[:, i, :], dx[i]) for i in range(8)] + [Arr(wbx[i][:], [Dep()]) for i in range(2)]
                    wbuf = [sb("abwb%d" % i, [128, 8, 128], BF16, sc) for i in range(2)]; d_wbuf = [Dep(), Dep()]
                    WCt = [sb("WC%d" % i, [128, 64], F32, sc) for i in range(2)]; d_WC = [Dep(), Dep()]
                    STt = [sb("ST%d" % i, [128, 128], F32, sc) for i in range(2)]; d_ST = [Dep(), Dep()]
                    NTOK, NMAT, NBG = 10, 44, 10
                    tokt = [(sb("tok%d" % i, [128, 128], F32, sc), Dep()) for i in range(NTOK)]
                    matt = [(sb("mat%d" % i, [128, 128], F32, sc), Dep()) for i in range(NMAT)]
                    bgt = [(sb("bg%d" % i, [128, 512], F32, sc), Dep()) for i in range(NBG)]
                    yo = [sb("yo%d" % i, [128, S], BF16, sc) for i in range(2)]; d_yo = [Dep(), Dep()]
                    tott = sb("tott", [128, 64], F32, sc); d_tott = Dep()

                    def tok():
                        cnt["tok"] += 1
                        return tokt[cnt["tok"] % NTOK]

                    def mat():
                        cnt["mat"] += 1
                        return matt[cnt["mat"] % NMAT]

                    def bg():
                        cnt["bg"] += 1
                        return bgt[cnt["bg"] % NBG]

                    def evac(out, in_, rd, wr, scale=None, eng=None):
                        cnt["ev"] += 1
                        if eng is None:
                            eng = "dve" if cnt["ev"] % 3 == 0 else "act"
                        if eng == "act":
                            return act(out, in_, AF.Copy, rd, wr, scale=(1.0 if scale is None else scale))
                        if scale is None:
                            return cp("dve", out, in_, rd, wr)
                        return ts("dve", out, in_, scale, None, ALU.mult, None, rd, wr)

                    F32R = mybir.dt.float32r

                    def mmr(out, lhsT, rhs, start, stop, rd, wr):
                        return mm(out, lhsT.bitcast(F32R), rhs.bitcast(F32R), start, stop, rd, wr)

                    def mms(out, dout, terms, last_stop=True, first_start=True, r32=False):
                        n = len(terms)
                        for i, (l, r, deps) in enumerate(terms):
                            (mmr if r32 else mm)(out, l, r, first_start and i == 0, last_stop and i == n - 1, deps, [dout])

                    def proj(col0, ncols, consume):
                        s = cnt["w"] % 2; cnt["w"] += 1
                        dma("pool", wbuf[s][:, :, 0:ncols], W_IN[:, col0:col0 + ncols].rearrange("(kc p) f -> p kc f", p=128),
                            [], [d_wbuf[s]], "abw%d" % s)
                        for tb in range(NTB):
                            bk, dbk_ = big()
                            for kc in range(8):
                                mm(bk[0:ncols, :], wbuf[s][:, kc, 0:ncols], hT[:, kc, tb * TB:(tb + 1) * TB], kc == 0, kc == 7,
                                   [d_wbuf[s], d_hT[kc]], [dbk_])
                            consume(tb, bk[0:ncols, :], dbk_)

                    def proj_to(col0, ncols, dst, func=AF.Copy, bias=0.0, extra_rd=()):
                        def c(tb, ps, dps):
                            act(dst.ap[0:ncols, tb * TB:(tb + 1) * TB], ps, func, [dps] + list(extra_rd), dst.deps, bias=bias)
                        proj(col0, ncols, c)

                    def shiftmix(raw, tmp, n, omu, hmu, dpar):
                        r = raw.ap; t = tmp.ap
                        tt("pool", t[0:n, 1:S - 1], r[0:n, 0:S - 2], r[0:n, 2:S], ALU.add, raw.deps, tmp.deps)
                        cp("pool", t[0:n, 0:1], r[0:n, 1:2], raw.deps, tmp.deps)
                        cp("pool", t[0:n, S - 1:S], r[0:n, S - 2:S - 1], raw.deps, tmp.deps)
                        ts("dve", r[0:n, :], r[0:n, :], omu, None, ALU.mult, None, raw.deps + [dpar], raw.deps)
                        stt("dve", r[0:n, :], t[0:n, :], hmu, r[0:n, :], ALU.mult, ALU.add, tmp.deps + raw.deps + [dpar], raw.deps)

                    def v3(ap, n):
                        return ap[0:n, :].rearrange("p (c t) -> p c t", t=32)

                    def cumsum(ld, cum, n):
                        P.op("dve", lambda e: e.tensor_tensor_scan(out=cum.ap[0:n, :], data0=m32[0:n, :],
                                                                   data1=ld.ap[0:n, :], initial=0.0, op0=ALU.mult, op1=ALU.add),
                             rd=ld.deps + [d_m32], wr=cum.deps)

                    def dir_arrays(dirn, n, LDa, CUMa, D2, D3, srcR, srcK, srcB, srcKK, wc, dwc):
                        cumsum(LDa, CUMa, n)
                        c3 = v3(CUMa.ap, n)
                        act(wc[0:n, :], c3[:, :, 31], AF.Exp, CUMa.deps, [dwc])
                        cp("dve", tott[0:n, :], c3[:, :, 31], CUMa.deps, [d_tott])
                        delta = srcB is not None
                        if dirn == 0:
                            act(D2.ap[0:n, :], CUMa.ap[0:n, :], AF.Exp, CUMa.deps, D2.deps)
                            tt("dve", D2.ap[0:n, :], D2.ap[0:n, :], srcR.ap[0:n, :], ALU.mult, D2.deps + srcR.deps, D2.deps)
                            act(D3.ap[0:n, :], CUMa.ap[0:n, :], AF.Exp, CUMa.deps, D3.deps, scale=-1.0)
                            if delta:
                                tt("dve", LDa.ap[0:n, :], CUMa.ap[0:n, :], LDa.ap[0:n, :], ALU.subtract, CUMa.deps + LDa.deps, LDa.deps)
                                act(LDa.ap[0:n, :], LDa.ap[0:n, :], AF.Exp, LDa.deps, LDa.deps)
                                tt("pool", LDa.ap[0:n, :], LDa.ap[0:n, :], srcKK.ap[0:n, :], ALU.mult, LDa.deps + srcKK.deps, LDa.deps)
                                tt("pool", CUMa.ap[0:n, :], D3.ap[0:n, :], srcB.ap[0:n, :], ALU.mult, D3.deps + srcB.deps + LDa.deps, CUMa.deps)
                            tt("dve", D3.ap[0:n, :], D3.ap[0:n, :], srcK.ap[0:n, :], ALU.mult, D3.deps + srcK.deps + CUMa.deps, D3.deps)
                            return D2, D3, CUMa, LDa
                        else:
                            tt("dve", c3, c3, tott[0:n, :, None].to_broadcast([n, 64, 32]), ALU.subtract, CUMa.deps + [d_tott], CUMa.deps)
                            if delta:
                                act(D2.ap[0:n, :], CUMa.ap[0:n, :], AF.Exp, CUMa.deps, D2.deps, scale=-1.0)
                                tt("pool", D2.ap[0:n, :], D2.ap[0:n, :], srcKK.ap[0:n, :], ALU.mult, D2.deps + srcKK.deps, D2.deps)
                            tt("dve", LDa.ap[0:n, :], LDa.ap[0:n, :], CUMa.ap[0:n, :], ALU.subtract, LDa.deps + CUMa.deps, LDa.deps)
                            act(D3.ap[0:n, :], LDa.ap[0:n, :], AF.Exp, LDa.deps, D3.deps)
                            tt("dve", D3.ap[0:n, :], D3.ap[0:n, :], srcR.ap[0:n, :], ALU.mult, D3.deps + srcR.deps, D3.deps)
                            act(LDa.ap[0:n, :], LDa.ap[0:n, :], AF.Exp, LDa.deps, LDa.deps, scale=-1.0)
                            if delta:
                                tt("pool", CUMa.ap[0:n, :], LDa.ap[0:n, :], srcB.ap[0:n, :], ALU.mult, LDa.deps + srcB.deps + D2.deps, CUMa.deps)
                            tt("dve", LDa.ap[0:n, :], LDa.ap[0:n, :], srcK.ap[0:n, :], ALU.mult, LDa.deps + srcK.deps + CUMa.deps, LDa.deps)
                            return D3, LDa, CUMa, D2

                    def scan(dk, delta, dirn, rT, kT_, vA, bT, aT, wc, dwc, osum, first):
                        fwd = dirn == 0
                        m_iu = IU if fwd else IL
                        m_su = SU if fwd else SLm
                        m_sl = SLm if fwd else SU
                        idk = identf[0:dk, 0:dk]
                        P.op("pool", lambda e: e.memset(STt[0][:], 0.0), wr=[d_ST[0]])
                        st = {"cur": 0, "seqdone": True}
                        tiles = list(range(16)) if fwd else list(range(15, -1, -1))
                        chunks = list(range(4)) if fwd else list(range(3, -1, -1))
                        res = {}

                        def tr(arr, n0, role, par):
                            reg, dreg = small()
                            P.op("pe", lambda e: e.transpose(reg[:, 0:dk], arr.ap[0:dk, n0:n0 + 128], idk), rd=arr.deps + [d_ident], wr=[dreg])
                            t, d = tokt[role * 2 + par]
                            evac(t[:, 0:dk].bitcast(F32R), reg[:, 0:dk], [dreg], [d])
                            return t, d

                        def mmev(terms, rows, cols, role, par, scale=None, mask=None, r32=False, add=None):
                            reg, dreg = small()
                            mms(reg[0:rows, 0:cols], dreg, terms, r32=r32)
                            t, d = matt[role * 2 + par]
                            o_ = t[0:rows, 0:cols].bitcast(F32R)
                            if add is not None:
                                aap, adeps = add
                                tt("dve", o_, reg[0:rows, 0:cols], aap, ALU.add, [dreg] + list(adeps), [d])
                            elif mask is None:
                                evac(o_, reg[0:rows, 0:cols], [dreg], [d], scale=scale)
                            else:
                                stt("dve", o_, reg[0:rows, 0:cols], (1.0 if scale is None else scale), mask[0:rows, 0:cols],
                                    ALU.mult, ALU.mult, [dreg, d_msk], [d])
                            return t, d

                        def prep(n, par):
                            n0 = n * 128
                            rs_ = rT.ap[0:dk, n0:n0 + 128]; ks_ = kT_.ap[0:dk, n0:n0 + 128]
                            ktok, d_ktok = tr(kT_, n0, 0, par)
                            vtok, d_vtok = tr(vA, n0, 1, par)
                            yield
                            MrkT, d_mrk = mmev([(ks_, rs_, kT_.deps + rT.deps)], 128, 128, 0, par, mask=m_iu)
                            vexp, d_vexp = bgt[0 * 2 + par]
                            tt("pool", vexp[:, 0:4 * dk].bitcast(F32R).rearrange("p (c k) -> p c k", c=4), vtok[:, None, 0:dk].to_broadcast([128, 4, dk]),
                               CM[:, :, 0:dk], ALU.mult, [d_vtok, d_msk], [d_vexp])
                            yield
                            QT = d_QT = GT = d_GT = None
                            if delta:
                                as_ = aT.ap[0:dk, n0:n0 + 128]; bs_ = bT.ap[0:dk, n0:n0 + 128]
                                btok, d_btok = tr(bT, n0, 2, par)
                                atok, d_atok = tr(aT, n0, 3, par)
                                yield
                                N1, d_N1 = mmev([(as_, bs_, aT.deps + bT.deps)], 128, 128, 1, par, scale=-1.0, mask=m_sl)
                                N1T, d_N1T = mmev([(bs_, as_, aT.deps + bT.deps)], 128, 128, 2, par, scale=-1.0, mask=m_su)
                                yield
                                Y, d_Y = matt[3 * 2 + par]
                                tt("pool", Y[:, :].bitcast(F32R), N1T[:, :], identf[:, :], ALU.add, [d_N1T, d_ident], [d_Y])
                                Np, d_Np, NpT, d_NpT = N1, d_N1, N1T, d_N1T
                                for lvl in range(4):
                                    N2, d_N2 = mmev([(NpT[:, :], Np[:, :], [d_Np, d_NpT])], 128, 128, 4 + 3 * lvl, par, r32=True)
                                    if lvl < 3:
                                        N2T, d_N2T = mmev([(Np[:, :], NpT[:, :], [d_Np, d_NpT])], 128, 128, 5 + 3 * lvl, par, r32=True)
                                    yield
                                    Y, d_Y = mmev([(N2[:, :], Y[:, :], [d_N2, d_Y])], 128, 128, 6 + 3 * lvl, par, r32=True, add=(Y[:, :], [d_Y]))
                                    yield
                                    Np, d_Np, NpT, d_NpT = N2, d_N2, N2T, d_N2T
                                TT, d_TT = Y, d_Y
                                LakT, d_lak = mmev([(ks_, as_, kT_.deps + aT.deps)], 128, 128, 16, par, mask=m_su)
                                MrbT, d_mrb = mmev([(bs_, rs_, bT.deps + rT.deps)], 128, 128, 20, par, mask=m_iu)
                                yield
                                Z, d_Z = mmev([(LakT[:, :], vtok[:, 0:dk], [d_lak, d_vtok])], 128, dk, 17, par, r32=True)
                                negTA, d_negTA = mmev([(TT[:, :], atok[:, 0:dk], [d_TT, d_atok])], 128, dk, 19, par, scale=-1.0, r32=True)
                                bexp, d_bexp = bgt[2 * 2 + par]
                                tt("pool", bexp[:, 0:4 * dk].bitcast(F32R).rearrange("p (c k) -> p c k", c=4), btok[:, None, 0:dk].to_broadcast([128, 4, dk]),
                                   CM[:, :, 0:dk], ALU.mult, [d_btok, d_msk], [d_bexp])
                                yield
                                while not st["seqdone"]:
                                    yield
                                negP, d_negP = mmev([(TT[:, :], Z[:, 0:dk], [d_TT, d_Z])], 128, dk, 18, par, scale=-1.0, r32=True)
                                QT, d_QT = mmev([(negTA[:, 0:dk], MrbT[:, :], [d_negTA, d_mrb])], dk, 128, 21, par, r32=True, add=(rs_, rT.deps))
                                yield
                                pexp, d_pexp = bgt[1 * 2 + par]
                                tt("pool", pexp[:, 0:4 * dk].bitcast(F32R).rearrange("p (c k) -> p c k", c=4), negP[:, None, 0:dk].to_broadcast([128, 4, dk]),
                                   CM[:, :, 0:dk], ALU.mult, [d_negP, d_msk], [d_pexp])
                                greg, d_greg = big()
                                mms(greg[0:dk, 0:4 * dk], d_greg, [(negTA[:, 0:dk], bexp[:, 0:4 * dk], [d_negTA, d_bexp])], r32=True)
                                GT, d_GT = bgt[3 * 2 + par]
                                tt("dve", GT[0:dk, 0:4 * dk].rearrange("p (c k) -> p c k", c=4), greg[0:dk, 0:4 * dk].rearrange("p (c k) -> p c k", c=4),
                                   IEX[0:dk, :, 0:dk], ALU.add, [d_greg, d_msk], [d_GT])
                                yield
                                hreg, d_hreg = big()
                                mms(hreg[0:dk, 0:4 * dk], d_hreg, [(ktok[:, 0:dk], vexp[:, 0:4 * dk], [d_ktok, d_vexp]),
                                                                  (btok[:, 0:dk], pexp[:, 0:4 * dk], [d_btok, d_pexp])], r32=True)
                            else:
                                while not st["seqdone"]:
                                    yield
                                hreg, d_hreg = big()
                                mms(hreg[0:dk, 0:4 * dk], d_hreg, [(ktok[:, 0:dk], vexp[:, 0:4 * dk], [d_ktok, d_vexp])], r32=True)
                            Hs, d_Hs = bgt[4 * 2 + par]
                            tt("dve", Hs[0:dk, 0:4 * dk].rearrange("p (c k) -> p c k", c=4), hreg[0:dk, 0:4 * dk].rearrange("p (c k) -> p c k", c=4),
                               wc[0:dk, n * 4:(n + 1) * 4][:, :, None].to_broadcast([dk, 4, dk]), ALU.mult, [d_hreg, dwc], [d_Hs])
                            yield
                            while not st["seqdone"]:
                                yield
                            yr, d_yr = yregs[par]
                            if delta:
                                mms(yr[0:dk, :], d_yr, [(vtok[:, 0:dk], MrkT[:, :], [d_vtok, d_mrk]),
                                                        (negP[:, 0:dk], MrbT[:, :], [d_negP, d_mrb])], last_stop=False, r32=True)
                            else:
                                mms(yr[0:dk, :], d_yr, [(vtok[:, 0:dk], MrkT[:, :], [d_vtok, d_mrk])], last_stop=False, r32=True)
                            res[n] = (QT, d_QT, GT, d_GT, Hs, d_Hs, yr, d_yr)

                        def seqs(ns):
                            for n in ns:
                                n0 = n * 128
                                QT, d_QT, GT, d_GT, Hs, d_Hs, yr, d_yr = res.pop(n)
                                for ci, c in enumerate(chunks):
                                    cc = slice(c * 32, (c + 1) * 32)
                                    cur = st["cur"]
                                    stc = STt[cur]; dstc = d_ST[cur]
                                    stn = STt[1 - cur]; dstn = d_ST[1 - cur]
                                    if delta:
                                        mm(yr[0:dk, cc], stc[0:dk, 0:dk], QT[0:dk, cc], False, ci == 3, [dstc, d_QT], [d_yr])
                                        sr, d_sr = small()
                                        mm(sr[0:dk, 0:dk], GT[0:dk, c * dk:(c + 1) * dk], stc[0:dk, 0:dk], True, True, [d_GT, dstc], [d_sr])
                                        stt("dve", stn[0:dk, 0:dk], sr[0:dk, 0:dk], wc[0:dk, n * 4 + c:n * 4 + c + 1], Hs[0:dk, c * dk:(c + 1) * dk],
                                            ALU.mult, ALU.add, [d_sr, dwc, d_Hs], [dstn])
                                    else:
                                        mm(yr[0:dk, cc], stc[0:dk, 0:dk], rT.ap[0:dk, n0 + c * 32:n0 + (c + 1) * 32], False, ci == 3,
                                           [dstc] + rT.deps, [d_yr])
                                        stt("dve", stn[0:dk, 0:dk], stc[0:dk, 0:dk], wc[0:dk, n * 4 + c:n * 4 + c + 1], Hs[0:dk, c * dk:(c + 1) * dk],
                                            ALU.mult, ALU.add, [dstc, dwc, d_Hs], [dstn])
                                    st["cur"] = 1 - cur
                                    yield
                                if first:
                                    evac(osum.ap[0:dk, n0:n0 + 128], yr[0:dk, :], [d_yr], osum.deps)
                                else:
                                    tt("dve", osum.ap[0:dk, n0:n0 + 128], yr[0:dk, :], osum.ap[0:dk, n0:n0 + 128], ALU.add, [d_yr] + osum.deps, osum.deps)
                                yield
                            st["seqdone"] = True

                        pairs = [tiles[i:i + 2] for i in range(0, 16, 2)]
                        prev = None
                        for pr in pairs + [None]:
                            gens = []
                            if prev is not None:
                                st["seqdone"] = False
                                gens.append(seqs(prev))
                            if pr is not None:
                                gens += [prep(n, i) for i, n in enumerate(pr)]
                            while gens:
                                for g in list(gens):
                                    try:
                                        next(g)
                                    except StopIteration:
                                        gens.remove(g)
                            prev = pr

                    ycount = [0]

                    def y_out(src_ap, n, row0, rd):
                        s = ycount[0] % 2; ycount[0] += 1
                        return s

                    for h in range(cfg.get('hg_heads', 4)):
                        Qs, Vv, FG, OS, D0, D1, D2, D3 = SLT[0], SLT[1], SLT[2], SLT[3], SLT[4], SLT[5], SLT[6], SLT[7]
                        proj_to(h * 128, 128, Qs, AF.Silu)
                        proj_to(1536 + h * 128, 128, Vv, AF.Copy)
                        for dirn in range(cfg.get('dirs', 2)):
                            col = dirn * 4 + h
                            proj_to(512 + dirn * 512 + h * 128, 128, FG, AF.Sigmoid)
                            ts("dve", FG.ap, FG.ap, oml[:, col:col + 1], lbc[:, col:col + 1], ALU.mult, ALU.add, FG.deps + [d_lb], FG.deps)
                            act(D0.ap, FG.ap, AF.Ln, FG.deps, D0.deps)
                            ts("dve", FG.ap, FG.ap, -1.0, 1.0, ALU.mult, ALU.add, FG.deps, FG.deps)
                            w = dirn
                            rT_, kT2, _, _ = dir_arrays(dirn, 128, D0, D1, D2, D3, Qs, FG, None, None, WCt[w], d_WC[w])
                            scan(128, False, dirn, rT_, kT2, Vv, None, None, WCt[w], d_WC[w], OS, dirn == 0)
                        sqa = D0
                        act(sqa.ap, OS.ap, AF.Square, OS.deps, sqa.deps)
                        for tb in range(NTB):
                            bk, dbk_ = big()
                            mm(bk[:, :], ones32[:, :], sqa.ap[:, tb * TB:(tb + 1) * TB], True, True, [d_o32] + sqa.deps, [dbk_])
                            act(D1.ap[:, tb * TB:(tb + 1) * TB], bk[:, :], AF.Sqrt, [dbk_, d_eps], D1.deps, bias=epsc[:, 0:1], scale=1.0 / 128)
                        P.op("dve", lambda e, a=D1.ap: e.reciprocal(out=a, in_=a), rd=D1.deps, wr=D1.deps)
                        proj_to(2048 + h * 128, 128, D2, AF.Silu)
                        stt("dve", D1.ap, OS.ap, hng[:, 0:1], D1.ap, ALU.mult, ALU.mult, OS.deps + D1.deps + [d_hng], D1.deps)
                        s = ycount[0] % 2; ycount[0] += 1
                        tt("dve", yo[s][:, :], D1.ap, D2.ap, ALU.mult, D1.deps + D2.deps, [d_yo[s]])
                        dma("sp", ydv[h * 128:(h + 1) * 128, :], yo[s][:, :], [d_yo[s]], [d_yd], "b_yo%d" % s)

                    BO = 2560
                    WL = sb("WL", [128, S], BF16, sc); d_WL = Dep()
                    AL = sb("AL", [64, S], BF16, sc); d_AL = Dep()
                    GL = sb("GL", [128, S], BF16, sc); d_GL = Dep()
                    T0, T1 = SLT[8], SLT[9]
                    proj_to(BO + 1536, 128, T0)
                    shiftmix(T0, T1, 128, rwo2[:, 0:1], rwh2[:, 0:1], d_rwp2)
                    act(WL[:, :], T0.ap, AF.Tanh, T0.deps, [d_WL])
                    proj_to(BO + 1664, 64, T0)
                    shiftmix(T0, T1, 64, rwo2[0:64, 1:2], rwh2[0:64, 1:2], d_rwp2)
                    cp("dve", AL[:, :], T0.ap[0:64, :], T0.deps, [d_AL])
                    proj_to(BO + 1728, 128, T0)
                    shiftmix(T0, T1, 128, rwo2[:, 2:3], rwh2[:, 2:3], d_rwp2)
                    act(GL[:, :], T0.ap, AF.Sigmoid, T0.deps, [d_GL])
                    for h in range(cfg.get('rw_heads', 8)):
                        R, K, V, Bv, KK, OS, D0, D1, D2, D3 = SLT
                        pc = lambda i: rwp[:, h * 11 + i:h * 11 + i + 1]
                        hs = slice(h * 64, (h + 1) * 64)
                        for (dst, c0, mi) in ((R, 0, 0), (K, 512, 1), (V, 1024, 2)):
                            proj_to(BO + c0 + h * 64, 64, dst)
                            shiftmix(dst, D0, 64, rwo[:, h * 11 + mi:h * 11 + mi + 1], rwh[:, h * 11 + mi:h * 11 + mi + 1], d_rwp)
                        for tb in range(NTB):
                            bk, dbk_ = big()
                            mm(bk[0:64, :], ra2b[:, hs], AL[:, tb * TB:(tb + 1) * TB], True, True, [d_ra2, d_AL], [dbk_])
                            act(Bv.ap[0:64, tb * TB:(tb + 1) * TB], bk[0:64, :], AF.Sigmoid, [dbk_, d_rwp], Bv.deps, bias=pc(5))
                        ts("dve", KK.ap[0:64, :], K.ap[0:64, :], pc(6), None, ALU.mult, None, K.deps + [d_rwp], KK.deps)
                        act(D0.ap[0:64, :], KK.ap[0:64, :], AF.Square, KK.deps, D0.deps)
                        for tb in range(NTB):
                            bk, dbk_ = big()
                            mm(bk[0:64, :], ones32[0:64, 0:64], D0.ap[0:64, tb * TB:(tb + 1) * TB], True, True, [d_o32] + D0.deps, [dbk_])
                            act(D1.ap[0:64, tb * TB:(tb + 1) * TB], bk[0:64, :], AF.Sqrt, [dbk_], D1.deps)
                        ts("dve", D1.ap[0:64, :], D1.ap[0:64, :], 1e-12, None, ALU.max, None, D1.deps, D1.deps)
                        P.op("dve", lambda e, a=D1.ap[0:64, :]: e.reciprocal(out=a, in_=a), rd=D1.deps, wr=D1.deps)
                        tt("dve", KK.ap[0:64, :], KK.ap[0:64, :], D1.ap[0:64, :], ALU.mult, KK.deps + D1.deps, KK.deps)
                        ts("dve", D0.ap[0:64, :], Bv.ap[0:64, :], -1.0, pc(7), ALU.add, ALU.mult, Bv.deps + D0.deps + [d_rwp], D0.deps)
                        stt("dve", K.ap[0:64, :], D0.ap[0:64, :], 1.0, K.ap[0:64, :], ALU.add, ALU.mult, D0.deps + K.deps, K.deps)
                        tt("dve", Bv.ap[0:64, :], Bv.ap[0:64, :], KK.ap[0:64, :], ALU.mult, Bv.deps + KK.deps + D0.deps, Bv.deps)
                        for dirn in range(cfg.get('dirs', 2)):
                            for tb in range(NTB):
                                bk, dbk_ = big()
                                mm(bk[0:64, :], rw2b[dirn * 64:(dirn + 1) * 64, hs], WL[dirn * 64:(dirn + 1) * 64, tb * TB:(tb + 1) * TB], True, True,
                                   [d_rw2, d_WL], [dbk_])
                                act(D0.ap[0:64, tb * TB:(tb + 1) * TB], bk[0:64, :], AF.Sigmoid, [dbk_, d_rwp], D0.deps, bias=pc(3 + dirn))
                            ts("dve", D0.ap[0:64, :], D0.ap[0:64, :], -float(np.exp(-0.5)), None, ALU.mult, None, D0.deps, D0.deps)
                            w = dirn
                            rT_, kT2, bT2, aT2 = dir_arrays(dirn, 64, D0, D1, D2, D3, R, K, Bv, KK, WCt[w], d_WC[w])
                            scan(64, True, dirn, rT_, kT2, V, bT2, aT2, WCt[w], d_WC[w], OS, dirn == 0)
                        Y_ = OS
                        for tb in range(NTB):
                            tsl = slice(tb * TB, (tb + 1) * TB)
                            bk, dbk_ = big()
                            mm(bk[0:64, :], ones32[0:64, 0:64], Y_.ap[0:64, tsl], True, True, [d_o32] + Y_.deps, [dbk_])
                            stt("dve", D0.ap[0:64, tsl], bk[0:64, :], -1.0 / 64, Y_.ap[0:64, tsl], ALU.mult, ALU.add, [dbk_] + Y_.deps + D0.deps, D0.deps)
                        act(D1.ap[0:64, :], D0.ap[0:64, :], AF.Square, D0.deps + D1.deps, D1.deps)
                        for tb in range(NTB):
                            tsl = slice(tb * TB, (tb + 1) * TB)
                            bk, dbk_ = big()
                            mm(bk[0:64, :], ones32[0:64, 0:64], D1.ap[0:64, tsl], True, True, [d_o32] + D1.deps, [dbk_])
                            act(D2.ap[0:64, tsl], bk[0:64, :], AF.Sqrt, [dbk_, d_epsln] + D2.deps, D2.deps, bias=epsln[0:64, 0:1], scale=1.0 / 64)
                        P.op("dve", lambda e, a=D2.ap[0:64, :]: e.reciprocal(out=a, in_=a), rd=D2.deps, wr=D2.deps)
                        tt("dve", D0.ap[0:64, :], D0.ap[0:64, :], D2.ap[0:64, :], ALU.mult, D0.deps + D2.deps, D0.deps)
                        ts("dve", D0.ap[0:64, :], D0.ap[0:64, :], pc(9), pc(10), ALU.mult, ALU.add, D0.deps + [d_rwp], D0.deps)
                        stt("dve", D1.ap[0:64, :], R.ap[0:64, :], pc(8), K.ap[0:64, :], ALU.mult, ALU.mult, R.deps + K.deps + D1.deps + [d_rwp], D1.deps)
                        for tb in range(NTB):
                            tsl = slice(tb * TB, (tb + 1) * TB)
                            bk, dbk_ = big()
                            mm(bk[0:64, :], ones32[0:64, 0:64], D1.ap[0:64, tsl], True, True, [d_o32] + D1.deps, [dbk_])
                            tt("dve", D2.ap[0:64, tsl], bk[0:64, :], V.ap[0:64, tsl], ALU.mult, [dbk_] + V.deps + D2.deps, D2.deps)
                        tt("dve", D0.ap[0:64, :], D0.ap[0:64, :], D2.ap[0:64, :], ALU.add, D0.deps + D2.deps, D0.deps)
                        s = ycount[0] % 2; ycount[0] += 1
                        for tb in range(NTB):
                            tsl = slice(tb * TB, (tb + 1) * TB)
                            bk, dbk_ = big()
                            mm(bk[0:64, :], rg2b[:, hs], GL[:, tsl], True, True, [d_rg2, d_GL], [dbk_])
                            tt("dve", yo[s][0:64, tsl], bk[0:64, :], D0.ap[0:64, tsl], ALU.mult, [dbk_] + D0.deps, [d_yo[s]])
                        dma("sp", ydv[512 + h * 64:512 + (h + 1) * 64, :], yo[s][0:64, :], [d_yo[s]], [d_yd], "b_yo%d" % s)
                    P.op("dve", lambda e: e.memset(scr[:], 0.0), rd=alld, wr=list(dbk))
                xTv2 = xT.rearrange("(c p) t -> p c t", p=128)
                for c in range(8):
                    dma("sp", x[:, c, :], xTv2[:, c, :], [], dx[c], "x%d" % c)
            with ExitStack() as ph:
                P.fence()
                yT = sb("yTm", [128, 8, S], BF16, ph); d_yT = [Dep() for _ in range(8)]
                yv = ydv.rearrange("(c p) t -> p c t", p=128)
                for c in range(8):
                    dma("sp", yT[:, c, :], yv[:, c, :], [d_yd], [d_yT[c]], "b_yl%d" % c)
                if "ydbg" in dbg:
                    yd = dbg["ydbg"].rearrange("(c p) t -> p c t", p=128)
                    for c in range(8):
                        out_dmas.append(dma("pool", yd[:, c, :], yT[:, c, :], [d_yT[c]], [], "dbg_y"))
                out_proj(ph, dr["ab_w_out"], yT, d_yT, gm_ap)


        def dump_x(name):
            if name in dbg:
                v = dbg[name].rearrange("(c p) t -> p c t", p=128)
                for c in range(8):
                    out_dmas.append(dma("sp", v[:, c, :], x[:, c, :], dx[c], [], "dbg_" + name))

        for layer in range(2):
            if layer == 1 and cfg.get("attn", True):
                attn_mixer(layer)
            if layer == 0 and cfg.get("ab", True):
                ab_mixer(layer)
            dump_x("xmix%d" % layer)
            if cfg.get("moe", True):
                moe(layer)
            dump_x("xffn%d" % layer)

        with ExitStack() as ph:
            P.fence()
            sqb = [sb("fsq%d" % i, [128, S], BF16, ph) for i in range(2)]; d_sq = [Dep(), Dep()]
            rstd = sb("frstd", [128, S], F32, ph); d_rstd = [Dep() for _ in range(NTB)]
            ot = [sb("fot%d" % i, [128, S], F32, ph) for i in range(2)]; d_ot = [Dep(), Dep()]
            for c in range(8):
                s = c % 2
                act(sqb[s][:], x[:, c, :], AF.Square, dx[c], [d_sq[s]])
                for tb in range(NTB):
                    mm(banks[tb][:, :], ones_bf[:], sqb[s][:, tb * TB:(tb + 1) * TB], c == 0, c == 7, [d_ones, d_sq[s]], [dbk[tb]])
            for tb in range(NTB):
                act(rstd[:, tb * TB:(tb + 1) * TB], banks[tb][:, :], AF.Sqrt, [dbk[tb], d_eps], [d_rstd[tb]], bias=epsc[:, 0:1], scale=1.0 / D)
                P.op("dve", lambda e, tb=tb: e.reciprocal(out=rstd[:, tb * TB:(tb + 1) * TB], in_=rstd[:, tb * TB:(tb + 1) * TB]),
                     rd=[d_rstd[tb]], wr=[d_rstd[tb]])
            ov = outT.rearrange("(c p) t -> p c t", p=128)
            for c in range(8):
                s = c % 2
                stt("dve", ot[s][:], x[:, c, :], ng[:, 32 + c:33 + c], rstd[:], ALU.mult, ALU.mult, dx[c] + d_rstd + [d_ng], [d_ot[s]])
                out_dmas.append(dma("sp", ov[:, c, :], ot[s][:], [d_ot[s]], [], "out%d" % s))
        P.emit(final_waits=out_dmas)
    return nc, P


def _bucket_onehot():
    jp = np.arange(4096)
    rel = 2047 - jp
    nb = 16
    max_exact = 8
    bucket = np.where(rel > 0, nb, 0)
    n = np.abs(rel)
    nf = np.maximum(n, 1).astype(np.float32)
    large = max_exact + (np.log(nf / max_exact) / np.float32(np.log(128 / max_exact)) * (nb - max_exact)).astype(np.int32)
    large = np.minimum(large, nb - 1)
    bucket = bucket + np.where(n < max_exact, n, large)
    oh = np.zeros((32, 4096), np.float32)
    oh[bucket, jp] = 1.0
    return oh


def host_inputs(inp, cfg={}):
    f32 = np.float32
    def colT(v, n):
        return np.ascontiguousarray(np.asarray(v, f32).reshape(n, 128).T)
    shared = {}
    shared["ada_w"] = np.ascontiguousarray(inp["ada_w"], f32)
    shared["ada_bT"] = np.concatenate([colT(inp["ada_b"][l], 48) for l in range(2)], axis=1)
    shared["ngT"] = np.concatenate([colT(inp["norm_mix_g"][0], 8), colT(inp["norm_mix_g"][1], 8),
                                    colT(inp["norm_ffn_g"][0], 8), colT(inp["norm_ffn_g"][1], 8),
                                    colT(inp["final_norm_g"], 8)], axis=1)
    rwl = []
    for l in range(2):
        r = np.asarray(inp["router_w"][l], f32).reshape(8, 128, NE).transpose(1, 0, 2).reshape(128, 8 * NE)
        rwl.append(r)
    shared["rw"] = np.ascontiguousarray(np.stack(rwl))
    shared["rb"] = np.ascontiguousarray(np.broadcast_to(np.asarray(inp["router_b"], f32)[:, None, :], (2, 128, NE)))
    w1 = np.asarray(inp["moe_w1"], f32)
    w1p = np.empty_like(w1)
    w1v = w1p.reshape(2, NE, D, 8, 256)
    w1v[..., 0:128] = w1[..., 0::2].reshape(2, NE, D, 8, 128)
    w1v[..., 128:256] = w1[..., 1::2].reshape(2, NE, D, 8, 128)
    shared["w1"] = np.ascontiguousarray(w1p[:, :cfg.get("nexp", NE)])
    b1 = np.asarray(inp["moe_b1"], f32)
    b1g = b1[..., 0::2].reshape(2, NE, 8, 128)
    b1l = b1[..., 1::2].reshape(2, NE, 8, 128)
    b1c = np.concatenate([b1g, b1l], axis=2)
    shared["b1T"] = np.ascontiguousarray(b1c.transpose(0, 3, 1, 2).reshape(2, 128, NE * 16))
    shared["w2"] = np.ascontiguousarray(np.asarray(inp["moe_w2"], f32)[:, :cfg.get("nexp", NE)])
    shared["b2"] = np.ascontiguousarray(inp["moe_b2"], f32)
    shared["ident"] = np.eye(128, dtype=f32)
    selm = np.zeros((NE, NE, 128), f32)
    for e in range(NE):
        selm[e, e, :] = 1.0
    shared["sel"] = selm.reshape(NE, NE * 128)
    shared["attn_w_in"] = np.ascontiguousarray(inp["attn_w_in"][0], f32)
    shared["attn_w_out"] = np.ascontiguousarray(inp["attn_w_out"][0], f32)
    shared["lamb"] = np.ascontiguousarray(np.broadcast_to(np.asarray(inp["attn_lambda"][0], f32).reshape(1, 256), (128, 256)))
    shared["sublnT"] = np.ascontiguousarray(np.asarray(inp["attn_subln_g"][0], f32).reshape(128, 1))
    tabv = np.asarray(inp["rel_bias_table"], f32)
    shared["reltab"] = np.ascontiguousarray(tabv)
    cf = np.stack([tabv[15, :], tabv[31, :]], axis=1).reshape(1, 16)
    shared["cfar"] = np.ascontiguousarray(np.broadcast_to(cf, (128, 16)))
    shared["onehot"] = _bucket_onehot()
    shared["antiident"] = np.ascontiguousarray(np.eye(128, dtype=f32)[::-1])
    shared["ab_w_in"] = np.ascontiguousarray(inp["ab_w_in"][0], f32)
    shared["ab_w_out"] = np.ascontiguousarray(inp["ab_w_out"][0], f32)
    lb = np.asarray(inp["hgrn_lb"], f32)
    shared["lbT"] = np.ascontiguousarray(lb.reshape(2, 2, 4, 128).transpose(3, 0, 1, 2).reshape(128, 16))
    shared["hngT"] = np.ascontiguousarray(np.asarray(inp["hgrn_norm_g"][0], f32).reshape(128, 1))
    mu = np.asarray(inp["rwkv_mu"][0], f32)
    items = [mu[0:512], mu[512:1024], mu[1024:1536], inp["rwkv_w0"][0, 0], inp["rwkv_w0"][0, 1], inp["rwkv_a0"][0],
             inp["rwkv_k_k"][0], inp["rwkv_k_a"][0], inp["rwkv_r_k"][0], inp["rwkv_ln_g"][0], inp["rwkv_ln_b"][0]]
    it = np.stack([np.asarray(v, f32).reshape(8, 64) for v in items])
    shared["rwp"] = np.ascontiguousarray(it.transpose(2, 1, 0).reshape(64, 88))
    r2 = np.zeros((128, 3), f32)
    r2[:, 0] = mu[1536:1664]; r2[0:64, 1] = mu[1664:1728]; r2[:, 2] = mu[1728:1856]
    shared["rwp2"] = r2
    shared["rw2"] = np.ascontiguousarray(np.asarray(inp["rwkv_w2"][0], f32).reshape(128, 512))
    shared["ra2"] = np.ascontiguousarray(inp["rwkv_a2"][0], f32)
    shared["rg2"] = np.ascontiguousarray(inp["rwkv_g2"][0], f32)
    idx = np.arange(128)
    same = (idx[:, None] // 32) == (idx[None, :] // 32)
    IU = (same & (idx[:, None] <= idx[None, :])).astype(f32)
    SU = (same & (idx[:, None] < idx[None, :])).astype(f32)
    CMm = np.zeros((128, 4, 128), f32)
    for c_ in range(4):
        CMm[c_ * 32:(c_ + 1) * 32, c_, :] = 1.0
    IEX = np.broadcast_to(np.eye(128, dtype=f32)[:, None, :], (128, 4, 128))
    shared["masks"] = np.ascontiguousarray(np.concatenate([IU, SU, IU.T, SU.T, CMm.reshape(128, 512), IEX.reshape(128, 512)], axis=1))
    maps = []
    for b in range(8):
        m = dict(shared)
        m["xT"] = np.ascontiguousarray(np.asarray(inp["x"][b], f32).T)
        m["cT"] = colT(inp["c"][b], 8)
        maps.append(m)
    return maps


from concourse.bass_utils import run_bass_kernel_spmd


def kernel(**inputs):
    cfg = {}
    nc, P = build(cfg)
    maps = host_inputs(inputs, cfg)
    res = run_bass_kernel_spmd(nc, maps, core_ids=list(range(8)))
    out = np.stack([np.ascontiguousarray(np.asarray(res.results[b]["outT"]).T) for b in range(8)])
    return out.astype(np.float32)
```

```python
import numpy as np
import concourse.bass as bass
import concourse.mybir as mybir

F32 = mybir.dt.float32
BF16 = mybir.dt.bfloat16
ALU = mybir.AluOpType
AF = mybir.ActivationFunctionType
AX = mybir.AxisListType

ENGS = ("pe", "dve", "act", "pool", "sp")
SEM_CAP = 16000


class Dep:
    __slots__ = ("name", "w", "r")

    def __init__(self, name=""):
        self.name = name
        self.w = None
        self.r = []


class Op:
    __slots__ = ("eng", "fn", "deps", "sig", "ev", "isdma", "dkey")

    def __init__(self, eng, fn, isdma, dkey):
        self.eng = eng
        self.fn = fn
        self.deps = []
        self.sig = False
        self.ev = None
        self.isdma = isdma
        self.dkey = dkey


class Prog:
    def __init__(self, nc):
        self.nc = nc
        self.ops = {e: [] for e in ENGS}
        self.dma_cnt = {}
        self.fence_ops = []
        self.need = {}

    def fence(self):
        f = []
        for e in ENGS:
            for o in reversed(self.ops[e]):
                if not o.isdma:
                    f.append(o)
                    break
        last = {}
        for e in ENGS:
            for o in self.ops[e]:
                if o.isdma:
                    last[o.dkey] = o
        f.extend(last.values())
        self.fence_ops = f
        self.need = {e: True for e in ENGS}

    def op(self, eng, fn, rd=(), wr=(), dma=None):
        o = Op(eng, fn, dma is not None, dma)
        deps = []
        for d in rd:
            if d.w is not None:
                deps.append(d.w)
        for d in wr:
            if d.w is not None:
                deps.append(d.w)
            deps.extend(d.r)
        seen = set()
        for y in deps:
            if id(y) in seen or y is o:
                continue
            seen.add(id(y))
            if (not y.isdma) and (not o.isdma) and y.eng == eng == "pe":
                continue
            o.deps.append(y)
            y.sig = True
        if self.need.get(eng):
            self.need[eng] = False
            for y in self.fence_ops:
                if id(y) in seen or y is o:
                    continue
                seen.add(id(y))
                o.deps.append(y)
                y.sig = True
        for d in rd:
            d.r.append(o)
        for d in wr:
            d.w = o
            d.r = []
        if o.isdma:
            o.sig = True
        self.ops[eng].append(o)
        return o

    def emit(self, final_waits=()):
        nc = self.nc
        from contextlib import ExitStack
        nsig = {}
        for e in ENGS:
            c = 0
            for o in self.ops[e]:
                if o.isdma:
                    k = o.dkey
                    self.dma_cnt[k] = self.dma_cnt.get(k, 0) + 16
                    o.ev = ("dma:" + str(k), self.dma_cnt[k])
                elif o.sig:
                    o.ev = ("%s:%d" % (e, c // SEM_CAP), c % SEM_CAP + 1)
                    c += 1
            nsig[e] = c
        semnames = set()
        for e in ENGS:
            for o in self.ops[e]:
                if o.ev is not None:
                    semnames.add(o.ev[0])
        semnames = sorted(semnames)
        self.n_sems = len(semnames)
        self.semnames = semnames
        with ExitStack() as st:
            sems = {}
            for i, n in enumerate(semnames):
                sems[n] = st.enter_context(nc.semaphore("s%d" % i))
            block = st.enter_context(nc.Block())
            prog = self

            def run(ename, eng):
                waited = {}
                for o in prog.ops[ename]:
                    for y in o.deps:
                        s, v = y.ev
                        if waited.get(s, 0) < v:
                            eng.wait_ge(sems[s], v)
                            waited[s] = v
                    ins = o.fn(eng)
                    if o.ev is not None:
                        ins.then_inc(sems[o.ev[0]], 16 if o.isdma else 1)
                if ename == "sp":
                    for y in final_waits:
                        s, v = y.ev
                        if waited.get(s, 0) < v:
                            eng.wait_ge(sems[s], v)
                            waited[s] = v

            @block.tensor
            def _(eng):
                run("pe", eng)

            @block.vector
            def _(eng):
                run("dve", eng)

            @block.scalar
            def _(eng):
                run("act", eng)

            @block.gpsimd
            def _(eng):
                run("pool", eng)

            @block.sync
            def _(eng):
                run("sp", eng)


from contextlib import ExitStack
import numpy as np

S = 2048
D = 1024
NE = 32
TB = 512
NTB = S // TB


class Ctx:
    pass


def build(cfg):
    nc = bass.Bass("TRN2", target_bir_lowering=False)
    P = Prog(nc)
    dr = {}

    def din(name, shape, dt=F32):
        dr[name] = nc.dram_tensor(name, list(shape), dt, kind="ExternalInput").ap()
        return dr[name]

    xT = din("xT", [D, S])
    cT = din("cT", [128, 8])
    ada_w = din("ada_w", [2, D, 6 * D])
    ada_bT = din("ada_bT", [128, 96])
    ngT = din("ngT", [128, 40])
    rw = din("rw", [2, 128, 8 * NE])
    rb = din("rb", [2, 128, NE])
    w1 = din("w1", [2, cfg.get("nexp", NE), D, 2 * D])
    b1T = din("b1T", [2, 128, NE * 16])
    w2 = din("w2", [2, cfg.get("nexp", NE), D, D])
    b2 = din("b2", [2, NE, D])
    ident = din("ident", [128, 128])
    sel = din("sel", [NE, NE * 128])
    din("attn_w_in", [D, 3 * D])
    din("attn_w_out", [D, D])
    din("lamb", [128, 256])
    din("sublnT", [128, 1])
    din("cfar", [128, 16])
    din("reltab", [32, 8])
    din("onehot", [32, 4096])
    din("antiident", [128, 128])
    din("ab_w_in", [D, 4416])
    din("ab_w_out", [D, D])
    din("lbT", [128, 16])
    din("hngT", [128, 1])
    din("rwp", [64, 88])
    din("rwp2", [128, 3])
    din("rw2", [128, 512])
    din("ra2", [64, 512])
    din("rg2", [128, 512])
    din("masks", [128, 1536])
    outT = nc.dram_tensor("outT", [D, S], F32, kind="ExternalOutput").ap()
    dbg = {}
    for name, shape in cfg.get("dbg", {}).items():
        dbg[name] = nc.dram_tensor("dbg_" + name, list(shape), F32, kind="ExternalOutput").ap()

    out_dmas = []
    es = ExitStack()
    with es:
        uid = [0]

        def sb(name, shape, dt=F32, stack=es):
            uid[0] += 1
            return stack.enter_context(nc.sbuf_tensor("%s_%d" % (name, uid[0]), list(shape), dt))

        x = sb("x", [128, 8, S]); dx = [[Dep() for _ in range(NTB)] for _ in range(8)]
        banks = [es.enter_context(nc.psum_tensor("bank%d" % i, [128, 512], F32)) for i in range(8)]
        dbk = [Dep("bank%d" % i) for i in range(8)]
        ones_bf = sb("ones_bf", [128, 128], BF16); d_ones = Dep()
        identf = sb("identf", [128, 128]); d_ident = Dep()
        modT = sb("modT", [128, 96]); d_mod = Dep()
        ng = sb("ng", [128, 40]); d_ng = Dep()
        gsm = sb("gsm", [128, 40]); d_gs = Dep()
        epsc = sb("epsc", [128, 1]); d_eps = Dep()

        def dma(q, out, in_, rd, wr, key):
            return P.op(q, lambda e: e.dma_start(out=out, in_=in_), rd=rd, wr=wr, dma=key)

        def mm(out, lhsT, rhs, start, stop, rd, wr):
            return P.op("pe", lambda e: e.matmul(out, lhsT=lhsT, rhs=rhs, start=start, stop=stop), rd=rd, wr=wr)

        def act(out, in_, func, rd, wr, bias=0.0, scale=1.0):
            return P.op("act", lambda e: e.activation(out=out, in_=in_, func=func, bias=bias, scale=scale), rd=rd, wr=wr)

        def tt(eng, out, in0, in1, op, rd, wr):
            return P.op(eng, lambda e: e.tensor_tensor(out=out, in0=in0, in1=in1, op=op), rd=rd, wr=wr)

        def ts(eng, out, in0, s1, s2, op0, op1, rd, wr):
            if op1 is None:
                return P.op(eng, lambda e: e.tensor_scalar(out=out, in0=in0, scalar1=s1, scalar2=None, op0=op0), rd=rd, wr=wr)
            return P.op(eng, lambda e: e.tensor_scalar(out=out, in0=in0, scalar1=s1, scalar2=s2, op0=op0, op1=op1), rd=rd, wr=wr)

        def stt(eng, out, in0, scalar, in1, op0, op1, rd, wr):
            return P.op(eng, lambda e: e.scalar_tensor_tensor(out=out, in0=in0, scalar=scalar, in1=in1, op0=op0, op1=op1), rd=rd, wr=wr)

        def cp(eng, out, in_, rd, wr):
            return P.op(eng, lambda e: e.tensor_copy(out=out, in_=in_), rd=rd, wr=wr)

        xTv = xT.rearrange("(c p) t -> p c t", p=128)
        for c in range(8):
            dma("sp", x[:, c, :], xTv[:, c, :], [], dx[c], "x%d" % c)
        dma("sp", identf[:], ident[:, :], [], [d_ident], "c_ident")
        dma("sp", ng[:], ngT[:, :], [], [d_ng], "c_ng")
        P.op("pool", lambda e: e.memset(ones_bf[:], 1.0), wr=[d_ones])
        P.op("pool", lambda e: e.memset(epsc[:], 1e-6), wr=[d_eps])

        with ExitStack() as ph:
            P.fence()
            cs = sb("cs", [128, 8], F32, ph); d_cs = Dep()
            cb = sb("cb", [128, 8], BF16, ph); d_cb = Dep()
            abT = sb("abT", [128, 96], F32, ph); d_ab = Dep()
            wb = [sb("adaw%d" % i, [128, 8, 512], BF16, ph) for i in range(2)]
            d_wb = [Dep(), Dep()]
            dma("sp", cs[:], cT[:, :], [], [d_cs], "c_cs")
            dma("sp", abT[:], ada_bT[:, :], [], [d_ab], "c_ab")
            act(cb[:], cs[:], AF.Silu, [d_cs], [d_cb])
            it = 0
            for layer in range(2):
                for fc in range(12):
                    s = it % 2
                    src = ada_w[layer, :, fc * 512:(fc + 1) * 512].rearrange("(kc p) f -> p kc f", p=128)
                    dma("pool", wb[s][:], src, [], [d_wb[s]], "adaw%d" % s)
                    for j in range(4):
                        col = layer * 48 + fc * 4 + j
                        for kc in range(8):
                            mm(banks[0][:, col:col + 1], wb[s][:, kc, j * 128:(j + 1) * 128], cb[:, kc:kc + 1],
                               kc == 0, kc == 7, [d_wb[s], d_cb], [dbk[0]])
                    it += 1
            tt("dve", modT[:], banks[0][:, 0:96], abT[:], ALU.add, [dbk[0], d_ab], [d_mod])
            for layer in range(2):
                stt("dve", gsm[:, layer * 8:(layer + 1) * 8], modT[:, layer * 48 + 8:layer * 48 + 16], 1.0,
                    ng[:, layer * 8:(layer + 1) * 8], ALU.add, ALU.mult, [d_mod, d_ng], [d_gs])
                stt("dve", gsm[:, 16 + layer * 8:16 + (layer + 1) * 8], modT[:, layer * 48 + 32:layer * 48 + 40], 1.0,
                    ng[:, 16 + layer * 8:16 + (layer + 1) * 8], ALU.add, ALU.mult, [d_mod, d_ng], [d_gs])
            if "modT" in dbg:
                out_dmas.append(dma("sp", dbg["modT"][:, :], modT[:], [d_mod], [], "dbg_mod"))

        def allx():
            return [d for row in dx for d in row]

        def norm_mod(ph, gs_ap, sh_ap, hT, d_hT, router_layer=None, extra=None):
            with ExitStack() as loc:
                P.fence()
                sqb = [sb("sqb%d" % i, [128, S], BF16, loc) for i in range(2)]
                d_sq = [Dep(), Dep()]
                rstd = sb("rstd", [128, S], F32, loc); d_rstd = [Dep() for _ in range(NTB)]
                tmp = [sb("ntmp%d" % i, [128, S], F32, loc) for i in range(2)]
                d_tmp = [Dep(), Dep()]
                for c in range(8):
                    s = c % 2
                    act(sqb[s][:], x[:, c, :], AF.Square, dx[c], [d_sq[s]])
                    for tb in range(NTB):
                        mm(banks[tb][:, :], ones_bf[:], sqb[s][:, tb * TB:(tb + 1) * TB], c == 0, c == 7,
                           [d_ones, d_sq[s]], [dbk[tb]])
                for tb in range(NTB):
                    act(rstd[:, tb * TB:(tb + 1) * TB], banks[tb][:, :], AF.Sqrt, [dbk[tb], d_eps], [d_rstd[tb]],
                        bias=epsc[:, 0:1], scale=1.0 / D)
                    P.op("dve", lambda e, tb=tb: e.reciprocal(out=rstd[:, tb * TB:(tb + 1) * TB], in_=rstd[:, tb * TB:(tb + 1) * TB]),
                         rd=[d_rstd[tb]], wr=[d_rstd[tb]])
                if router_layer is not None:
                    h32 = [sb("h32_%d" % i, [128, S], F32, loc) for i in range(2)]
                    d_h32 = [Dep(), Dep()]
                    rw32, d_rw = extra
                for c in range(8):
                    s = c % 2
                    tt("dve", tmp[s][:], x[:, c, :], rstd[:], ALU.mult, dx[c] + d_rstd, [d_tmp[s]])
                    if router_layer is None:
                        act(hT[:, c, :], tmp[s][:], AF.Identity, [d_tmp[s], d_gs, d_mod], [d_hT[c]],
                            bias=sh_ap[:, c:c + 1], scale=gs_ap[:, c:c + 1])
                    else:
                        act(h32[s][:], tmp[s][:], AF.Identity, [d_tmp[s], d_gs, d_mod], [d_h32[s]],
                            bias=sh_ap[:, c:c + 1], scale=gs_ap[:, c:c + 1])
                        cp("pool", hT[:, c, :], h32[s][:], [d_h32[s]], [d_hT[c]])
                        for tb in range(NTB):
                            mm(banks[4 + tb][0:NE, :], rw32[:, c * NE:(c + 1) * NE], h32[s][:, tb * TB:(tb + 1) * TB],
                               c == 0, c == 7, [d_rw, d_h32[s]], [dbk[4 + tb]])

        def moe(layer):
            with ExitStack() as ph:
                P.fence()
                hT = sb("hT", [128, 8, S], BF16, ph); d_hT = [Dep() for _ in range(8)]
                rw32 = sb("rw32", [128, 8 * NE], F32, ph); d_rw = Dep()
                rbs = sb("rbs", [128, NE], F32, ph); d_rb = Dep()
                b1s = sb("b1s", [128, NE * 16], F32, ph); d_b1 = Dep()
                b2s = sb("b2s", [NE, D], BF16, ph); d_b2 = Dep()
                gatesT = sb("gatesT", [NE, S], BF16, ph); d_gT = Dep()
                selsb = sb("selsb", [NE, NE * 128], BF16, ph); d_sel = Dep()
                dma("pool", selsb[:], sel[:, :], [], [d_sel], "c_sel")
                dma("sp", rw32[:], rw[layer, :, :], [], [d_rw], "m_rw")
                dma("sp", rbs[:], rb[layer, :, :], [], [d_rb], "m_rb")
                dma("sp", b1s[:], b1T[layer, :, :], [], [d_b1], "m_b1")
                dma("pool", b2s[:], b2[layer, :, :], [], [d_b2], "m_b2")
                b1v = b1s[:].rearrange("p (e t c) -> p e t c", e=NE, t=2)
                ts("dve", b1v[:, :, 1, :], b1v[:, :, 1, :], 1.0, None, ALU.add, None, [d_b1], [d_b1])
                gs_ap = gsm[:, 16 + layer * 8:16 + (layer + 1) * 8]
                sh_ap = modT[:, layer * 48 + 24:layer * 48 + 32]
                gf_ap = modT[:, layer * 48 + 40:layer * 48 + 48]
                norm_mod(ph, gs_ap, sh_ap, hT, d_hT, router_layer=layer, extra=(rw32, d_rw))
                if ("hffn%d" % layer) in dbg:
                    pass
                with ExitStack() as loc:
                    P.fence()
                    lgT = sb("lgT", [NE, S], F32, loc); d_lgT = Dep()
                    lg = sb("lg", [128, 16, NE], F32, loc); d_lg = Dep()
                    m8 = sb("m8", [128, 16, 8], F32, loc); d_m8 = Dep()
                    msk = sb("msk", [128, 16, NE], F32, loc); d_msk = Dep()
                    ex = sb("ex", [128, 16, NE], F32, loc); d_ex = Dep()
                    zz = sb("zz", [128, 16], F32, loc); d_zz = Dep()
                    for tb in range(NTB):
                        act(lgT[:, tb * TB:(tb + 1) * TB], banks[4 + tb][0:NE, :], AF.Copy, [dbk[4 + tb]], [d_lgT])
                    for t in range(16):
                        P.op("pe", lambda e, t=t: e.transpose(banks[0][:, t * NE:(t + 1) * NE], lgT[:, t * 128:(t + 1) * 128], identf[0:NE, 0:NE]),
                             rd=[d_lgT, d_ident], wr=[dbk[0]])
                    tt("dve", lg[:], banks[0][:, :].rearrange("p (t e) -> p t e", e=NE),
                       rbs[:, None, :].to_broadcast([128, 16, NE]), ALU.add, [dbk[0], d_rb], [d_lg])
                    for t in range(16):
                        P.op("dve", lambda e, t=t: e.max(out=m8[:, t, :], in_=lg[:, t, :]), rd=[d_lg], wr=[d_m8])
                    tt("dve", msk[:], lg[:], m8[:, :, 3:4].to_broadcast([128, 16, NE]), ALU.is_ge, [d_lg, d_m8], [d_msk])
                    tt("dve", ex[:], lg[:], m8[:, :, 0:1].to_broadcast([128, 16, NE]), ALU.subtract, [d_lg, d_m8], [d_ex])
                    act(ex[:], ex[:], AF.Exp, [d_ex], [d_ex])
                    tt("dve", ex[:], ex[:], msk[:], ALU.mult, [d_ex, d_msk], [d_ex])
                    P.op("dve", lambda e: e.tensor_reduce(out=zz[:], in_=ex[:], axis=AX.X, op=ALU.add), rd=[d_ex], wr=[d_zz])
                    P.op("dve", lambda e: e.reciprocal(out=zz[:], in_=zz[:]), rd=[d_zz], wr=[d_zz])
                    tt("dve", ex[:], ex[:], zz[:, :, None].to_broadcast([128, 16, NE]), ALU.mult, [d_ex, d_zz], [d_ex])
                    if ("gates%d" % layer) in dbg:
                        out_dmas.append(dma("sp", dbg["gates%d" % layer].rearrange("(t p) e -> p t e", p=128), ex[:], [d_ex], [], "dbg_g"))
                    for t in range(16):
                        tb, o = t // 4, (t % 4) * 128
                        P.op("pe", lambda e, t=t, tb=tb, o=o: e.transpose(banks[4 + tb][0:NE, o:o + 128], ex[:, t, :], identf[:, :]),
                             rd=[d_ex, d_ident], wr=[dbk[4 + tb]])
                    for tb in range(NTB):
                        act(gatesT[:, tb * TB:(tb + 1) * TB], banks[4 + tb][0:NE, :], AF.Copy, [dbk[4 + tb]], [d_gT])
                with ExitStack() as loc:
                    P.fence()
                    actb = sb("actb", [128, 8, S], BF16, loc); d_act = [[Dep() for _ in range(NTB)] for _ in range(8)]
                    NR = 4
                    w1r = [sb("w1r%d" % i, [128, 8, 256], BF16, loc) for i in range(NR)]; d_w1 = [Dep() for _ in range(NR)]
                    w2r = [sb("w2r%d" % i, [128, 8, D], BF16, loc) for i in range(1)]; d_w2 = [Dep()]
                    gbc = [sb("gbc%d" % i, [128, S], BF16, loc) for i in range(2)]; d_gbc = [Dep(), Dep()]
                    NT = 2
                    xg = [sb("xg%d" % i, [128, TB], F32, loc) for i in range(NT)]; d_xg = [Dep() for _ in range(NT)]
                    sg = [sb("sg%d" % i, [128, TB], F32, loc) for i in range(NT)]; d_sg = [Dep() for _ in range(NT)]
                    xl = [sb("xl%d" % i, [128, TB], F32, loc) for i in range(NT)]; d_xl = [Dep() for _ in range(NT)]
                    uu = [sb("uu%d" % i, [128, TB], F32, loc) for i in range(NT)]; d_uu = [Dep() for _ in range(NT)]
                    nexp = cfg.get("nexp", NE)
                    pieces = [(e, j) for e in range(nexp) for j in range(8)]
                    def load_piece(n):
                        e, j = pieces[n]
                        s = n % NR
                        src = w1[layer, e, :, j * 256:(j + 1) * 256].rearrange("(kc p) f -> p kc f", p=128)
                        dma("pool", w1r[s][:], src, [], [d_w1[s]], "w1r%d" % s)
                    def load_w2(e):
                        s = 0
                        src = w2[layer, e, :, :].rearrange("(kc p) f -> p kc f", p=128)
                        dma("pool", w2r[s][:], src, [], [d_w2[s]], "w2r%d" % s)
                    for n in range(min(NR - 1, len(pieces))):
                        load_piece(n)
                    load_w2(0)
                    blk = 0
                    ob = 0
                    for e in range(nexp):
                        gs_ = e % 2
                        for tb in range(NTB):
                            mm(banks[6][:, :], selsb[:, e * 128:(e + 1) * 128], gatesT[:, tb * TB:(tb + 1) * TB], True, True,
                               [d_sel, d_gT], [dbk[6]])
                            act(gbc[gs_][:, tb * TB:(tb + 1) * TB], banks[6][:, :], AF.Copy, [dbk[6]], [d_gbc[gs_]], scale=1.0 / 1.702)
                        for j in range(8):
                            n = e * 8 + j
                            if n + NR - 1 < len(pieces):
                                load_piece(n + NR - 1)
                            s = n % NR
                            for tb in range(NTB):
                                pa, pb = (blk % 2) * 2, (blk % 2) * 2 + 1
                                k = blk % NT
                                for kc in range(8):
                                    mm(banks[pa][:, :], w1r[s][:, kc, 0:128], hT[:, kc, tb * TB:(tb + 1) * TB], kc == 0, kc == 7,
                                       [d_w1[s], d_hT[kc]], [dbk[pa]])
                                for kc in range(8):
                                    mm(banks[pb][:, :], w1r[s][:, kc, 128:256], hT[:, kc, tb * TB:(tb + 1) * TB], kc == 0, kc == 7,
                                       [d_w1[s], d_hT[kc]], [dbk[pb]])
                                ts("dve", xg[k][:], banks[pa][:, :], b1s[:, e * 16 + j:e * 16 + j + 1], 7.0, ALU.add, ALU.min,
                                   [dbk[pa], d_b1], [d_xg[k]])
                                act(sg[k][:], xg[k][:], AF.Silu, [d_xg[k]], [d_sg[k]], scale=1.702)
                                ts("dve", xl[k][:], banks[pb][:, :], b1s[:, e * 16 + 8 + j:e * 16 + 8 + j + 1], 8.0, ALU.add, ALU.min,
                                   [dbk[pb], d_b1], [d_xl[k]])
                                stt("dve", uu[k][:], xl[k][:], -6.0, sg[k][:], ALU.max, ALU.mult, [d_xl[k], d_sg[k]], [d_uu[k]])
                                tt("dve", actb[:, j, tb * TB:(tb + 1) * TB], uu[k][:], gbc[gs_][:, tb * TB:(tb + 1) * TB], ALU.mult,
                                   [d_uu[k], d_gbc[gs_]], [d_act[j][tb]])
                                blk += 1
                        s2 = 0
                        for i in range(8):
                            for tb in range(NTB):
                                bo = 4 + (ob % 2)
                                for j in range(8):
                                    mm(banks[bo][:, :], w2r[s2][:, j, i * 128:(i + 1) * 128], actb[:, j, tb * TB:(tb + 1) * TB],
                                       j == 0, j == 7, [d_w2[s2], d_act[j][tb]], [dbk[bo]])
                                stt("dve", x[:, i, tb * TB:(tb + 1) * TB], banks[bo][:, :], gf_ap[:, i:i + 1], x[:, i, tb * TB:(tb + 1) * TB],
                                    ALU.mult, ALU.add, [dbk[bo], d_mod, dx[i][tb]], [dx[i][tb]])
                                ob += 1
                        if e + 1 < nexp:
                            load_w2(e + 1)
                    for i in range(8):
                        for tb in range(NTB):
                            bo = 4 + (ob % 2)
                            mm(banks[bo][:, :], b2s[:, i * 128:(i + 1) * 128], gatesT[:, tb * TB:(tb + 1) * TB], True, True,
                               [d_b2, d_gT], [dbk[bo]])
                            stt("dve", x[:, i, tb * TB:(tb + 1) * TB], banks[bo][:, :], gf_ap[:, i:i + 1], x[:, i, tb * TB:(tb + 1) * TB],
                                ALU.mult, ALU.add, [dbk[bo], d_mod, dx[i][tb]], [dx[i][tb]])
                            ob += 1


        def attn_mixer(layer):
            LAM_INIT = 0.8 - 0.6 * float(np.exp(-0.3 * layer))
            with ExitStack() as ph:
                P.fence()
                hT = sb("hTa", [128, 8, S], BF16, ph); d_hT = [Dep() for _ in range(8)]
                yT = sb("yTa", [128, 8, S], BF16, ph); d_yT = [Dep() for _ in range(8)]
                norm_mod(ph, gsm[:, layer * 8:(layer + 1) * 8], modT[:, layer * 48:layer * 48 + 8], hT, d_hT)
                gm_ap = modT[:, layer * 48 + 16:layer * 48 + 24]
                with ExitStack() as loc:
                    P.fence()
                    lamb = sb("lamb", [128, 256], F32, loc); d_lam = Dep()
                    lsc = sb("lsc", [128, 8], F32, loc); d_lsc = Dep()
                    ltmp = sb("ltmp", [128, 128], F32, loc)
                    sgc = sb("sgc", [128, 1], F32, loc); d_sgc = Dep()
                    cfar = sb("cfar", [128, 16], F32, loc); d_cfar = Dep()
                    identb = sb("identb", [128, 128], BF16, loc); d_idb = Dep()
                    dma("sp", lamb[:], dr["lamb"][:, :], [], [d_lam], "a_lam")
                    dma("sp", sgc[:], dr["sublnT"][:, :], [], [d_sgc], "a_sg")
                    dma("sp", cfar[:], dr["cfar"][:, :], [], [d_cfar], "a_cf")
                    dma("pool", identb[:], dr["antiident"][:, :], [], [d_idb], "a_aid")
                    ts("dve", sgc[:], sgc[:], 1.0 - LAM_INIT, None, ALU.mult, None, [d_sgc], [d_sgc])
                    tt("dve", ltmp[:, 0:64], lamb[:, 0:64], lamb[:, 64:128], ALU.mult, [d_lam], [d_lsc])
                    tt("dve", ltmp[:, 64:128], lamb[:, 128:192], lamb[:, 192:256], ALU.mult, [d_lam], [d_lsc])
                    P.op("dve", lambda e: e.tensor_reduce(out=lsc[:, 0:2], in_=ltmp[:].rearrange("p (a b) -> p a b", a=2), axis=AX.X, op=ALU.add),
                         rd=[d_lsc], wr=[d_lsc])
                    act(lsc[:, 2:4], lsc[:, 0:2], AF.Exp, [d_lsc], [d_lsc])
                    tt("dve", lsc[:, 4:5], lsc[:, 3:4], lsc[:, 2:3], ALU.subtract, [d_lsc], [d_lsc])
                    ts("dve", lsc[:, 4:5], lsc[:, 4:5], -LAM_INIT, None, ALU.add, None, [d_lsc], [d_lsc])
                    gdr = nc.dram_tensor("gr_scratch", [8, 4096], F32)
                    d_gdr = Dep()
                    with ExitStack() as tbs:
                        P.fence()
                        tab = sb("tab", [32, 8], F32, tbs); d_tab = Dep()
                        oh = sb("oh", [32, 4096], F32, tbs); d_oh = Dep()
                        gsb = sb("gsb", [8, 4096], F32, tbs); d_gsb = Dep()
                        dma("sp", tab[:], dr["reltab"][:, :], [], [d_tab], "a_tab")
                        dma("sp", oh[:], dr["onehot"][:, :], [], [d_oh], "a_oh")
                        for cb_ in range(8):
                            mm(banks[7][0:8, :], tab[:, :], oh[:, cb_ * 512:(cb_ + 1) * 512], True, True, [d_tab, d_oh], [dbk[7]])
                            act(gsb[:, cb_ * 512:(cb_ + 1) * 512], banks[7][0:8, :], AF.Copy, [dbk[7]], [d_gsb])
                        dma("sp", gdr.ap()[:, :], gsb[:], [d_gsb], [d_gdr], "a_gdr")
                        if "gsb" in dbg:
                            out_dmas.append(dma("sp", dbg["gsb"][:, :], gsb[:], [d_gsb], [], "dbg_gsb"))
                    P.fence()
                    wq = sb("wq", [128, 8, 128], BF16, loc); d_wq = Dep()
                    wk = sb("wk", [128, 8, 128], BF16, loc); d_wk = Dep()
                    wv = sb("wv", [128, 8, 128], BF16, loc); d_wv = Dep()
                    qT = sb("qT", [128, S], BF16, loc); d_q = Dep()
                    kT = sb("kT", [128, S], BF16, loc); d_k = Dep()
                    vt = sb("vt", [128, 16, 128], BF16, loc); d_v = Dep()
                    bt = sb("bt", [128, 6, 512], BF16, loc); d_bt = Dep()
                    E = [sb("E%d" % i, [128, 512], BF16, loc) for i in range(3)]; d_E = [Dep() for _ in range(3)]
                    oh_ = sb("ohd", [128, S], F32, loc); d_ohd = [Dep() for _ in range(NTB)]
                    rz = sb("rz", [128, 512], F32, loc); d_rz = Dep()
                    o0 = sb("o0", [128, 512], F32, loc); d_o0 = Dep()
                    o1 = sb("o1", [128, 512], F32, loc); d_o1 = Dep()
                    sq = sb("asq", [128, S], BF16, loc); d_sq = Dep()
                    rs = sb("ars", [128, S], F32, loc); d_rs = [Dep() for _ in range(NTB)]
                    w_in = dr["attn_w_in"]
                    ei = 0
                    sbk = 0
                    for h in range(8):
                        for (wt, dw, c0) in ((wq, d_wq, h * 128), (wk, d_wk, 1024 + h * 128), (wv, d_wv, 2048 + h * 128)):
                            dma("pool", wt[:], w_in[:, c0:c0 + 128].rearrange("(kc p) f -> p kc f", p=128), [], [dw], "a_w%d" % c0)
                        for di in range(6):
                            delta = -128 + 128 * di
                            src = bass.AP(tensor=gdr, offset=h * 4096 + 2047 - delta - 127, ap=[[1, 128], [1, 512]])
                            dma("pool", bt[:, di, :], src, [d_gdr], [d_bt], "a_bt")
                        for tb in range(NTB):
                            for (wt, dw, dst, dd, sc_) in ((wq, d_wq, qT, d_q, 0.125), (wk, d_wk, kT, d_k, 1.0)):
                                bk = sbk % 2; sbk += 1
                                for kc in range(8):
                                    mm(banks[bk][:, :], wt[:, kc, :], hT[:, kc, tb * TB:(tb + 1) * TB], kc == 0, kc == 7, [dw, d_hT[kc]], [dbk[bk]])
                                act(dst[:, tb * TB:(tb + 1) * TB], banks[bk][:, :], AF.Copy, [dbk[bk]], [dd], scale=sc_)
                        for t in range(16):
                            bk = sbk % 2; sbk += 1
                            for kc in range(8):
                                mm(banks[bk][:, 0:128], hT[:, kc, t * 128:(t + 1) * 128], wv[:, kc, :], kc == 0, kc == 7, [d_wv, d_hT[kc]], [dbk[bk]])
                            cp("dve", vt[:, t, :], banks[bk][:, 0:128], [dbk[bk]], [d_v])
                        for qb in range(NTB):
                            for m in range(2):
                                bo, bz = 2 + 2 * m, 3 + 2 * m
                                def score(kt, bk):
                                    delta = 128 * kt - 512 * qb
                                    near = -255 < delta < 639
                                    mm(banks[bk][:, :], kT[m * 64:(m + 1) * 64, kt * 128:(kt + 1) * 128], qT[m * 64:(m + 1) * 64, qb * TB:(qb + 1) * TB],
                                       True, not near, [d_k, d_q], [dbk[bk]])
                                    if near:
                                        di = (delta + 128) // 128
                                        mm(banks[bk][:, :], identb[:], bt[:, di, :], False, True, [d_idb, d_bt], [dbk[bk]])
                                    return bk, near, delta
                                pend = score(0, sbk % 2); sbk += 1
                                for kt in range(16):
                                    bk, near, delta = pend
                                    if kt + 1 < 16:
                                        pend = score(kt + 1, sbk % 2); sbk += 1
                                    e_ = ei % 3; ei += 1
                                    if near:
                                        act(E[e_][:], banks[bk][:, :], AF.Exp, [dbk[bk]], [d_E[e_]])
                                    else:
                                        col = h * 2 + (1 if delta > 0 else 0)
                                        act(E[e_][:], banks[bk][:, :], AF.Exp, [dbk[bk], d_cfar], [d_E[e_]], bias=cfar[:, col:col + 1])
                                    mm(banks[bo][:, :], vt[:, kt, :], E[e_][:], kt == 0, kt == 15, [d_v, d_E[e_]], [dbk[bo]])
                                    mm(banks[bz][:, :], ones_bf[:], E[e_][:], kt == 0, kt == 15, [d_ones, d_E[e_]], [dbk[bz]])
                            P.op("dve", lambda e: e.reciprocal(out=rz[:], in_=banks[3][:, :]), rd=[dbk[3]], wr=[d_rz])
                            tt("dve", o0[:], banks[2][:, :], rz[:], ALU.mult, [dbk[2], d_rz], [d_o0])
                            P.op("dve", lambda e: e.reciprocal(out=rz[:], in_=banks[5][:, :]), rd=[dbk[5]], wr=[d_rz])
                            tt("dve", o1[:], banks[4][:, :], rz[:], ALU.mult, [dbk[4], d_rz], [d_o1])
                            stt("dve", oh_[:, qb * TB:(qb + 1) * TB], o1[:], lsc[:, 4:5], o0[:], ALU.mult, ALU.add, [d_o0, d_o1, d_lsc], [d_ohd[qb]])
                        if "ohd" in dbg and h == 7:
                            out_dmas.append(dma("sp", dbg["ohd"][:, :], oh_[:], d_ohd, [], "dbg_ohd"))
                        if "bt" in dbg and h == 7:
                            out_dmas.append(dma("pool", dbg["bt"][:, :], bt[:].rearrange("p a b -> p (a b)"), [d_bt], [], "dbg_bt"))
                            out_dmas.append(dma("pool", dbg["qT"][:, :], qT[:], [d_q], [], "dbg_qT"))
                            out_dmas.append(dma("pool", dbg["kT"][:, :], kT[:], [d_k], [], "dbg_kT"))
                            out_dmas.append(dma("pool", dbg["vt"][:, :], vt[:].rearrange("p a b -> p (a b)"), [d_v], [], "dbg_vt"))
                        act(sq[:], oh_[:], AF.Square, d_ohd, [d_sq])
                        for tb in range(NTB):
                            bk = 6 + tb % 2
                            mm(banks[bk][:, :], ones_bf[:], sq[:, tb * TB:(tb + 1) * TB], True, True, [d_ones, d_sq], [dbk[bk]])
                            act(rs[:, tb * TB:(tb + 1) * TB], banks[bk][:, :], AF.Sqrt, [dbk[bk], d_eps], [d_rs[tb]], bias=epsc[:, 0:1], scale=1.0 / 128)
                            P.op("dve", lambda e, tb=tb: e.reciprocal(out=rs[:, tb * TB:(tb + 1) * TB], in_=rs[:, tb * TB:(tb + 1) * TB]), rd=[d_rs[tb]], wr=[d_rs[tb]])
                        stt("dve", yT[:, h, :], oh_[:], sgc[:, 0:1], rs[:], ALU.mult, ALU.mult, d_ohd + d_rs + [d_sgc], [d_yT[h]])
                out_proj(ph, dr["attn_w_out"], yT, d_yT, gm_ap)

        def out_proj(ph, w_out, yT, d_yT, gm_ap):
            with ExitStack() as loc:
                P.fence()
                wo = [sb("wo%d" % i, [128, 8, 128], BF16, loc) for i in range(2)]; d_wo = [Dep(), Dep()]
                ob = 0
                for i in range(8):
                    s = i % 2
                    dma("pool", wo[s][:], w_out[:, i * 128:(i + 1) * 128].rearrange("(kc p) f -> p kc f", p=128), [], [d_wo[s]], "wo%d" % s)
                    for tb in range(NTB):
                        bo = ob % 2; ob += 1
                        for kc in range(8):
                            mm(banks[bo][:, :], wo[s][:, kc, :], yT[:, kc, tb * TB:(tb + 1) * TB], kc == 0, kc == 7, [d_wo[s], d_yT[kc]], [dbk[bo]])
                        stt("dve", x[:, i, tb * TB:(tb + 1) * TB], banks[bo][:, :], gm_ap[:, i:i + 1], x[:, i, tb * TB:(tb + 1) * TB],
                            ALU.mult, ALU.add, [dbk[bo], d_mod, dx[i][tb]], [dx[i][tb]])

        class Arr:
            def __init__(self, ap, deps):
                self.ap = ap
                self.deps = deps

        def ab_mixer(layer):
            W_IN = dr["ab_w_in"]
            ydram = nc.dram_tensor("y_scratch", [D, S], BF16)
            ydv = ydram.ap()
            d_yd = Dep()
            gm_ap = modT[:, layer * 48 + 16:layer * 48 + 24]
            with ExitStack() as ph:
                P.fence()
                hT = sb("hTm", [128, 8, S], BF16, ph); d_hT = [Dep() for _ in range(8)]
                norm_mod(ph, gsm[:, layer * 8:(layer + 1) * 8], modT[:, layer * 48:layer * 48 + 8], hT, d_hT)
                with ExitStack() as sc:
                    P.fence()
                    d_big = [Dep(), Dep()]
                    bigs = [(banks[0], d_big[0]), (banks[1], d_big[1])]
                    smalls = [(banks[b][:, 0:128], Dep()) for b in (2, 3, 4, 5)]
                    yregs = [(banks[b][:, 0:128], Dep()) for b in (6, 7)]
                    alld = d_big + [d for _, d in smalls] + [d for _, d in yregs]
                    scr = sb("barscr", [128, 1], F32, sc)
                    P.op("dve", lambda e: e.memset(scr[:], 0.0), rd=list(dbk), wr=alld)
                    cnt = {"big": 0, "small": 0, "y": 0, "ev": 0, "w": 0, "tok": 0, "mat": 0, "bg": 0}

                    def big():
                        cnt["big"] += 1
                        return bigs[cnt["big"] % 2]

                    def small():
                        cnt["small"] += 1
                        return smalls[cnt["small"] % 4]

                    def yreg():
                        cnt["y"] += 1
                        return yregs[cnt["y"] % 2]

                    msk = sb("msk", [128, 1536], F32, sc); d_msk = Dep()
                    dma("sp", msk[:], dr["masks"][:, :], [], [d_msk], "b_msk")
                    IU = msk[:, 0:128]; SU = msk[:, 128:256]; IL = msk[:, 256:384]; SLm = msk[:, 384:512]
                    CM = msk[:, 512:1024].rearrange("p (c k) -> p c k", c=4)
                    IEX = msk[:, 1024:1536].rearrange("p (c k) -> p c k", c=4)
                    ones32 = sb("ones32", [128, 128], F32, sc); d_o32 = Dep()
                    P.op("pool", lambda e: e.memset(ones32[:], 1.0), wr=[d_o32])
                    epsln = sb("epsln", [128, 1], F32, sc); d_epsln = Dep()
                    P.op("pool", lambda e: e.memset(epsln[:], 64e-5), wr=[d_epsln])
                    m32 = sb("m32", [128, S], BF16, sc); d_m32 = Dep()
                    P.op("pool", lambda e: e.memset(m32[:], 1.0), wr=[d_m32])
                    P.op("pool", lambda e: e.memset(m32[:].rearrange("p (c t) -> p c t", t=32)[:, :, 0:1], 0.0), rd=[d_m32], wr=[d_m32])
                    lbT = sb("lbT", [128, 16], F32, sc); d_lb = Dep()
                    lbc = sb("lbc", [128, 8], F32, sc); oml = sb("oml", [128, 8], F32, sc)
                    dma("sp", lbT[:], dr["lbT"][:, :], [], [d_lb], "b_lb")
                    lbv = lbT[:].rearrange("p (d s h) -> p d s h", d=2, s=2)
                    tt("dve", lbc[:].rearrange("p (d h) -> p d h", d=2), lbv[:, :, 0, :], lbv[:, :, 1, :], ALU.subtract, [d_lb], [d_lb])
                    act(lbc[:], lbc[:], AF.Sigmoid, [d_lb], [d_lb])
                    ts("dve", oml[:], lbc[:], -1.0, 1.0, ALU.mult, ALU.add, [d_lb], [d_lb])
                    hng = sb("hng", [128, 1], F32, sc); d_hng = Dep()
                    dma("sp", hng[:], dr["hngT"][:, :], [], [d_hng], "b_hng")
                    rwp = sb("rwp", [64, 88], F32, sc); d_rwp = Dep()
                    rwo = sb("rwo", [64, 88], F32, sc); rwh = sb("rwh", [64, 88], F32, sc)
                    dma("sp", rwp[:], dr["rwp"][:, :], [], [d_rwp], "b_rwp")
                    ts("dve", rwo[:], rwp[:], -1.0, 1.0, ALU.mult, ALU.add, [d_rwp], [d_rwp])
                    ts("dve", rwh[:], rwp[:], 0.5, None, ALU.mult, None, [d_rwp], [d_rwp])
                    rwp2 = sb("rwp2", [128, 3], F32, sc); d_rwp2 = Dep()
                    rwo2 = sb("rwo2", [128, 3], F32, sc); rwh2 = sb("rwh2", [128, 3], F32, sc)
                    dma("sp", rwp2[:], dr["rwp2"][:, :], [], [d_rwp2], "b_rwp2")
                    ts("dve", rwo2[:], rwp2[:], -1.0, 1.0, ALU.mult, ALU.add, [d_rwp2], [d_rwp2])
                    ts("dve", rwh2[:], rwp2[:], 0.5, None, ALU.mult, None, [d_rwp2], [d_rwp2])
                    rw2b = sb("rw2b", [128, 512], BF16, sc); d_rw2 = Dep()
                    ra2b = sb("ra2b", [64, 512], BF16, sc); d_ra2 = Dep()
                    rg2b = sb("rg2b", [128, 512], BF16, sc); d_rg2 = Dep()
                    dma("pool", rw2b[:], dr["rw2"][:, :], [], [d_rw2], "b_rw2")
                    dma("pool", ra2b[:], dr["ra2"][:, :], [], [d_ra2], "b_ra2")
                    dma("pool", rg2b[:], dr["rg2"][:, :], [], [d_rg2], "b_rg2")

                    wbx = [sb("wbx%d" % i, [128, S], F32, sc) for i in range(2)]
                    SLT = [Arr(x[:, i, :], dx[i]) for i in range(8)] + [Arr(wbx[i][:], [Dep()]) for i in range(2)]
                    wbuf = [sb("abwb%d" % i, [128, 8, 128], BF16, sc) for i in range(2)]; d_wbuf = [Dep(), Dep()]
                    WCt = [sb("WC%d" % i, [128, 64], F32, sc) for i in range(2)]; d_WC = [Dep(), Dep()]
                    STt = [sb("ST%d" % i, [128, 128], F32, sc) for i in range(2)]; d_ST = [Dep(), Dep()]
                    NTOK, NMAT, NBG = 10, 44, 10
                    tokt = [(sb("tok%d" % i, [128, 128], F32, sc), Dep()) for i in range(NTOK)]
                    matt = [(sb("mat%d" % i, [128, 128], F32, sc), Dep()) for i in range(NMAT)]
                    bgt = [(sb("bg%d" % i, [128, 512], F32, sc), Dep()) for i in range(NBG)]
                    yo = [sb("yo%d" % i, [128, S], BF16, sc) for i in range(2)]; d_yo = [Dep(), Dep()]
                    tott = sb("tott", [128, 64], F32, sc); d_tott = Dep()

                    def tok():
                        cnt["tok"] += 1
                        return tokt[cnt["tok"] % NTOK]

                    def mat():
                        cnt["mat"] += 1
                        return matt[cnt["mat"] % NMAT]

                    def bg():
                        cnt["bg"] += 1
                        return bgt[cnt["bg"] % NBG]

                    def evac(out, in_, rd, wr, scale=None, eng=None):
                        cnt["ev"] += 1
                        if eng is None:
                            eng = "act"
                        if eng == "act":
                            return act(out, in_, AF.Copy, rd, wr, scale=(1.0 if scale is None else scale))
                        if scale is None:
                            return cp("dve", out, in_, rd, wr)
                        return ts("dve", out, in_, scale, None, ALU.mult, None, rd, wr)

                    F32R = mybir.dt.float32r

                    def mmr(out, lhsT, rhs, start, stop, rd, wr):
                        return mm(out, lhsT.bitcast(F32R), rhs.bitcast(F32R), start, stop, rd, wr)

                    def mms(out, dout, terms, last_stop=True, first_start=True, r32=False):
                        n = len(terms)
                        for i, (l, r, deps) in enumerate(terms):
                            (mmr if r32 else mm)(out, l, r, first_start and i == 0, last_stop and i == n - 1, deps, [dout])

                    def proj(col0, ncols, consume):
                        s = cnt["w"] % 2; cnt["w"] += 1
                        dma("pool", wbuf[s][:, :, 0:ncols], W_IN[:, col0:col0 + ncols].rearrange("(kc p) f -> p kc f", p=128),
                            [], [d_wbuf[s]], "abw%d" % s)
                        for tb in range(NTB):
                            bk, dbk_ = big()
                            for kc in range(8):
                                mm(bk[0:ncols, :], wbuf[s][:, kc, 0:ncols], hT[:, kc, tb * TB:(tb + 1) * TB], kc == 0, kc == 7,
                                   [d_wbuf[s], d_hT[kc]], [dbk_])
                            consume(tb, bk[0:ncols, :], dbk_)

                    def proj_to(col0, ncols, dst, func=AF.Copy, bias=0.0, extra_rd=()):
                        def c(tb, ps, dps):
                            act(dst.ap[0:ncols, tb * TB:(tb + 1) * TB], ps, func, [dps] + list(extra_rd), dst.deps, bias=bias)
                        proj(col0, ncols, c)

                    def shiftmix(raw, tmp, n, omu, hmu, dpar):
                        r = raw.ap; t = tmp.ap
                        tt("pool", t[0:n, 1:S - 1], r[0:n, 0:S - 2], r[0:n, 2:S], ALU.add, raw.deps, tmp.deps)
                        cp("pool", t[0:n, 0:1], r[0:n, 1:2], raw.deps, tmp.deps)
                        cp("pool", t[0:n, S - 1:S], r[0:n, S - 2:S - 1], raw.deps, tmp.deps)
                        ts("dve", r[0:n, :], r[0:n, :], omu, None, ALU.mult, None, raw.deps + [dpar], raw.deps)
                        stt("dve", r[0:n, :], t[0:n, :], hmu, r[0:n, :], ALU.mult, ALU.add, tmp.deps + raw.deps + [dpar], raw.deps)

                    def v3(ap, n):
                        return ap[0:n, :].rearrange("p (c t) -> p c t", t=32)

                    def cumsum(ld, cum, n):
                        P.op("dve", lambda e: e.tensor_tensor_scan(out=cum.ap[0:n, :], data0=m32[0:n, :],
                                                                   data1=ld.ap[0:n, :], initial=0.0, op0=ALU.mult, op1=ALU.add),
                             rd=ld.deps + [d_m32], wr=cum.deps)

                    def dir_arrays(dirn, n, LDa, CUMa, D2, D3, srcR, srcK, srcB, srcKK, wc, dwc):
                        cumsum(LDa, CUMa, n)
                        c3 = v3(CUMa.ap, n)
                        act(wc[0:n, :], c3[:, :, 31], AF.Exp, CUMa.deps, [dwc])
                        cp("dve", tott[0:n, :], c3[:, :, 31], CUMa.deps, [d_tott])
                        delta = srcB is not None
                        if dirn == 0:
                            act(D2.ap[0:n, :], CUMa.ap[0:n, :], AF.Exp, CUMa.deps, D2.deps)
                            tt("dve", D2.ap[0:n, :], D2.ap[0:n, :], srcR.ap[0:n, :], ALU.mult, D2.deps + srcR.deps, D2.deps)
                            act(D3.ap[0:n, :], CUMa.ap[0:n, :], AF.Exp, CUMa.deps, D3.deps, scale=-1.0)
                            if delta:
                                tt("dve", LDa.ap[0:n, :], CUMa.ap[0:n, :], LDa.ap[0:n, :], ALU.subtract, CUMa.deps + LDa.deps, LDa.deps)
                                act(LDa.ap[0:n, :], LDa.ap[0:n, :], AF.Exp, LDa.deps, LDa.deps)
                                tt("pool", LDa.ap[0:n, :], LDa.ap[0:n, :], srcKK.ap[0:n, :], ALU.mult, LDa.deps + srcKK.deps, LDa.deps)
                                tt("pool", CUMa.ap[0:n, :], D3.ap[0:n, :], srcB.ap[0:n, :], ALU.mult, D3.deps + srcB.deps + LDa.deps, CUMa.deps)
                            tt("dve", D3.ap[0:n, :], D3.ap[0:n, :], srcK.ap[0:n, :], ALU.mult, D3.deps + srcK.deps + CUMa.deps, D3.deps)
                            return D2, D3, CUMa, LDa
                        else:
                            tt("dve", c3, c3, tott[0:n, :, None].to_broadcast([n, 64, 32]), ALU.subtract, CUMa.deps + [d_tott], CUMa.deps)
                            if delta:
                                act(D2.ap[0:n, :], CUMa.ap[0:n, :], AF.Exp, CUMa.deps, D2.deps, scale=-1.0)
                                tt("pool", D2.ap[0:n, :], D2.ap[0:n, :], srcKK.ap[0:n, :], ALU.mult, D2.deps + srcKK.deps, D2.deps)
                            tt("dve", LDa.ap[0:n, :], LDa.ap[0:n, :], CUMa.ap[0:n, :], ALU.subtract, LDa.deps + CUMa.deps, LDa.deps)
                            act(D3.ap[0:n, :], LDa.ap[0:n, :], AF.Exp, LDa.deps, D3.deps)
                            tt("dve", D3.ap[0:n, :], D3.ap[0:n, :], srcR.ap[0:n, :], ALU.mult, D3.deps + srcR.deps, D3.deps)
                            act(LDa.ap[0:n, :], LDa.ap[0:n, :], AF.Exp, LDa.deps, LDa.deps, scale=-1.0)
                            if delta:
                                tt("pool", CUMa.ap[0:n, :], LDa.ap[0:n, :], srcB.ap[0:n, :], ALU.mult, LDa.deps + srcB.deps + D2.deps, CUMa.deps)
                            tt("dve", LDa.ap[0:n, :], LDa.ap[0:n, :], srcK.ap[0:n, :], ALU.mult, LDa.deps + srcK.deps + CUMa.deps, LDa.deps)
                            return D3, LDa, CUMa, D2

                    def scan(dk, delta, dirn, rT, kT_, vA, bT, aT, wc, dwc, osum, first):
                        fwd = dirn == 0
                        m_iu = IU if fwd else IL
                        m_su = SU if fwd else SLm
                        m_sl = SLm if fwd else SU
                        idk = identf[0:dk, 0:dk]
                        P.op("pool", lambda e: e.memset(STt[0][:], 0.0), wr=[d_ST[0]])
                        st = {"cur": 0, "seqdone": True}
                        tiles = list(range(16)) if fwd else list(range(15, -1, -1))
                        chunks = list(range(4)) if fwd else list(range(3, -1, -1))
                        res = {}

                        def tr(arr, n0, role, par):
                            reg, dreg = small()
                            P.op("pe", lambda e: e.transpose(reg[:, 0:dk], arr.ap[0:dk, n0:n0 + 128], idk), rd=arr.deps + [d_ident], wr=[dreg])
                            t, d = tokt[role * 2 + par]
                            evac(t[:, 0:dk].bitcast(F32R), reg[:, 0:dk], [dreg], [d])
                            return t, d

                        def mmev(terms, rows, cols, role, par, scale=None, mask=None, r32=False, add=None):
                            reg, dreg = small()
                            mms(reg[0:rows, 0:cols], dreg, terms, r32=r32)
                            t, d = matt[role * 2 + par]
                            o_ = t[0:rows, 0:cols].bitcast(F32R)
                            if add is not None:
                                aap, adeps = add
                                tt("dve", o_, reg[0:rows, 0:cols], aap, ALU.add, [dreg] + list(adeps), [d])
                            elif mask is None:
                                evac(o_, reg[0:rows, 0:cols], [dreg], [d], scale=scale)
                            else:
                                stt("dve", o_, reg[0:rows, 0:cols], (1.0 if scale is None else scale), mask[0:rows, 0:cols],
                                    ALU.mult, ALU.mult, [dreg, d_msk], [d])
                            return t, d

                        def prep(n, par):
                            n0 = n * 128
                            rs_ = rT.ap[0:dk, n0:n0 + 128]; ks_ = kT_.ap[0:dk, n0:n0 + 128]
                            ktok, d_ktok = tr(kT_, n0, 0, par)
                            vtok, d_vtok = tr(vA, n0, 1, par)
                            yield
                            MrkT, d_mrk = mmev([(ks_, rs_, kT_.deps + rT.deps)], 128, 128, 0, par, mask=m_iu)
                            vexp, d_vexp = bgt[0 * 2 + par]
                            tt("pool", vexp[:, 0:4 * dk].bitcast(F32R).rearrange("p (c k) -> p c k", c=4), vtok[:, None, 0:dk].to_broadcast([128, 4, dk]),
                               CM[:, :, 0:dk], ALU.mult, [d_vtok, d_msk], [d_vexp])
                            yield
                            QT = d_QT = GT = d_GT = None
                            if delta:
                                as_ = aT.ap[0:dk, n0:n0 + 128]; bs_ = bT.ap[0:dk, n0:n0 + 128]
                                btok, d_btok = tr(bT, n0, 2, par)
                                atok, d_atok = tr(aT, n0, 3, par)
                                yield
                                N1, d_N1 = mmev([(as_, bs_, aT.deps + bT.deps)], 128, 128, 1, par, scale=-1.0, mask=m_sl)
                                N1T, d_N1T = mmev([(bs_, as_, aT.deps + bT.deps)], 128, 128, 2, par, scale=-1.0, mask=m_su)
                                yield
                                Y, d_Y = matt[3 * 2 + par]
                                tt("pool", Y[:, :].bitcast(F32R), N1T[:, :], identf[:, :], ALU.add, [d_N1T, d_ident], [d_Y])
                                Np, d_Np, NpT, d_NpT = N1, d_N1, N1T, d_N1T
                                for lvl in range(4):
                                    N2, d_N2 = mmev([(NpT[:, :], Np[:, :], [d_Np, d_NpT])], 128, 128, 4 + 3 * lvl, par, r32=True)
                                    if lvl < 3:
                                        N2T, d_N2T = mmev([(Np[:, :], NpT[:, :], [d_Np, d_NpT])], 128, 128, 5 + 3 * lvl, par, r32=True)
                                    yield
                                    Y, d_Y = mmev([(N2[:, :], Y[:, :], [d_N2, d_Y])], 128, 128, 6 + 3 * lvl, par, r32=True, add=(Y[:, :], [d_Y]))
                                    yield
                                    Np, d_Np, NpT, d_NpT = N2, d_N2, N2T, d_N2T
                                TT, d_TT = Y, d_Y
                                LakT, d_lak = mmev([(ks_, as_, kT_.deps + aT.deps)], 128, 128, 16, par, mask=m_su)
                                MrbT, d_mrb = mmev([(bs_, rs_, bT.deps + rT.deps)], 128, 128, 20, par, mask=m_iu)
                                yield
                                Z, d_Z = mmev([(LakT[:, :], vtok[:, 0:dk], [d_lak, d_vtok])], 128, dk, 17, par, r32=True)
                                negTA, d_negTA = mmev([(TT[:, :], atok[:, 0:dk], [d_TT, d_atok])], 128, dk, 19, par, scale=-1.0, r32=True)
                                bexp, d_bexp = bgt[2 * 2 + par]
                                tt("pool", bexp[:, 0:4 * dk].bitcast(F32R).rearrange("p (c k) -> p c k", c=4), btok[:, None, 0:dk].to_broadcast([128, 4, dk]),
                                   CM[:, :, 0:dk], ALU.mult, [d_btok, d_msk], [d_bexp])
                                yield
                                while not st["seqdone"]:
                                    yield
                                negP, d_negP = mmev([(TT[:, :], Z[:, 0:dk], [d_TT, d_Z])], 128, dk, 18, par, scale=-1.0, r32=True)
                                QT, d_QT = mmev([(negTA[:, 0:dk], MrbT[:, :], [d_negTA, d_mrb])], dk, 128, 21, par, r32=True, add=(rs_, rT.deps))
                                yield
                                pexp, d_pexp = bgt[1 * 2 + par]
                                tt("pool", pexp[:, 0:4 * dk].bitcast(F32R).rearrange("p (c k) -> p c k", c=4), negP[:, None, 0:dk].to_broadcast([128, 4, dk]),
                                   CM[:, :, 0:dk], ALU.mult, [d_negP, d_msk], [d_pexp])
                                greg, d_greg = big()
                                mms(greg[0:dk, 0:4 * dk], d_greg, [(negTA[:, 0:dk], bexp[:, 0:4 * dk], [d_negTA, d_bexp])], r32=True)
                                GT, d_GT = bgt[3 * 2 + par]
                                tt("dve", GT[0:dk, 0:4 * dk].rearrange("p (c k) -> p c k", c=4), greg[0:dk, 0:4 * dk].rearrange("p (c k) -> p c k", c=4),
                                   IEX[0:dk, :, 0:dk], ALU.add, [d_greg, d_msk], [d_GT])
                                yield
                                hreg, d_hreg = big()
                                mms(hreg[0:dk, 0:4 * dk], d_hreg, [(ktok[:, 0:dk], vexp[:, 0:4 * dk], [d_ktok, d_vexp]),
                                                                  (btok[:, 0:dk], pexp[:, 0:4 * dk], [d_btok, d_pexp])], r32=True)
                            else:
                                while not st["seqdone"]:
                                    yield
                                hreg, d_hreg = big()
                                mms(hreg[0:dk, 0:4 * dk], d_hreg, [(ktok[:, 0:dk], vexp[:, 0:4 * dk], [d_ktok, d_vexp])], r32=True)
                            Hs, d_Hs = bgt[4 * 2 + par]
                            tt("dve", Hs[0:dk, 0:4 * dk].rearrange("p (c k) -> p c k", c=4), hreg[0:dk, 0:4 * dk].rearrange("p (c k) -> p c k", c=4),
                               wc[0:dk, n * 4:(n + 1) * 4][:, :, None].to_broadcast([dk, 4, dk]), ALU.mult, [d_hreg, dwc], [d_Hs])
                            yield
                            while not st["seqdone"]:
                                yield
                            yr, d_yr = yregs[par]
                            if delta:
                                mms(yr[0:dk, :], d_yr, [(vtok[:, 0:dk], MrkT[:, :], [d_vtok, d_mrk]),
                                                        (negP[:, 0:dk], MrbT[:, :], [d_negP, d_mrb])], last_stop=False, r32=True)
                            else:
                                mms(yr[0:dk, :], d_yr, [(vtok[:, 0:dk], MrkT[:, :], [d_vtok, d_mrk])], last_stop=False, r32=True)
                            res[n] = (QT, d_QT, GT, d_GT, Hs, d_Hs, yr, d_yr)

                        def seqs(ns):
                            for n in ns:
                                n0 = n * 128
                                QT, d_QT, GT, d_GT, Hs, d_Hs, yr, d_yr = res.pop(n)
                                for ci, c in enumerate(chunks):
                                    cc = slice(c * 32, (c + 1) * 32)
                                    cur = st["cur"]
                                    stc = STt[cur]; dstc = d_ST[cur]
                                    stn = STt[1 - cur]; dstn = d_ST[1 - cur]
                                    if delta:
                                        mm(yr[0:dk, cc], stc[0:dk, 0:dk], QT[0:dk, cc], False, ci == 3, [dstc, d_QT], [d_yr])
                                        sr, d_sr = small()
                                        mm(sr[0:dk, 0:dk], GT[0:dk, c * dk:(c + 1) * dk], stc[0:dk, 0:dk], True, True, [d_GT, dstc], [d_sr])
                                        stt("dve", stn[0:dk, 0:dk], sr[0:dk, 0:dk], wc[0:dk, n * 4 + c:n * 4 + c + 1], Hs[0:dk, c * dk:(c + 1) * dk],
                                            ALU.mult, ALU.add, [d_sr, dwc, d_Hs], [dstn])
                                    else:
                                        mm(yr[0:dk, cc], stc[0:dk, 0:dk], rT.ap[0:dk, n0 + c * 32:n0 + (c + 1) * 32], False, ci == 3,
                                           [dstc] + rT.deps, [d_yr])
                                        stt("dve", stn[0:dk, 0:dk], stc[0:dk, 0:dk], wc[0:dk, n * 4 + c:n * 4 + c + 1], Hs[0:dk, c * dk:(c + 1) * dk],
                                            ALU.mult, ALU.add, [dstc, dwc, d_Hs], [dstn])
                                    st["cur"] = 1 - cur
                                    yield
                                if first:
                                    evac(osum.ap[0:dk, n0:n0 + 128], yr[0:dk, :], [d_yr], osum.deps)
                                else:
                                    tt("dve", osum.ap[0:dk, n0:n0 + 128], yr[0:dk, :], osum.ap[0:dk, n0:n0 + 128], ALU.add, [d_yr] + osum.deps, osum.deps)
                                yield
                            st["seqdone"] = True

                        pairs = [tiles[i:i + 2] for i in range(0, 16, 2)]
                        prev = None
                        for pr in pairs + [None]:
                            gens = []
                            if prev is not None:
                                st["seqdone"] = False
                                gens.append(seqs(prev))
                            if pr is not None:
                                gens += [prep(n, i) for i, n in enumerate(pr)]
                            while gens:
                                for g in list(gens):
                                    try:
                                        next(g)
                                    except StopIteration:
                                        gens.remove(g)
                            prev = pr

                    ycount = [0]

                    def y_out(src_ap, n, row0, rd):
                        s = ycount[0] % 2; ycount[0] += 1
                        return s

                    for h in range(cfg.get('hg_heads', 4)):
                        Qs, Vv, FG, OS, D0, D1, D2, D3 = SLT[0], SLT[1], SLT[2], SLT[3], SLT[4], SLT[5], SLT[6], SLT[7]
                        proj_to(h * 128, 128, Qs, AF.Silu)
                        proj_to(1536 + h * 128, 128, Vv, AF.Copy)
                        for dirn in range(cfg.get('dirs', 2)):
                            col = dirn * 4 + h
                            proj_to(512 + dirn * 512 + h * 128, 128, FG, AF.Sigmoid)
                            ts("dve", FG.ap, FG.ap, oml[:, col:col + 1], lbc[:, col:col + 1], ALU.mult, ALU.add, FG.deps + [d_lb], FG.deps)
                            act(D0.ap, FG.ap, AF.Ln, FG.deps, D0.deps)
                            ts("dve", FG.ap, FG.ap, -1.0, 1.0, ALU.mult, ALU.add, FG.deps, FG.deps)
                            w = dirn
                            rT_, kT2, _, _ = dir_arrays(dirn, 128, D0, D1, D2, D3, Qs, FG, None, None, WCt[w], d_WC[w])
                            scan(128, False, dirn, rT_, kT2, Vv, None, None, WCt[w], d_WC[w], OS, dirn == 0)
                        sqa = D0
                        act(sqa.ap, OS.ap, AF.Square, OS.deps, sqa.deps)
                        for tb in range(NTB):
                            bk, dbk_ = big()
                            mm(bk[:, :], ones32[:, :], sqa.ap[:, tb * TB:(tb + 1) * TB], True, True, [d_o32] + sqa.deps, [dbk_])
                            act(D1.ap[:, tb * TB:(tb + 1) * TB], bk[:, :], AF.Sqrt, [dbk_, d_eps], D1.deps, bias=epsc[:, 0:1], scale=1.0 / 128)
                        P.op("dve", lambda e, a=D1.ap: e.reciprocal(out=a, in_=a), rd=D1.deps, wr=D1.deps)
                        proj_to(2048 + h * 128, 128, D2, AF.Silu)
                        stt("dve", D1.ap, OS.ap, hng[:, 0:1], D1.ap, ALU.mult, ALU.mult, OS.deps + D1.deps + [d_hng], D1.deps)
                        s = ycount[0] % 2; ycount[0] += 1
                        tt("dve", yo[s][:, :], D1.ap, D2.ap, ALU.mult, D1.deps + D2.deps, [d_yo[s]])
                        dma("sp", ydv[h * 128:(h + 1) * 128, :], yo[s][:, :], [d_yo[s]], [d_yd], "b_yo%d" % s)

                    BO = 2560
                    WL = sb("WL", [128, S], BF16, sc); d_WL = Dep()
                    AL = sb("AL", [64, S], BF16, sc); d_AL = Dep()
                    GL = sb("GL", [128, S], BF16, sc); d_GL = Dep()
                    T0, T1 = SLT[8], SLT[9]
                    proj_to(BO + 1536, 128, T0)
                    shiftmix(T0, T1, 128, rwo2[:, 0:1], rwh2[:, 0:1], d_rwp2)
                    act(WL[:, :], T0.ap, AF.Tanh, T0.deps, [d_WL])
                    proj_to(BO + 1664, 64, T0)
                    shiftmix(T0, T1, 64, rwo2[0:64, 1:2], rwh2[0:64, 1:2], d_rwp2)
                    cp("dve", AL[:, :], T0.ap[0:64, :], T0.deps, [d_AL])
                    proj_to(BO + 1728, 128, T0)
                    shiftmix(T0, T1, 128, rwo2[:, 2:3], rwh2[:, 2:3], d_rwp2)
                    act(GL[:, :], T0.ap, AF.Sigmoid, T0.deps, [d_GL])
                    for h in range(cfg.get('rw_heads', 8)):
                        R, K, V, Bv, KK, OS, D0, D1, D2, D3 = SLT
                        pc = lambda i: rwp[:, h * 11 + i:h * 11 + i + 1]
                        hs = slice(h * 64, (h + 1) * 64)
                        for (dst, c0, mi) in ((R, 0, 0), (K, 512, 1), (V, 1024, 2)):
                            proj_to(BO + c0 + h * 64, 64, dst)
                            shiftmix(dst, D0, 64, rwo[:, h * 11 + mi:h * 11 + mi + 1], rwh[:, h * 11 + mi:h * 11 + mi + 1], d_rwp)
                        for tb in range(NTB):
                            bk, dbk_ = big()
                            mm(bk[0:64, :], ra2b[:, hs], AL[:, tb * TB:(tb + 1) * TB], True, True, [d_ra2, d_AL], [dbk_])
                            act(Bv.ap[0:64, tb * TB:(tb + 1) * TB], bk[0:64, :], AF.Sigmoid, [dbk_, d_rwp], Bv.deps, bias=pc(5))
                        ts("dve", KK.ap[0:64, :], K.ap[0:64, :], pc(6), None, ALU.mult, None, K.deps + [d_rwp], KK.deps)
                        act(D0.ap[0:64, :], KK.ap[0:64, :], AF.Square, KK.deps, D0.deps)
                        for tb in range(NTB):
                            bk, dbk_ = big()
                            mm(bk[0:64, :], ones32[0:64, 0:64], D0.ap[0:64, tb * TB:(tb + 1) * TB], True, True, [d_o32] + D0.deps, [dbk_])
                            act(D1.ap[0:64, tb * TB:(tb + 1) * TB], bk[0:64, :], AF.Sqrt, [dbk_], D1.deps)
                        ts("dve", D1.ap[0:64, :], D1.ap[0:64, :], 1e-12, None, ALU.max, None, D1.deps, D1.deps)
                        P.op("dve", lambda e, a=D1.ap[0:64, :]: e.reciprocal(out=a, in_=a), rd=D1.deps, wr=D1.deps)
                        tt("dve", KK.ap[0:64, :], KK.ap[0:64, :], D1.ap[0:64, :], ALU.mult, KK.deps + D1.deps, KK.deps)
                        ts("dve", D0.ap[0:64, :], Bv.ap[0:64, :], -1.0, pc(7), ALU.add, ALU.mult, Bv.deps + D0.deps + [d_rwp], D0.deps)
                        stt("dve", K.ap[0:64, :], D0.ap[0:64, :], 1.0, K.ap[0:64, :], ALU.add, ALU.mult, D0.deps + K.deps, K.deps)
                        tt("dve", Bv.ap[0:64, :], Bv.ap[0:64, :], KK.ap[0:64, :], ALU.mult, Bv.deps + KK.deps + D0.deps, Bv.deps)
                        for dirn in range(cfg.get('dirs', 2)):
                            for tb in range(NTB):
                                bk, dbk_ = big()
                                mm(bk[0:64, :], rw2b[dirn * 64:(dirn + 1) * 64, hs], WL[dirn * 64:(dirn + 1) * 64, tb * TB:(tb + 1) * TB], True, True,
                                   [d_rw2, d_WL], [dbk_])
                                act(D0.ap[0:64, tb * TB:(tb + 1) * TB], bk[0:64, :], AF.Sigmoid, [dbk_, d_rwp], D0.deps, bias=pc(3 + dirn))
                            ts("dve", D0.ap[0:64, :], D0.ap[0:64, :], -float(np.exp(-0.5)), None, ALU.mult, None, D0.deps, D0.deps)
                            w = dirn
                            rT_, kT2, bT2, aT2 = dir_arrays(dirn, 64, D0, D1, D2, D3, R, K, Bv, KK, WCt[w], d_WC[w])
                            scan(64, True, dirn, rT_, kT2, V, bT2, aT2, WCt[w], d_WC[w], OS, dirn == 0)
                        Y_ = OS
                        for tb in range(NTB):
                            tsl = slice(tb * TB, (tb + 1) * TB)
                            bk, dbk_ = big()
                            mm(bk[0:64, :], ones32[0:64, 0:64], Y_.ap[0:64, tsl], True, True, [d_o32] + Y_.deps, [dbk_])
                            stt("dve", D0.ap[0:64, tsl], bk[0:64, :], -1.0 / 64, Y_.ap[0:64, tsl], ALU.mult, ALU.add, [dbk_] + Y_.deps + D0.deps, D0.deps)
                        act(D1.ap[0:64, :], D0.ap[0:64, :], AF.Square, D0.deps + D1.deps, D1.deps)
                        for tb in range(NTB):
                            tsl = slice(tb * TB, (tb + 1) * TB)
                            bk, dbk_ = big()
                            mm(bk[0:64, :], ones32[0:64, 0:64], D1.ap[0:64, tsl], True, True, [d_o32] + D1.deps, [dbk_])
                            act(D2.ap[0:64, tsl], bk[0:64, :], AF.Sqrt, [dbk_, d_epsln] + D2.deps, D2.deps, bias=epsln[0:64, 0:1], scale=1.0 / 64)
                        P.op("dve", lambda e, a=D2.ap[0:64, :]: e.reciprocal(out=a, in_=a), rd=D2.deps, wr=D2.deps)
                        tt("dve", D0.ap[0:64, :], D0.ap[0:64, :], D2.ap[0:64, :], ALU.mult, D0.deps + D2.deps, D0.deps)
                        ts("dve", D0.ap[0:64, :], D0.ap[0:64, :], pc(9), pc(10), ALU.mult, ALU.add, D0.deps + [d_rwp], D0.deps)
                        stt("dve", D1.ap[0:64, :], R.ap[0:64, :], pc(8), K.ap[0:64, :], ALU.mult, ALU.mult, R.deps + K.deps + D1.deps + [d_rwp], D1.deps)
                        for tb in range(NTB):
                            tsl = slice(tb * TB, (tb + 1) * TB)
                            bk, dbk_ = big()
                            mm(bk[0:64, :], ones32[0:64, 0:64], D1.ap[0:64, tsl], True, True, [d_o32] + D1.deps, [dbk_])
                            tt("dve", D2.ap[0:64, tsl], bk[0:64, :], V.ap[0:64, tsl], ALU.mult, [dbk_] + V.deps + D2.deps, D2.deps)
                        tt("dve", D0.ap[0:64, :], D0.ap[0:64, :], D2.ap[0:64, :], ALU.add, D0.deps + D2.deps, D0.deps)
                        s = ycount[0] % 2; ycount[0] += 1
                        for tb in range(NTB):
                            tsl = slice(tb * TB, (tb + 1) * TB)
                            bk, dbk_ = big()
                            mm(bk[0:64, :], rg2b[:, hs], GL[:, tsl], True, True, [d_rg2, d_GL], [dbk_])
                            tt("dve", yo[s][0:64, tsl], bk[0:64, :], D0.ap[0:64, tsl], ALU.mult, [dbk_] + D0.deps, [d_yo[s]])
                        dma("sp", ydv[512 + h * 64:512 + (h + 1) * 64, :], yo[s][0:64, :], [d_yo[s]], [d_yd], "b_yo%d" % s)
                    P.op("dve", lambda e: e.memset(scr[:], 0.0), rd=alld, wr=list(dbk))
                xTv2 = xT.rearrange("(c p) t -> p c t", p=128)
                for c in range(8):
                    dma("sp", x[:, c, :], xTv2[:, c, :], [], dx[c], "x%d" % c)
            with ExitStack() as ph:
                P.fence()
                yT = sb("yTm", [128, 8, S], BF16, ph); d_yT = [Dep() for _ in range(8)]
                yv = ydv.rearrange("(c p) t -> p c t", p=128)
                for c in range(8):
                    dma("sp", yT[:, c, :], yv[:, c, :], [d_yd], [d_yT[c]], "b_yl%d" % c)
                if "ydbg" in dbg:
                    yd = dbg["ydbg"].rearrange("(c p) t -> p c t", p=128)
                    for c in range(8):
                        out_dmas.append(dma("pool", yd[:, c, :], yT[:, c, :], [d_yT[c]], [], "dbg_y"))
                out_proj(ph, dr["ab_w_out"], yT, d_yT, gm_ap)


        def dump_x(name):
            if name in dbg:
                v = dbg[name].rearrange("(c p) t -> p c t", p=128)
                for c in range(8):
                    out_dmas.append(dma("sp", v[:, c, :], x[:, c, :], dx[c], [], "dbg_" + name))

        for layer in range(2):
            if layer == 1 and cfg.get("attn", True):
                attn_mixer(layer)
            if layer == 0 and cfg.get("ab", True):
                ab_mixer(layer)
            dump_x("xmix%d" % layer)
            if cfg.get("moe", True):
                moe(layer)
            dump_x("xffn%d" % layer)

        with ExitStack() as ph:
            P.fence()
            sqb = [sb("fsq%d" % i, [128, S], BF16, ph) for i in range(2)]; d_sq = [Dep(), Dep()]
            rstd = sb("frstd", [128, S], F32, ph); d_rstd = [Dep() for _ in range(NTB)]
            ot = [sb("fot%d" % i, [128, S], F32, ph) for i in range(2)]; d_ot = [Dep(), Dep()]
            for c in range(8):
                s = c % 2
                act(sqb[s][:], x[:, c, :], AF.Square, dx[c], [d_sq[s]])
                for tb in range(NTB):
                    mm(banks[tb][:, :], ones_bf[:], sqb[s][:, tb * TB:(tb + 1) * TB], c == 0, c == 7, [d_ones, d_sq[s]], [dbk[tb]])
            for tb in range(NTB):
                act(rstd[:, tb * TB:(tb + 1) * TB], banks[tb][:, :], AF.Sqrt, [dbk[tb], d_eps], [d_rstd[tb]], bias=epsc[:, 0:1], scale=1.0 / D)
                P.op("dve", lambda e, tb=tb: e.reciprocal(out=rstd[:, tb * TB:(tb + 1) * TB], in_=rstd[:, tb * TB:(tb + 1) * TB]),
                     rd=[d_rstd[tb]], wr=[d_rstd[tb]])
            ov = outT.rearrange("(c p) t -> p c t", p=128)
            for c in range(8):
                s = c % 2
                stt("dve", ot[s][:], x[:, c, :], ng[:, 32 + c:33 + c], rstd[:], ALU.mult, ALU.mult, dx[c] + d_rstd + [d_ng], [d_ot[s]])
                out_dmas.append(dma("sp", ov[:, c, :], ot[s][:], [d_ot[s]], [], "out%d" % s))
        P.emit(final_waits=out_dmas)
    return nc, P


def _bucket_onehot():
    jp = np.arange(4096)
    rel = 2047 - jp
    nb = 16
    max_exact = 8
    bucket = np.where(rel > 0, nb, 0)
    n = np.abs(rel)
    nf = np.maximum(n, 1).astype(np.float32)
    large = max_exact + (np.log(nf / max_exact) / np.float32(np.log(128 / max_exact)) * (nb - max_exact)).astype(np.int32)
    large = np.minimum(large, nb - 1)
    bucket = bucket + np.where(n < max_exact, n, large)
    oh = np.zeros((32, 4096), np.float32)
    oh[bucket, jp] = 1.0
    return oh


def host_inputs(inp, cfg={}):
    f32 = np.float32
    def colT(v, n):
        return np.ascontiguousarray(np.asarray(v, f32).reshape(n, 128).T)
    shared = {}
    shared["ada_w"] = np.ascontiguousarray(inp["ada_w"], f32)
    shared["ada_bT"] = np.concatenate([colT(inp["ada_b"][l], 48) for l in range(2)], axis=1)
    shared["ngT"] = np.concatenate([colT(inp["norm_mix_g"][0], 8), colT(inp["norm_mix_g"][1], 8),
                                    colT(inp["norm_ffn_g"][0], 8), colT(inp["norm_ffn_g"][1], 8),
                                    colT(inp["final_norm_g"], 8)], axis=1)
    rwl = []
    for l in range(2):
        r = np.asarray(inp["router_w"][l], f32).reshape(8, 128, NE).transpose(1, 0, 2).reshape(128, 8 * NE)
        rwl.append(r)
    shared["rw"] = np.ascontiguousarray(np.stack(rwl))
    shared["rb"] = np.ascontiguousarray(np.broadcast_to(np.asarray(inp["router_b"], f32)[:, None, :], (2, 128, NE)))
    w1 = np.asarray(inp["moe_w1"], f32)
    w1p = np.empty_like(w1)
    w1v = w1p.reshape(2, NE, D, 8, 256)
    w1v[..., 0:128] = w1[..., 0::2].reshape(2, NE, D, 8, 128)
    w1v[..., 128:256] = w1[..., 1::2].reshape(2, NE, D, 8, 128)
    shared["w1"] = np.ascontiguousarray(w1p[:, :cfg.get("nexp", NE)])
    b1 = np.asarray(inp["moe_b1"], f32)
    b1g = b1[..., 0::2].reshape(2, NE, 8, 128)
    b1l = b1[..., 1::2].reshape(2, NE, 8, 128)
    b1c = np.concatenate([b1g, b1l], axis=2)
    shared["b1T"] = np.ascontiguousarray(b1c.transpose(0, 3, 1, 2).reshape(2, 128, NE * 16))
    shared["w2"] = np.ascontiguousarray(np.asarray(inp["moe_w2"], f32)[:, :cfg.get("nexp", NE)])
    shared["b2"] = np.ascontiguousarray(inp["moe_b2"], f32)
    shared["ident"] = np.eye(128, dtype=f32)
    selm = np.zeros((NE, NE, 128), f32)
    for e in range(NE):
        selm[e, e, :] = 1.0
    shared["sel"] = selm.reshape(NE, NE * 128)
    shared["attn_w_in"] = np.ascontiguousarray(inp["attn_w_in"][0], f32)
    shared["attn_w_out"] = np.ascontiguousarray(inp["attn_w_out"][0], f32)
    shared["lamb"] = np.ascontiguousarray(np.broadcast_to(np.asarray(inp["attn_lambda"][0], f32).reshape(1, 256), (128, 256)))
    shared["sublnT"] = np.ascontiguousarray(np.asarray(inp["attn_subln_g"][0], f32).reshape(128, 1))
    tabv = np.asarray(inp["rel_bias_table"], f32)
    shared["reltab"] = np.ascontiguousarray(tabv)
    cf = np.stack([tabv[15, :], tabv[31, :]], axis=1).reshape(1, 16)
    shared["cfar"] = np.ascontiguousarray(np.broadcast_to(cf, (128, 16)))
    shared["onehot"] = _bucket_onehot()
    shared["antiident"] = np.ascontiguousarray(np.eye(128, dtype=f32)[::-1])
    shared["ab_w_in"] = np.ascontiguousarray(inp["ab_w_in"][0], f32)
    shared["ab_w_out"] = np.ascontiguousarray(inp["ab_w_out"][0], f32)
    lb = np.asarray(inp["hgrn_lb"], f32)
    shared["lbT"] = np.ascontiguousarray(lb.reshape(2, 2, 4, 128).transpose(3, 0, 1, 2).reshape(128, 16))
    shared["hngT"] = np.ascontiguousarray(np.asarray(inp["hgrn_norm_g"][0], f32).reshape(128, 1))
    mu = np.asarray(inp["rwkv_mu"][0], f32)
    items = [mu[0:512], mu[512:1024], mu[1024:1536], inp["rwkv_w0"][0, 0], inp["rwkv_w0"][0, 1], inp["rwkv_a0"][0],
             inp["rwkv_k_k"][0], inp["rwkv_k_a"][0], inp["rwkv_r_k"][0], inp["rwkv_ln_g"][0], inp["rwkv_ln_b"][0]]
    it = np.stack([np.asarray(v, f32).reshape(8, 64) for v in items])
    shared["rwp"] = np.ascontiguousarray(it.transpose(2, 1, 0).reshape(64, 88))
    r2 = np.zeros((128, 3), f32)
    r2[:, 0] = mu[1536:1664]; r2[0:64, 1] = mu[1664:1728]; r2[:, 2] = mu[1728:1856]
    shared["rwp2"] = r2
    shared["rw2"] = np.ascontiguousarray(np.asarray(inp["rwkv_w2"][0], f32).reshape(128, 512))
    shared["ra2"] = np.ascontiguousarray(inp["rwkv_a2"][0], f32)
    shared["rg2"] = np.ascontiguousarray(inp["rwkv_g2"][0], f32)
    idx = np.arange(128)
    same = (idx[:, None] // 32) == (idx[None, :] // 32)
    IU = (same & (idx[:, None] <= idx[None, :])).astype(f32)
    SU = (same & (idx[:, None] < idx[None, :])).astype(f32)
    CMm = np.zeros((128, 4, 128), f32)
    for c_ in range(4):
        CMm[c_ * 32:(c_ + 1) * 32, c_, :] = 1.0
    IEX = np.broadcast_to(np.eye(128, dtype=f32)[:, None, :], (128, 4, 128))
    shared["masks"] = np.ascontiguousarray(np.concatenate([IU, SU, IU.T, SU.T, CMm.reshape(128, 512), IEX.reshape(128, 512)], axis=1))
    maps = []
    for b in range(8):
        m = dict(shared)
        m["xT"] = np.ascontiguousarray(np.asarray(inp["x"][b], f32).T)
        m["cT"] = colT(inp["c"][b], 8)
        maps.append(m)
    return maps


from concourse.bass_utils import run_bass_kernel_spmd


def kernel(**inputs):
    cfg = {}
    nc, P = build(cfg)
    maps = host_inputs(inputs, cfg)
    res = run_bass_kernel_spmd(nc, maps, core_ids=list(range(8)))
    out = np.stack([np.ascontiguousarray(np.asarray(res.results[b]["outT"]).T) for b in range(8)])
    return out.astype(np.float32)
```
